# Optimizing a Trainium2 kernel written in Bass

```python
import math
import jax
import jax.numpy as jnp
from jax import lax

D_MODEL = 2048
BATCH = 1
SEQ = 8192
DEPTH = 4

GRID_W = 64
CTX_LEN = 256
MIX_W = D_MODEL // 2
CHUNK = 64
GLA_HEADS = 4
GLA_DV = MIX_W // GLA_HEADS
GLA_DK = GLA_DV // 2
GLA_LR = 16
GLA_GATE_NORM = 16.0
RWKV_HEAD = 64
RWKV_HEADS = MIX_W // RWKV_HEAD
DECAY_LORA = 64
ICLR_LORA = 64
GATE_LORA = 128
RWKV_GN_EPS = 64e-5
GDN_HEAD = 128
GDN_HEADS = MIX_W // GDN_HEAD
CONV_K = 3
N_EXPERTS = 16
N_GROUPS = 4
TOP_K = 2
D_EXPERT = 1408
DISPATCH_BLOCK = 256

GLA_COLS = (GLA_HEADS * GLA_DK, GLA_HEADS * GLA_DK, MIX_W, MIX_W, GLA_LR, GLA_LR)
RWKV_COLS = (MIX_W, MIX_W, MIX_W, DECAY_LORA, DECAY_LORA, ICLR_LORA, ICLR_LORA, GATE_LORA)
GDN_COLS = (3 * MIX_W, MIX_W, GDN_HEADS, GDN_HEADS, GDN_HEADS, GDN_HEADS)
GLA_PROJ = sum(GLA_COLS)
RWKV_PROJ = sum(RWKV_COLS)
GDN_PROJ = sum(GDN_COLS)
IN_COLS = (GLA_PROJ, RWKV_PROJ, GDN_PROJ, 3 * D_MODEL)
D_IN = sum(IN_COLS)

kernel_name = 'hybrid_gla_rwkv7_gdn_moe_flow_block'


def _split_cols(p, widths):
    out, s = [], 0
    for w in widths:
        out.append(p[..., s:s + w])
        s += w
    return out


def _layer_norm(x, g, b, eps=1e-5):
    xf = x.astype(jnp.float32)
    mu = xf.mean(-1, keepdims=True)
    var = jnp.square(xf - mu).mean(-1, keepdims=True)
    return ((xf - mu) * lax.rsqrt(var + eps)).astype(x.dtype) * g + b


def _rms_norm(x, w, eps=1e-6):
    xf = x.astype(jnp.float32)
    return (xf * lax.rsqrt(jnp.square(xf).mean(-1, keepdims=True) + eps)).astype(x.dtype) * w


def _l2_normalize(x, eps=1e-6):
    xf = x.astype(jnp.float32)
    return (xf * lax.rsqrt(jnp.square(xf).sum(-1, keepdims=True) + eps)).astype(x.dtype)


def _to_chunks(t):
    b, n = t.shape[:2]
    t = t.reshape((b, n // CHUNK, CHUNK) + t.shape[2:])
    return jnp.swapaxes(t, 2, 3)


def _from_chunks(t):
    t = jnp.swapaxes(t, 2, 3)
    return t.reshape((t.shape[0], t.shape[1] * t.shape[2]) + t.shape[3:])


def _token_shift(p):
    pad = jnp.pad(p, ((0, 0), (1, 1), (0, 0)))
    return 0.5 * (pad[:, :-2] + pad[:, 2:])


def _grid_conv(t, w):
    b, n, ch = t.shape
    rows = n // GRID_W
    y = lax.conv_general_dilated(t.reshape(b, rows, GRID_W, ch), w[:, :, None, :], (1, 1), 'SAME',
                                 dimension_numbers=('NHWC', 'HWIO', 'NHWC'), feature_group_count=ch)
    return y.reshape(b, n, ch)


def _seq_conv(t, w):
    return lax.conv_general_dilated(t, w[:, None, :], (1,), 'SAME',
                                    dimension_numbers=('NWC', 'WIO', 'NWC'), feature_group_count=t.shape[-1])


def _two_stage(scan_fn, ctx_args, lat_args, state0, reverse):
    if reverse:
        ctx_args = tuple(jnp.flip(t, axis=1) for t in ctx_args)
        lat_args = tuple(jnp.flip(t, axis=1) for t in lat_args)
    o_ctx, s_ctx = scan_fn(*ctx_args, state0)
    o_lat, _ = scan_fn(*lat_args, s_ctx)
    if reverse:
        o_ctx, o_lat = jnp.flip(o_ctx, axis=1), jnp.flip(o_lat, axis=1)
    return o_ctx, o_lat


def _gla_chunked(q, k, v, g, s0):
    dt = v.dtype
    q, k, v, g = (_to_chunks(t) for t in (q, k, v, g))
    b = jnp.cumsum(g.astype(jnp.float32), axis=3)
    b_ref = b[:, :, :, CHUNK // 2 - 1:CHUNK // 2]
    b_last = b[:, :, :, -1:]
    lower = jnp.tril(jnp.ones((CHUNK, CHUNK), bool))
    q_rel = q * jnp.exp(b - b_ref).astype(dt)
    k_rel = k * jnp.exp(b_ref - b).astype(dt)
    att = jnp.where(lower, jnp.einsum('bnhid,bnhjd->bnhij', q_rel, k_rel), 0)
    o_intra = jnp.einsum('bnhij,bnhjv->bnhiv', att, v)
    k_end = k * jnp.exp(b_last - b).astype(dt)
    ds = jnp.einsum('bnhjd,bnhjv->bnhdv', k_end, v)
    dec = jnp.exp(b_last[:, :, :, 0]).astype(dt)

    def step(s, inp):
        dec_n, ds_n = inp
        return dec_n[..., None] * s + ds_n, s

    s_fin, s_in = lax.scan(step, s0, (jnp.moveaxis(dec, 1, 0), jnp.moveaxis(ds, 1, 0)))
    o = o_intra + jnp.einsum('bnhid,bnhdv->bnhiv', q * jnp.exp(b).astype(dt), jnp.moveaxis(s_in, 0, 1))
    return _from_chunks(o), s_fin


def _gdn_chunked(q, k, v, log_a, beta, s0):
    dt = v.dtype
    f32 = jnp.float32
    q, k, v, log_a, beta = (_to_chunks(t.astype(f32)) for t in (q, k, v, log_a, beta))
    gam = jnp.cumsum(log_a, axis=-1)
    incl = jnp.tril(jnp.ones((CHUNK, CHUNK), bool))
    strict = jnp.tril(jnp.ones((CHUNK, CHUNK), bool), -1)
    decay = jnp.exp(jnp.where(incl, gam[..., :, None] - gam[..., None, :], -jnp.inf))
    a_mat = jnp.where(strict, beta[..., :, None] * jnp.einsum('bnhid,bnhjd->bnhij', k, k) * decay, 0.0)
    rhs = jnp.concatenate([v * beta[..., None], k * (beta * jnp.exp(gam))[..., None]], axis=-1)
    sol = lax.linalg.triangular_solve(a_mat, rhs, left_side=True, lower=True, unit_diagonal=True)
    dv = v.shape[-1]
    u, w = sol[..., :dv], sol[..., dv:]
    qk = jnp.einsum('bnhid,bnhjd->bnhij', q, k) * decay
    q_dec = q * jnp.exp(gam)[..., None]
    k_end = k * jnp.exp(gam[..., -1:] - gam)[..., None]
    dec = jnp.exp(gam[..., -1])

    def step(s, inp):
        u_n, w_n, qk_n, q_n, k_n, dec_n = inp
        delta = u_n - jnp.einsum('bhcd,bhdv->bhcv', w_n, s)
        o_n = jnp.einsum('bhcd,bhdv->bhcv', q_n, s) + jnp.einsum('bhij,bhjv->bhiv', qk_n, delta)
        s = dec_n[..., None, None] * s + jnp.einsum('bhcd,bhcv->bhdv', k_n, delta)
        return s, o_n

    xs = tuple(jnp.moveaxis(t, 1, 0) for t in (u, w, qk, q_dec, k_end, dec))
    s_fin, o = lax.scan(step, s0, xs)
    return _from_chunks(jnp.moveaxis(o, 0, 1)).astype(dt), s_fin


def _rwkv7_scan(r, w, k, v, kk, b, s0):
    def step(s, inp):
        r_t, w_t, k_t, v_t, kk_t, b_t = inp
        sa = jnp.einsum('bhvk,bhk->bhv', s, kk_t)
        s = s * w_t[:, :, None, :] - sa[..., None] * b_t[:, :, None, :] + v_t[..., None] * k_t[:, :, None, :]
        return s, jnp.einsum('bhvk,bhk->bhv', s, r_t)

    s_fin, y = lax.scan(step, s0, tuple(jnp.moveaxis(t, 1, 0) for t in (r, w, k, v, kk, b)))
    return jnp.moveaxis(y, 0, 1), s_fin


def _gla_mixer(pc, pl, dec_w2, dec_b, norm_w):
    def prep(p):
        bsz, n = p.shape[:2]
        q, k, v, og, lr_f, lr_b = _split_cols(p, GLA_COLS)
        heads = lambda t, d: t.reshape(bsz, n, GLA_HEADS, d)
        g = [heads(jax.nn.log_sigmoid((lr @ dec_w2[d] + dec_b[d]).astype(jnp.float32)) / GLA_GATE_NORM, GLA_DK)
             for d, lr in enumerate((lr_f, lr_b))]
        return heads(q, GLA_DK) * GLA_DK ** -0.5, heads(k, GLA_DK), heads(v, GLA_DV), og, g

    qc, kc, vc, ogc, gc = prep(pc)
    ql, kl, vl, ogl, gl = prep(pl)
    s0 = jnp.zeros((pc.shape[0], GLA_HEADS, GLA_DK, GLA_DV), pc.dtype)
    cf, lf = _two_stage(_gla_chunked, (qc, kc, vc, gc[0]), (ql, kl, vl, gl[0]), s0, False)
    cb, lb = _two_stage(_gla_chunked, (qc, kc, vc, gc[1]), (ql, kl, vl, gl[1]), s0, True)

    def finish(o, og):
        return _rms_norm(o, norm_w).reshape(og.shape) * jax.nn.silu(og)

    return finish(cf + cb, ogc), finish(lf + lb, ogl)


def _rwkv_mixer(pc, pl, mu, w2, w0, a2, a0, g2, k_k, k_a, r_k, ln_w, ln_b):
    def prep(p):
        bsz, n = p.shape[:2]
        heads = lambda t: t.reshape(bsz, n, RWKV_HEADS, RWKV_HEAD)
        p = p + (_token_shift(p) - p) * mu
        r, k, v, wl_f, wl_b, al_f, al_b, gl = _split_cols(p, RWKV_COLS)
        kk = _l2_normalize(heads(k * k_k))
        per_dir = []
        for d, (wl, al) in enumerate(((wl_f, al_f), (wl_b, al_b))):
            z = w0[d] + jnp.tanh(wl) @ w2[d]
            decay = jnp.exp(-jnp.exp(-jax.nn.softplus(-z) - 0.5))
            a = jax.nn.sigmoid(a0[d] + al @ a2[d])
            per_dir.append((heads(decay), heads(k * (1 + (a - 1) * k_a)), kk * heads(a)))
        g = jax.nn.sigmoid(gl) @ g2
        return heads(r), heads(k), heads(v), kk, per_dir, g

    rc, kc, vc, kkc, dc, gc = prep(pc)
    rl, kl, vl, kkl, dl, gl = prep(pl)
    s0 = jnp.zeros((pc.shape[0], RWKV_HEADS, RWKV_HEAD, RWKV_HEAD), pc.dtype)
    cf, lf = _two_stage(_rwkv7_scan, (rc, dc[0][0], dc[0][1], vc, kkc, dc[0][2]),
                        (rl, dl[0][0], dl[0][1], vl, kkl, dl[0][2]), s0, False)
    cb, lb = _two_stage(_rwkv7_scan, (rc, dc[1][0], dc[1][1], vc, kkc, dc[1][2]),
                        (rl, dl[1][0], dl[1][1], vl, kkl, dl[1][2]), s0, True)

    def finish(y, r, k, v, g):
        yf = y.astype(jnp.float32)
        m = yf.mean(-1, keepdims=True)
        var = jnp.square(yf - m).mean(-1, keepdims=True)
        y = ((yf - m) * lax.rsqrt(var + RWKV_GN_EPS)).astype(y.dtype)
        y = y * ln_w.reshape(RWKV_HEADS, RWKV_HEAD) + ln_b.reshape(RWKV_HEADS, RWKV_HEAD)
        y = y + jnp.sum(r * k * r_k, axis=-1, keepdims=True) * v
        return y.reshape(g.shape) * g

    return finish(cf + cb, rc, kc, vc, gc), finish(lf + lb, rl, kl, vl, gl)


def _gdn_mixer(pc, pl, conv_w, a_log, dt_bias, norm_w):
    def prep(p, on_grid):
        bsz, n = p.shape[:2]
        heads = lambda t: t.reshape(bsz, n, GDN_HEADS, GDN_HEAD)
        qkv, z, a_f, a_b, b_f, b_b = _split_cols(p, GDN_COLS)
        qkv = jax.nn.silu(_grid_conv(qkv, conv_w) if on_grid else _seq_conv(qkv, conv_w[CONV_K // 2]))
        q, k, v = jnp.split(qkv, 3, axis=-1)
        per_dir = [(-jnp.exp(a_log[d]) * jax.nn.softplus(a + dt_bias[d]), jax.nn.sigmoid(b))
                   for d, (a, b) in enumerate(((a_f, b_f), (a_b, b_b)))]
        return _l2_normalize(heads(q)) * GDN_HEAD ** -0.5, _l2_normalize(heads(k)), heads(v), z, per_dir

    qc, kc, vc, zc, dc = prep(pc, False)
    ql, kl, vl, zl, dl = prep(pl, True)
    s0 = jnp.zeros((pc.shape[0], GDN_HEADS, GDN_HEAD, GDN_HEAD), jnp.float32)
    cf, lf = _two_stage(_gdn_chunked, (qc, kc, vc) + dc[0], (ql, kl, vl) + dl[0], s0, False)
    cb, lb = _two_stage(_gdn_chunked, (qc, kc, vc) + dc[1], (ql, kl, vl) + dl[1], s0, True)

    def finish(o, z):
        return _rms_norm(o, norm_w).reshape(z.shape) * jax.nn.silu(z)

    return finish(cf + cb, zc), finish(lf + lb, zl)


def _merge(gate_pre, outs, w_branch, w_out):
    gates = jax.nn.sigmoid(gate_pre)
    y = 0
    for m, o in enumerate(outs):
        y = y + gates[..., m * D_MODEL:(m + 1) * D_MODEL] * (o @ w_branch[m])
    return y @ w_out


def _moe_ffn(h, router_w, router_bias, w1, w3, w2):
    n_tok, d = h.shape
    scores = jax.nn.sigmoid((h @ router_w).astype(jnp.float32))
    sel = scores + router_bias.astype(jnp.float32)
    grp_top = lax.top_k(sel.reshape(n_tok, N_GROUPS, N_EXPERTS // N_GROUPS), 2)[0]
    best_group = jnp.argmax(grp_top.sum(-1), axis=-1)
    in_group = (jnp.arange(N_EXPERTS) // (N_EXPERTS // N_GROUPS))[None, :] == best_group[:, None]
    _, top_e = lax.top_k(jnp.where(in_group, sel, -jnp.inf), TOP_K)
    top_s = jnp.take_along_axis(scores, top_e, axis=-1)
    top_w = (top_s / top_s.sum(-1, keepdims=True)).astype(h.dtype)
    n_asg = n_tok * TOP_K
    flat_e = top_e.reshape(-1)
    order = jnp.argsort(flat_e)
    sorted_e = flat_e[order]
    counts = jnp.bincount(flat_e, length=N_EXPERTS)
    padded = (counts + DISPATCH_BLOCK - 1) // DISPATCH_BLOCK * DISPATCH_BLOCK
    pad_end = jnp.cumsum(padded)
    first = jnp.cumsum(counts) - counts
    dest = (pad_end - padded)[sorted_e] + jnp.arange(n_asg) - first[sorted_e]
    n_blk = -(-n_asg // DISPATCH_BLOCK) + N_EXPERTS
    tok = jnp.repeat(jnp.arange(n_tok, dtype=jnp.int32), TOP_K)
    slot_tok = jnp.full((n_blk * DISPATCH_BLOCK,), n_tok, jnp.int32).at[dest].set(tok[order])
    slot_w = jnp.zeros((n_blk * DISPATCH_BLOCK,), h.dtype).at[dest].set(top_w.reshape(-1)[order])
    blk_e = jnp.minimum(jnp.searchsorted(pad_end, jnp.arange(n_blk) * DISPATCH_BLOCK, side='right'), N_EXPERTS - 1)
    h_pad = jnp.concatenate([h, jnp.zeros((1, d), h.dtype)], axis=0)
    xb = h_pad[slot_tok].reshape(n_blk, DISPATCH_BLOCK, d)

    def expert_block(args):
        xe, e = args
        return (jax.nn.silu(xe @ w1[e]) * (xe @ w3[e])) @ w2[e]

    yb = lax.map(expert_block, (xb, blk_e))
    y = jnp.zeros((n_tok + 1, d), h.dtype).at[slot_tok].add(yb.reshape(-1, d) * slot_w[:, None])
    return y[:n_tok]


def setup_inputs(seed: int = 0) -> dict:
    key = jax.random.key(seed)
    keys = iter(jax.random.split(key, 48))

    def nrm(shape, scale):
        return jax.random.normal(next(keys), shape, jnp.float32) * scale

    def unif(shape, lo, hi):
        return jax.random.uniform(next(keys), shape, jnp.float32, lo, hi)

    D, L = D_MODEL, DEPTH
    beta = (8.0 * DEPTH) ** -0.25
    dt = jnp.exp(unif((L, 2, GDN_HEADS), math.log(1e-3), math.log(1e-1)))
    return {
        'x': nrm((BATCH, SEQ, D), 1.0),
        'c': nrm((BATCH, D), 1.0),
        'ctx': nrm((BATCH, CTX_LEN, D), 1.0),
        'c_ctx': nrm((D,), 1.0),
        'w_ada': nrm((L, D, 6 * D), 0.5 * D ** -0.5),
        'b_ada': nrm((L, 6 * D), 0.01),
        'w_in': nrm((L, D, D_IN), D ** -0.5),
        'b_in': nrm((L, D_IN), 0.01),
        'gla_dec_w': nrm((L, 2, GLA_LR, GLA_HEADS * GLA_DK), GLA_LR ** -0.5),
        'gla_dec_b': 1.0 + nrm((L, 2, GLA_HEADS * GLA_DK), 0.1),
        'gla_norm_w': 1.0 + nrm((L, GLA_DV), 0.02),
        'rwkv_mu': unif((L, RWKV_PROJ), 0.2, 0.8),
        'rwkv_w2': nrm((L, 2, DECAY_LORA, MIX_W), 0.1 * DECAY_LORA ** -0.5),
        'rwkv_w0': -2.0 + nrm((L, 2, MIX_W), 0.5),
        'rwkv_a2': nrm((L, 2, ICLR_LORA, MIX_W), 0.1 * ICLR_LORA ** -0.5),
        'rwkv_a0': nrm((L, 2, MIX_W), 0.1),
        'rwkv_g2': nrm((L, GATE_LORA, MIX_W), GATE_LORA ** -0.5),
        'rwkv_kk': 0.85 + nrm((L, MIX_W), 0.02),
        'rwkv_ka': 1.0 + nrm((L, MIX_W), 0.02),
        'rwkv_rk': nrm((L, RWKV_HEADS, RWKV_HEAD), 0.1),
        'rwkv_ln_w': 1.0 + nrm((L, MIX_W), 0.02),
        'rwkv_ln_b': nrm((L, MIX_W), 0.01),
        'gdn_conv_w': nrm((L, CONV_K, CONV_K, 3 * MIX_W), 1.0 / CONV_K),
        'gdn_a_log': jnp.log(unif((L, 2, GDN_HEADS), 1.0, 16.0)),
        'gdn_dt_bias': dt + jnp.log(-jnp.expm1(-dt)),
        'gdn_norm_w': 1.0 + nrm((L, GDN_HEAD), 0.02),
        'w_branch': nrm((L, 3, MIX_W, D), MIX_W ** -0.5),
        'w_out': nrm((L, D, D), beta * D ** -0.5),
        'ln1_g': 1.0 + nrm((L, D), 0.02),
        'ln1_b': nrm((L, D), 0.01),
        'router_w': nrm((D, N_EXPERTS), D ** -0.5),
        'router_bias': nrm((N_EXPERTS,), 0.01),
        'moe_w1': nrm((L, N_EXPERTS, D, D_EXPERT), D ** -0.5),
        'moe_w3': nrm((L, N_EXPERTS, D, D_EXPERT), D ** -0.5),
        'moe_w2': nrm((L, N_EXPERTS, D_EXPERT, D), beta * D_EXPERT ** -0.5),
        'ln2_g': 1.0 + nrm((L, D), 0.02),
        'ln2_b': nrm((L, D), 0.01),
    }


def reference(x, c, ctx, c_ctx, w_ada, b_ada, w_in, b_in,
              gla_dec_w, gla_dec_b, gla_norm_w,
              rwkv_mu, rwkv_w2, rwkv_w0, rwkv_a2, rwkv_a0, rwkv_g2, rwkv_kk, rwkv_ka, rwkv_rk,
              rwkv_ln_w, rwkv_ln_b,
              gdn_conv_w, gdn_a_log, gdn_dt_bias, gdn_norm_w,
              w_branch, w_out, ln1_g, ln1_b,
              router_w, router_bias, moe_w1, moe_w3, moe_w2, ln2_g, ln2_b):
    alpha = (2.0 * DEPTH) ** 0.25
    bsz, n_lat, d = x.shape
    n_ctx = ctx.shape[1]
    silu_c = jax.nn.silu(c)[:, None, :]
    silu_cc = jax.nn.silu(c_ctx)
    xl, xc = x, ctx
    for l in range(DEPTH):
        last = l == DEPTH - 1
        mod_l = jnp.split(silu_c @ w_ada[l] + b_ada[l], 6, axis=-1)
        mod_c = jnp.split(silu_cc @ w_ada[l] + b_ada[l], 6, axis=-1)
        hl = xl * (1 + mod_l[1]) + mod_l[0]
        hc = xc * (1 + mod_c[1]) + mod_c[0]
        pl = hl @ w_in[l] + b_in[l]
        pc = hc @ w_in[l] + b_in[l]
        gla_c, rwkv_c, gdn_c, gate_c = _split_cols(pc, IN_COLS)
        gla_l, rwkv_l, gdn_l, gate_l = _split_cols(pl, IN_COLS)
        oa_c, oa_l = _gla_mixer(gla_c, gla_l, gla_dec_w[l], gla_dec_b[l], gla_norm_w[l])
        ob_c, ob_l = _rwkv_mixer(rwkv_c, rwkv_l, rwkv_mu[l], rwkv_w2[l], rwkv_w0[l], rwkv_a2[l], rwkv_a0[l],
                                 rwkv_g2[l], rwkv_kk[l], rwkv_ka[l], rwkv_rk[l], rwkv_ln_w[l], rwkv_ln_b[l])
        oc_c, oc_l = _gdn_mixer(gdn_c, gdn_l, gdn_conv_w[l], gdn_a_log[l], gdn_dt_bias[l], gdn_norm_w[l])
        mix_l = _merge(gate_l, (oa_l, ob_l, oc_l), w_branch[l], w_out[l])
        xl = _layer_norm(alpha * xl + mod_l[2] * mix_l, ln1_g[l], ln1_b[l])
        h2l = xl * (1 + mod_l[4]) + mod_l[3]
        if last:
            yl = _moe_ffn(h2l.reshape(-1, d), router_w, router_bias,
                          moe_w1[l], moe_w3[l], moe_w2[l]).reshape(xl.shape)
        else:
            mix_c = _merge(gate_c, (oa_c, ob_c, oc_c), w_branch[l], w_out[l])
            xc = _layer_norm(alpha * xc + mod_c[2] * mix_c, ln1_g[l], ln1_b[l])
            h2c = xc * (1 + mod_c[4]) + mod_c[3]
            y = _moe_ffn(jnp.concatenate([h2c, h2l], axis=1).reshape(-1, d), router_w, router_bias,
                         moe_w1[l], moe_w3[l], moe_w2[l]).reshape(bsz, n_ctx + n_lat, d)
            xc = _layer_norm(alpha * xc + mod_c[5] * y[:, :n_ctx], ln2_g[l], ln2_b[l])
            yl = y[:, n_ctx:]
        xl = _layer_norm(alpha * xl + mod_l[5] * yl, ln2_g[l], ln2_b[l])
    return xl
```

```python
import numpy as np
from contextlib import ExitStack
import concourse.bass as bass
import concourse.mybir as mybir
from concourse.bass_utils import run_bass_kernel_spmd

F32 = mybir.dt.float32
BF16 = mybir.dt.bfloat16
I32 = mybir.dt.int32
AF = mybir.ActivationFunctionType
ALU = mybir.AluOpType

P = 128
MASKS = ['BD', 'LINC0', 'LINC1', 'LSTR0', 'LSTR1', 'MREL0', 'MREL1', 'MEND0', 'MEND1', 'NEG0', 'NEG1', 'NEGI0', 'NEGI1']
NMASK = len(MASKS)
MI = {k: i for i, k in enumerate(MASKS)}


class Cfg:
    def __init__(self, n_ctx=256, n_lat=8192, depth=4, d_expert=1408, grid_w=64, dblk=512):
        self.D = 2048
        self.n_ctx, self.n_lat, self.depth = n_ctx, n_lat, depth
        self.n_tok = n_ctx + n_lat
        self.grid_w = grid_w
        self.MIX = 1024
        self.d_expert = d_expert
        self.n_exp = 16
        self.dblk = dblk
        self.gla_off = 0
        self.rwkv_off = 3104
        self.gdn_off = 3104 + 3456
        self.gate_off = 3104 + 3456 + 4128
        self.D_IN = self.gate_off + 3 * self.D
        self.n_asg = 2 * self.n_tok
        self.n_blk = -(-self.n_asg // dblk) + self.n_exp
        self.n_slot = self.n_blk * dblk


class Buf:
    def __init__(self, name):
        self.name = name
        self.w = {}
        self.wf = {}
        self.r = {}


class T:
    def __init__(self, t, name):
        self.t = t
        self.b = Buf(name)

    def __getitem__(self, idx):
        return self.t[idx]


class BankT(T):
    def __init__(self, t, bank, name):
        self.t = t
        self.bank = bank
        self.b = Buf(name)

    def __getitem__(self, idx):
        if not isinstance(idx, tuple):
            idx = (idx, slice(None))
        return self.t[(idx[0], self.bank) + tuple(idx[1:])]


class SegT:
    def __init__(self, segs, name):
        self.segs = segs
        self.b = Buf(name)

    def __getitem__(self, idx):
        rs, cs = idx
        for (r0, r1, t) in self.segs:
            if rs.start >= r0 and rs.stop <= r1:
                return t[rs.start - r0:rs.stop - r0, cs]
        raise ValueError("row range %s straddles DRAM segments" % (rs,))

    def pieces(self, a, b_):
        out = []
        for (r0, r1, t) in self.segs:
            lo, hi = max(a, r0), min(b_, r1)
            if lo < hi:
                out.append((lo, hi))
        return out


def _merge(d, s):
    for k, v in s.items():
        if d.get(k, 0) < v:
            d[k] = v


class KB:
    EPOCH = 30000
    RING = 8

    def __init__(self, nc, es):
        self.nc, self.es = nc, es
        self.eng = {"pe": nc.tensor, "act": nc.scalar, "dve": nc.vector, "pool": nc.gpsimd, "sp": nc.sync}
        self.sems = {}
        self.cur = {}
        self.seen = {e: {} for e in self.eng}
        self.nsem = 0
        self.ring = {}
        self.ring_i = {}
        self.ninst = 0
        self.bc_regs = {}
        for q in ("sp", "pool", "act"):
            self.ring[q] = [[self._newsem("dq_%s" % q), 0] for _ in range(self.RING)]
            self.ring_i[q] = 0
        for e in ("pe", "act", "dve", "pool"):
            self.cur[e] = [self._newsem("pg_%s" % e), 0]

    def _newsem(self, base):
        key = "%s_%d" % (base, self.nsem)
        self.nsem += 1
        self.sems[key] = self.es.enter_context(self.nc.semaphore(key))
        return key

    def _wait(self, e, deps):
        seen = self.seen[e]
        h = self.eng[e]
        for k, v in deps.items():
            if v <= 0:
                continue
            if e == "pe" and k.startswith("pg_pe"):
                continue
            if seen.get(k, 0) < v:
                h.wait_ge(self.sems[k], v)
                seen[k] = v
                self.ninst += 1

    def _deps(self, reads, writes, pwrites):
        deps = {}
        for b in reads:
            _merge(deps, b.b.w)
            if isinstance(b, BankT):
                _merge(deps, b.b.r)
        for b in writes:
            _merge(deps, b.b.w)
            _merge(deps, b.b.r)
        for b in pwrites:
            _merge(deps, b.b.r)
            _merge(deps, b.b.wf)
        return deps

    def _post(self, key, val, reads, writes, pwrites):
        for b in reads:
            if b.b.r.get(key, 0) < val:
                b.b.r[key] = val
        for b in writes:
            b.b.w = {key: val}
            b.b.wf = {key: val}
            b.b.r = {}
        for b in pwrites:
            if b.b.w.get(key, 0) < val:
                b.b.w[key] = val

    def op(self, e, fn, reads=(), writes=(), pwrites=()):
        deps = self._deps(reads, writes, pwrites)
        self._wait(e, deps)
        c = self.cur[e]
        if c[1] >= self.EPOCH:
            c = self.cur[e] = [self._newsem("pg_%s" % e), 0]
        ins = fn(self.eng[e])
        c[1] += 1
        ins.then_inc(self.sems[c[0]], 1)
        self.ninst += 1
        self._post(c[0], c[1], reads, writes, pwrites)

    def dma(self, q, out, in_, reads=(), writes=(), pwrites=(), **kw):
        deps = self._deps(reads, writes, pwrites)
        i = self.ring_i[q]
        self.ring_i[q] = (i + 1) % self.RING
        slot = self.ring[q][i]
        deps2 = dict(deps)
        if deps2.get(slot[0], 0) < slot[1]:
            deps2[slot[0]] = slot[1]
        self._wait(q, deps2)
        ins = self.eng[q].dma_start(out=out, in_=in_, **kw)
        slot[1] += 16
        ins.then_inc(self.sems[slot[0]], 16)
        self.ninst += 1
        self._post(slot[0], slot[1], reads, writes, pwrites)

    def fence_pe(self):
        c = self.cur["pe"]
        if c[1] > 0:
            self.eng["pe"].wait_ge(self.sems[c[0]], c[1])
            self.ninst += 1

    def idma(self, out, out_off, in_, in_off, nrows, reads=(), writes=(), pwrites=()):
        q = "pool"
        deps = self._deps(reads, writes, pwrites)
        i = self.ring_i[q]
        self.ring_i[q] = (i + 1) % self.RING
        slot = self.ring[q][i]
        if deps.get(slot[0], 0) < slot[1]:
            deps[slot[0]] = slot[1]
        self._wait(q, deps)
        if nrows not in self.bc_regs:
            regs = self.nc.alloc_registers("bc%d" % nrows, engines=[mybir.EngineType.Pool])
            self.nc.regs_mov(regs, nrows - 1)
            self.bc_regs[nrows] = regs[mybir.EngineType.Pool]
        ins = self.nc.gpsimd.indirect_dma_start(out=out, out_offset=out_off, in_=in_, in_offset=in_off,
                                                bounds_check=self.bc_regs[nrows], oob_is_err=False)
        slot[1] += 16
        ins.then_inc(self.sems[slot[0]], 16)
        self.ninst += 1
        self._post(slot[0], slot[1], reads, writes, pwrites)

    def barrier(self):
        deps = {}
        for e, c in self.cur.items():
            deps[c[0]] = c[1]
        for q, r in self.ring.items():
            for s in r:
                deps[s[0]] = s[1]
        for e in self.eng:
            self._wait(e, deps)

    def finish(self, engines=("sp", "pool", "act")):
        deps = {}
        for q, r in self.ring.items():
            for s in r:
                deps[s[0]] = s[1]
        for e in engines:
            self._wait(e, deps)

    def sb(self, st, name, shape, dt=F32):
        self.uid = getattr(self, "uid", 0) + 1
        name = "%s_u%d" % (name, self.uid)
        return T(st.enter_context(self.nc.sbuf_tensor(name, list(shape), dt)), name)

    def dram(self, name, shape, dt=F32, kind="Internal"):
        return T(self.nc.dram_tensor(name, list(shape), dt, kind=kind), name)


def layer_norm(kb, x_t, st_t, mv_t, gam, bet, eps):
    for c in range(4):
        kb.op("dve", lambda e, c=c: e.bn_stats(st_t[:, c, :], x_t[:, c * 512:(c + 1) * 512]),
              reads=[x_t], writes=[st_t] if c == 0 else (), pwrites=() if c == 0 else [st_t])
    kb.op("dve", lambda e: e.bn_aggr(mv_t[:, 0:2], st_t[:].rearrange("p c s -> p (c s)")), reads=[st_t], writes=[mv_t])
    kb.op("dve", lambda e: e.tensor_scalar(mv_t[:, 2:3], mv_t[:, 1:2], eps, None, ALU.add), reads=[mv_t], pwrites=[mv_t])
    kb.op("act", lambda e: e.activation(out=mv_t[:, 3:4], in_=mv_t[:, 2:3], func=AF.Sqrt), reads=[mv_t], pwrites=[mv_t])
    kb.op("dve", lambda e: e.reciprocal(mv_t[:, 2:3], mv_t[:, 3:4]), reads=[mv_t], pwrites=[mv_t])
    kb.op("dve", lambda e: e.tensor_scalar(x_t[:], x_t[:], mv_t[:, 0:1], mv_t[:, 2:3], ALU.subtract, ALU.mult),
          reads=[x_t, mv_t], writes=[x_t])
    kb.op("pool", lambda e: e.tensor_tensor(x_t[:], x_t[:], gam[:], ALU.mult), reads=[x_t, gam], writes=[x_t])
    kb.op("pool", lambda e: e.tensor_tensor(x_t[:], x_t[:], bet[:], ALU.add), reads=[x_t, bet], writes=[x_t])

def build_program(cfg, phases=("mod", "inproj"), dbg=(), ext_in=()):
    nc = bass.Bass("TRN2", target_bir_lowering=False)
    es = ExitStack()
    kb = KB(nc, es)
    D, n = cfg.D, cfg.n_tok
    L = cfg.depth

    def din(name, shape, dt=F32, ph=None):
        if ph is not None and not any(p_ in phases for p_ in ph):
            return None
        return T(nc.dram_tensor(name, list(shape), dt, kind="ExternalInput"), name)

    def dscr(name, shape, dt=F32):
        kind = "ExternalOutput" if name in dbg else ("ExternalInput" if name in ext_in else "Internal")
        return T(nc.dram_tensor(name, list(shape), dt, kind=kind), name)

    xin = din("xin", [n, D], ph=("inproj", "merge"))
    ccT = din("ccT", [D, 2], ph=("mod",))
    w_ada = din("w_ada", [L, D, 6 * D], ph=("mod",))
    b_ada = din("b_ada", [L, 1, 6 * D], ph=("mod",))
    w_in = din("w_in", [L, D, cfg.D_IN], ph=("inproj",))
    b_inT = din("b_inT", [L, P, 132], ph=("inproj",))

    w_branch = din("w_branch", [L, 3, cfg.MIX, D], ph=("merge",))
    w_out = din("w_out", [L, D, D], ph=("merge",))
    ln1_g = din("ln1_g", [L, 1, D], ph=("merge",))
    ln1_b = din("ln1_b", [L, 1, D], ph=("merge",))
    ln2_g = din("ln2_g", [L, 1, D], ph=("moe",))
    ln2_b = din("ln2_b", [L, 1, D], ph=("moe",))
    cfg_alpha = float((2.0 * 4) ** 0.25)
    router_w = din("router_w", [D, 16], ph=("moe",))
    router_bias = din("router_bias", [1, 16], ph=("moe",))
    moe_w1 = din("moe_w1", [L * 16 * D, cfg.d_expert], ph=("moe",))
    moe_w3 = din("moe_w3", [L * 16 * D, cfg.d_expert], ph=("moe",))
    moe_w2 = din("moe_w2", [L * 16 * cfg.d_expert * 4, 512], ph=("moe",))
    c_ut16 = din("c_ut16", [16, 16], ph=("moe",))
    c_bbrow = din("c_bbrow", [16, cfg.n_blk], ph=("moe",))
    c_idb = din("c_idb", [P, 32], ph=("moe",))
    per_layer = getattr(cfg, "per_layer", False)
    yout = T(nc.dram_tensor("yout", [cfg.n_lat, D], F32, kind="ExternalOutput"), "yout") if ("moe" in phases and not per_layer) else None
    MIXP = ("gla", "gdn", "rwkv")
    c_masks = din("c_masks", [NMASK, P, P], ph=MIXP)
    c_bdcol = din("c_bdcol", [P, 2], ph=MIXP)
    gla_dec_w = din("gla_dec_w", [L, 2, 16, 512], ph=("gla",))
    gla_dec_b = din("gla_dec_b", [L, 2, 1, 512], ph=("gla",))
    gla_norm_wT = din("gla_norm_wT", [L, P, 2], ph=("gla",))
    rwkv_muT = din("rwkv_muT", [L, P, 27], ph=("rwkv",))
    rwkv_w2 = din("rwkv_w2", [L, 2, 64, 1024], ph=("rwkv",))
    rwkv_a2 = din("rwkv_a2", [L, 2, 64, 1024], ph=("rwkv",))
    rwkv_g2 = din("rwkv_g2", [L, P, 1024], ph=("rwkv",))
    rwkv_p4T = din("rwkv_p4T", [L, P, 4, 8], ph=("rwkv",))
    rwkv_w0a0T = din("rwkv_w0a0T", [L, P, 2, 2, 8], ph=("rwkv",))
    rwkv_lnT = din("rwkv_lnT", [L, P, 2, 8], ph=("rwkv",))
    gdn_conv_wT = din("gdn_conv_wT", [L, 24, P, 9], ph=("gdn",))
    gdn_a_log = din("gdn_a_log", [L, 1, 16], ph=("gdn",))
    gdn_dt_bias = din("gdn_dt_bias", [L, 1, 16], ph=("gdn",))
    gdn_norm_wT = din("gdn_norm_wT", [L, P, 1], ph=("gdn",))
    MODD = dscr("MODD", [2, 6 * D])
    if "PT" in dbg or "PT" in ext_in:
        PT = dscr("PT", [cfg.D_IN, n])
    else:
        bnd = [0, cfg.gdn_off, cfg.gate_off, cfg.D_IN]
        PT = SegT([(bnd[i], bnd[i + 1], nc.dram_tensor("PT%d" % i, [bnd[i + 1] - bnd[i], n], F32, kind="Internal")) for i in range(3)], "PT")
    OT = dscr("OT", [3 * cfg.MIX, n], BF16)
    ACC = dscr("ACC", [D, n], BF16)
    OFG = dscr("OFG", [cfg.MIX, n])
    GC = dscr("GC", [3 * cfg.MIX, n])
    RW = {k_: dscr("RW_" + k_, [cfg.MIX, n]) for k_ in ("R", "V", "KK", "G", "BON", "LW0", "LW1", "KH0", "KH1", "BB0", "BB1")}
    OFR = dscr("OFR", [n, cfg.MIX])
    OFD = dscr("OFD", [n, cfg.MIX])
    XR = dscr("XR", [n, D])
    XM = dscr("XM", [n, D])
    H2 = dscr("H2", [n, D])
    EOFF = dscr("EOFF", [P, cfg.n_blk])
    SLOT = dscr("SLOT", [n // P, 2, P, 1], I32)
    W12D = dscr("W12D", [P, n // P, 2])
    XS = dscr("XS", [cfg.n_slot, D], BF16)
    YS = dscr("YS", [cfg.n_slot, D])

    g = ExitStack()
    es.enter_context(g)
    PSA = g.enter_context(nc.psum_tensor("psa", [P, 8, 512], F32))
    PS = [BankT(PSA, i, "ps%d" % i) for i in range(8)]
    ident_f = kb.sb(g, "ident_f", [P, P], F32)
    ident_b = kb.sb(g, "ident_b", [P, P], BF16)
    ones_f = kb.sb(g, "ones_f", [P, P], F32)
    identD = din("identD", [P, P])
    kb.dma("sp", ident_f[:], identD[:, :], reads=[identD], writes=[ident_f])
    kb.op("dve", lambda e: e.tensor_copy(ident_b[:], ident_f[:]), reads=[ident_f], writes=[ident_b])
    kb.op("dve", lambda e: e.memset(ones_f[:], 1.0), writes=[ones_f])

    def tile_is_ctx(tt):
        return tt * P < cfg.n_ctx

    for l in range(L):
        if "mod" in phases:
            with ExitStack() as ph:
                sc = kb.sb(ph, "sc", [P, 16, 2], F32)
                scs = kb.sb(ph, "scs", [P, 16, 2], F32)
                one2 = kb.sb(ph, "one2", [1, 2], F32)
                brow = kb.sb(ph, "brow", [1, 6 * D], F32)
                wa = [kb.sb(ph, "wa%d" % i, [P, 16, 512], F32) for i in range(2)]
                mo = [kb.sb(ph, "mo%d" % i, [2, 512], F32) for i in range(2)]
                kb.dma("sp", sc[:], ccT[:, :].rearrange("(k p) r -> p k r", p=P), reads=[ccT], writes=[sc])
                kb.op("act", lambda e: e.activation(out=scs[:], in_=sc[:], func=AF.Silu), reads=[sc], writes=[scs])
                kb.op("dve", lambda e: e.memset(one2[:], 1.0), writes=[one2])
                kb.dma("sp", brow[:], b_ada[l, :, :], reads=[b_ada], writes=[brow])
                for cg in range(24):
                    w = wa[cg % 2]
                    kb.dma("sp", w[:], w_ada[l, :, cg * 512:(cg + 1) * 512].rearrange("(k p) c -> p k c", p=P),
                           reads=[w_ada], writes=[w])
                    ps = PS[cg % 2]
                    for k in range(16):
                        kb.op("pe", lambda e, k=k, w=w, ps=ps: e.matmul(ps[0:2, :], scs[:, k, :], w[:, k, :],
                                                                         start=(k == 0), stop=False),
                              reads=[scs, w], writes=[ps] if k == 0 else (), pwrites=() if k == 0 else [ps])
                    kb.op("pe", lambda e, ps=ps, cg=cg: e.matmul(ps[0:2, :], one2[:], brow[:, cg * 512:(cg + 1) * 512],
                                                                  start=False, stop=True),
                          reads=[one2, brow], pwrites=[ps])
                    m = mo[cg % 2]
                    addc = 1.0 if (cg // 4) in (1, 4) else 0.0
                    kb.op("dve", lambda e, m=m, ps=ps, addc=addc: e.tensor_scalar(m[:], ps[0:2, :], addc, None, ALU.add),
                          reads=[ps], writes=[m])
                    kb.dma("pool", MODD[:, cg * 512:(cg + 1) * 512], m[:], reads=[m], pwrites=[MODD])
                kb.barrier()

        if "inproj" in phases:
            xsrc = xin if l == 0 else None
            with ExitStack() as ph:
                TB = 2048 if n >= 2048 else n
                hT = kb.sb(ph, "hT", [P, 16, TB], BF16)
                s1b = [kb.sb(ph, "s1b%d" % r, [P, D], F32) for r in range(2)]
                sh1b = [kb.sb(ph, "sh1b%d" % r, [P, D], F32) for r in range(2)]
                xt = [kb.sb(ph, "xt%d" % i, [P, D], F32) for i in range(2)]
                hb = [kb.sb(ph, "hb%d" % i, [P, D], BF16) for i in range(2)]
                wb = [kb.sb(ph, "wb%d" % i, [P, 16, 512], BF16) for i in range(2)]
                bt = kb.sb(ph, "bt", [P, 132], F32)
                po = [kb.sb(ph, "po%d" % i, [P, 512], F32) for i in range(3)]
                kb.dma("sp", bt[:], b_inT[l, :, :], reads=[b_inT], writes=[bt])
                for r in range(2):
                    kb.dma("sp", s1b[r][:], MODD[r:r + 1, D:2 * D].partition_broadcast(P), reads=[MODD], writes=[s1b[r]])
                    kb.dma("sp", sh1b[r][:], MODD[r:r + 1, 0:D].partition_broadcast(P), reads=[MODD], writes=[sh1b[r]])
                n_cg = -(-cfg.D_IN // 512)
                ti = 0
                wi = 0
                oi = 0
                for t0 in range(0, n, TB):
                    tb = min(TB, n - t0)
                    for tt in range(tb // P):
                        gt = (t0 // P) + tt
                        r = 1 if tile_is_ctx(gt) else 0
                        x_t, h_b = xt[ti % 2], hb[ti % 2]
                        ti += 1
                        kb.dma("sp", x_t[:], xsrc[gt * P:(gt + 1) * P, :], reads=[xsrc], writes=[x_t])
                        kb.op("dve", lambda e, x_t=x_t, r=r: e.tensor_tensor(x_t[:], x_t[:], s1b[r][:], ALU.mult),
                              reads=[x_t, s1b[r]], writes=[x_t])
                        kb.op("pool", lambda e, x_t=x_t, h_b=h_b, r=r: e.tensor_tensor(h_b[:], x_t[:], sh1b[r][:], ALU.add),
                              reads=[x_t, sh1b[r]], writes=[h_b])
                        for half in range(2):
                            pa, pb_ = PS[4 + 2 * half], PS[5 + 2 * half]
                            for kk in range(8):
                                k = half * 8 + kk
                                ps = pa if kk < 4 else pb_
                                o = ps[:].bitcast(BF16)[:, (kk % 4) * P:(kk % 4 + 1) * P]
                                kb.op("pe", lambda e, o=o, h_b=h_b, k=k: e.transpose(o, h_b[:, k * P:(k + 1) * P], ident_b[:]),
                                      reads=[h_b, ident_b], writes=[ps] if kk % 4 == 0 else (),
                                      pwrites=() if kk % 4 == 0 else [ps])
                            for q4, ps in enumerate((pa, pb_)):
                                k0 = half * 8 + q4 * 4
                                src = ps[:].bitcast(BF16)[:, 0:4 * P].rearrange("p (k t) -> p k t", k=4)
                                kb.op("act", lambda e, src=src, k0=k0, tt=tt: e.activation(
                                    out=hT[:, k0:k0 + 4, tt * P:(tt + 1) * P], in_=src, func=AF.Copy),
                                    reads=[ps], pwrites=[hT])
                    for cg in range(n_cg):
                        c0 = cg * 512
                        cw = min(512, cfg.D_IN - c0)
                        w = wb[wi % 2]
                        wi += 1
                        kb.dma("pool", w[:, :, 0:cw], w_in[l, :, c0:c0 + cw].rearrange("(k p) c -> p k c", p=P),
                               reads=[w_in], writes=[w])
                        for jj in range(-(-cw // P)):
                            jw = min(P, cw - jj * P)
                            j = cg * 4 + jj
                            for ts in range(0, tb, 512):
                                tw = min(512, tb - ts)
                                ps = PS[oi % 4]
                                o_t = po[oi % 3]
                                oi += 1
                                for k in range(16):
                                    kb.op("pe", lambda e, ps=ps, w=w, k=k, jj=jj, jw=jw, ts=ts, tw=tw: e.matmul(
                                        ps[0:jw, 0:tw], w[:, k, jj * P:jj * P + jw], hT[:, k, ts:ts + tw],
                                        start=(k == 0), stop=(k == 15)),
                                        reads=[w, hT], writes=[ps] if k == 0 else (), pwrites=() if k == 0 else [ps])
                                kb.op("act", lambda e, ps=ps, o_t=o_t, j=j, jw=jw, tw=tw: e.activation(
                                    out=o_t[0:jw, 0:tw], in_=ps[0:jw, 0:tw], func=AF.Identity, bias=bt[0:jw, j:j + 1], scale=1.0),
                                    reads=[ps, bt], writes=[o_t])
                                pcs = PT.pieces(j * P, j * P + jw) if isinstance(PT, SegT) else [(j * P, j * P + jw)]
                                for (lo_, hi_) in pcs:
                                    kb.dma("sp", PT[lo_:hi_, t0 + ts:t0 + ts + tw], o_t[lo_ - j * P:hi_ - j * P, 0:tw],
                                           reads=[o_t], pwrites=[PT])
                kb.barrier()


        NU = n // P
        NUc = cfg.n_ctx // P

        def unit_order(d):
            if d == 0:
                return list(range(NU))
            return list(range(NUc - 1, -1, -1)) + list(range(NU - 1, NUc - 1, -1))

        def cmask(ph, name):
            t_ = kb.sb(ph, "cm_" + name, [P, P], F32)
            kb.dma("sp", t_[:], c_masks[MI[name], :, :], reads=[c_masks], writes=[t_])
            return t_

        def bc(ap, shape, at):
            for a in at:
                ap = ap.unsqueeze(a)
            return ap.to_broadcast(list(shape))

        if "gla" in phases:
            with ExitStack() as ph:
                QS = 128.0 ** -0.5
                CAT = [kb.sb(ph, "cat%d" % d, [P, 258], F32) for d in range(2)]
                for d in range(2):
                    kb.dma("sp", CAT[d][:, 0:128], c_masks[MI["LINC%d" % d], :, :], reads=[c_masks], writes=[CAT[d]])
                    kb.dma("sp", CAT[d][:, 128:256], c_masks[MI["MREL%d" % d], :, :], reads=[c_masks], pwrites=[CAT[d]])
                    kb.dma("sp", CAT[d][:, 256:258], c_bdcol[:, :], reads=[c_bdcol], pwrites=[CAT[d]])
                MEND = [cmask(ph, "MEND0"), cmask(ph, "MEND1")]
                LINC = [cmask(ph, "LINC0"), cmask(ph, "LINC1")]
                decw = kb.sb(ph, "decw", [16, 2, 512], F32)
                decb = kb.sb(ph, "decb", [1, 2, 512], F32)
                nw = kb.sb(ph, "nw", [P, 2], F32)
                one1 = kb.sb(ph, "one1", [1, P], F32)
                kb.dma("sp", decw[:], gla_dec_w[l, :, :, :].rearrange("d r c -> r d c"), reads=[gla_dec_w], writes=[decw])
                kb.dma("sp", decb[:], gla_dec_b[l, :, :, :].rearrange("d o c -> o d c"), reads=[gla_dec_b], writes=[decb])
                kb.dma("sp", nw[:], gla_norm_wT[l, :, :], reads=[gla_norm_wT], writes=[nw])
                kb.op("dve", lambda e: e.memset(one1[:], 1.0), writes=[one1])
                qT4 = [kb.sb(ph, "g_qT%d" % i, [P, 4, P], F32) for i in range(2)]
                kT4 = [kb.sb(ph, "g_kT%d" % i, [P, 4, P], F32) for i in range(2)]
                vT8 = [kb.sb(ph, "g_vT%d" % i, [P, 8, P], F32) for i in range(2)]
                lrT = [kb.sb(ph, "g_lr%d" % i, [16, P], F32) for i in range(2)]
                ofl = [kb.sb(ph, "g_of%d" % i, [P, 8, P], F32) for i in range(2)]
                ogT = [kb.sb(ph, "g_og%d" % i, [P, 8, P], F32) for i in range(2)]
                gt = kb.sb(ph, "g_gt", [P, 512], F32)
                vtm = kb.sb(ph, "g_vtm", [P, 1024], F32)
                eend = kb.sb(ph, "g_eend", [P, 512], F32)
                kend = kb.sb(ph, "g_kend", [P, 512], F32)
                E12 = kb.sb(ph, "g_E12", [P, 4, 256], F32)
                E3 = kb.sb(ph, "g_E3", [P, 4, P], F32)
                dec4 = kb.sb(ph, "g_dec", [P, 4, 2], F32)
                qb4 = kb.sb(ph, "g_qb", [P, 4, P], F32)
                qr4 = kb.sb(ph, "g_qr", [P, 4, P], F32)
                kr4 = kb.sb(ph, "g_kr", [P, 4, P], F32)
                att4 = kb.sb(ph, "g_att", [P, 4, P], F32)
                S4 = kb.sb(ph, "g_S", [P, 4, 256], F32)
                o8 = kb.sb(ph, "g_o8", [P, 8, P], F32)
                sq8 = kb.sb(ph, "g_sq8", [P, 8, P], F32)
                rs4 = kb.sb(ph, "g_rs4", [P, 4, P], F32)
                sg8 = kb.sb(ph, "g_sg8", [P, 8, P], F32)
                ob8 = [kb.sb(ph, "g_ob%d" % i, [P, 8, P], BF16) for i in range(2)]
                g0 = cfg.gla_off
                for d in range(2):
                    kb.op("dve", lambda e: e.memset(S4[:], 0.0), writes=[S4])
                    for ui, u in enumerate(unit_order(d)):
                        t0 = u * P
                        q_, k_, v_, lr_ = qT4[ui % 2], kT4[ui % 2], vT8[ui % 2], lrT[ui % 2]
                        kb.dma("sp", q_[:], PT[g0:g0 + 512, t0:t0 + P].rearrange("(h p) t -> p h t", p=P), reads=[PT], writes=[q_])
                        kb.dma("sp", k_[:], PT[g0 + 512:g0 + 1024, t0:t0 + P].rearrange("(h p) t -> p h t", p=P), reads=[PT], writes=[k_])
                        kb.dma("sp", v_[:], PT[g0 + 1024:g0 + 2048, t0:t0 + P].rearrange("(h p) t -> p h t", p=P), reads=[PT], writes=[v_])
                        kb.dma("sp", lr_[:], PT[g0 + 3072 + 16 * d:g0 + 3088 + 16 * d, t0:t0 + P], reads=[PT], writes=[lr_])
                        if d == 1:
                            of_, og_ = ofl[ui % 2], ogT[ui % 2]
                            kb.dma("sp", of_[:], OFG[:, t0:t0 + P].rearrange("(c p) t -> p c t", p=P), reads=[OFG], writes=[of_])
                            kb.dma("sp", og_[:], PT[g0 + 2048:g0 + 3072, t0:t0 + P].rearrange("(c p) t -> p c t", p=P), reads=[PT], writes=[og_])
                        kb.op("pe", lambda e, lr_=lr_: e.matmul(PS[0][:, :], lr_[:, :], decw[:, d, :], start=True, stop=False), reads=[lr_, decw], writes=[PS[0]])
                        kb.op("pe", lambda e: e.matmul(PS[0][:, :], one1[:, :], decb[:, d, :], start=False, stop=True), reads=[one1, decb], pwrites=[PS[0]])
                        kb.op("act", lambda e: e.activation(out=gt[:], in_=PS[0][:, :], func=AF.Exp, scale=-1.0), reads=[PS[0]], writes=[gt])
                        kb.op("act", lambda e: e.activation(out=gt[:], in_=gt[:], func=AF.Ln, bias=ones_f[:, 0:1], scale=1.0), reads=[gt, ones_f], writes=[gt])
                        kb.op("dve", lambda e: e.tensor_scalar(gt[:], gt[:], -1.0 / 16.0, None, ALU.mult), reads=[gt], writes=[gt])
                        for h in range(4):
                            kb.op("pe", lambda e, h=h, k_=k_: e.transpose(PS[1][:, h * P:(h + 1) * P], k_[:, h, :], ident_f[:]), reads=[k_, ident_f],
                                  writes=[PS[1]] if h == 0 else (), pwrites=() if h == 0 else [PS[1]])
                        for c in range(8):
                            pb_ = PS[2 + c // 4]
                            kb.op("pe", lambda e, c=c, v_=v_, pb_=pb_: e.transpose(pb_[:, (c % 4) * P:(c % 4 + 1) * P], v_[:, c, :], ident_f[:]), reads=[v_, ident_f],
                                  writes=[pb_] if c % 4 == 0 else (), pwrites=() if c % 4 == 0 else [pb_])
                        kb.op("act", lambda e: e.activation(out=vtm[:].rearrange("p (b c) -> p b c", b=2), in_=PSA[:, 2:4, :], func=AF.Copy), reads=[PS[2], PS[3]], writes=[vtm])
                        kb.op("pe", lambda e: e.matmul(PS[4][:, :], MEND[d][:, :], gt[:, :], start=True, stop=True), reads=[MEND[d], gt], writes=[PS[4]])
                        kb.op("act", lambda e: e.activation(out=eend[:], in_=PS[4][:, :], func=AF.Exp), reads=[PS[4]], writes=[eend])
                        kb.op("dve", lambda e: e.tensor_tensor(kend[:], PS[1][:, :], eend[:], ALU.mult), reads=[PS[1], eend], writes=[kend])
                        for h in range(4):
                            kb.op("pe", lambda e, h=h: e.matmul(PS[4 + h][:, 0:258], gt[:, h * P:(h + 1) * P], CAT[d][:, :], start=True, stop=True), reads=[gt, CAT[d]], writes=[PS[4 + h]])
                        pa4 = [PS[4], PS[5], PS[6], PS[7]]
                        kb.op("act", lambda e: e.activation(out=E12[:], in_=PSA[:, 4:8, 0:256], func=AF.Exp), reads=pa4, writes=[E12])
                        kb.op("act", lambda e: e.activation(out=E3[:], in_=PSA[:, 4:8, 128:256], func=AF.Exp, scale=-1.0), reads=pa4, writes=[E3])
                        kb.op("act", lambda e: e.activation(out=dec4[:], in_=PSA[:, 4:8, 256:258], func=AF.Exp), reads=pa4, writes=[dec4])
                        kb.op("dve", lambda e, q_=q_: e.scalar_tensor_tensor(out=qb4[:], in0=q_[:], scalar=QS, in1=E12[:, :, 0:128], op0=ALU.mult, op1=ALU.mult), reads=[q_, E12], writes=[qb4])
                        kb.op("dve", lambda e, q_=q_: e.scalar_tensor_tensor(out=qr4[:], in0=q_[:], scalar=QS, in1=E12[:, :, 128:256], op0=ALU.mult, op1=ALU.mult), reads=[q_, E12], writes=[qr4])
                        kb.op("pool", lambda e, k_=k_: e.tensor_tensor(kr4[:], k_[:], E3[:], ALU.mult), reads=[k_, E3], writes=[kr4])
                        for h in range(4):
                            kb.op("pe", lambda e, h=h: e.matmul(PS[0][:, h * P:(h + 1) * P], kr4[:, h, :], qr4[:, h, :], start=True, stop=True), reads=[kr4, qr4],
                                  writes=[PS[0]] if h == 0 else (), pwrites=() if h == 0 else [PS[0]])
                        kb.op("dve", lambda e: e.tensor_tensor(att4[:], PS[0][:, :].rearrange("p (h t) -> p h t", h=4), bc(LINC[d][:], [P, 4, P], [1]), ALU.mult),
                              reads=[PS[0], LINC[d]], writes=[att4])
                        first_o = {4: True, 5: True}
                        for s in ((0, 1) if d == 0 else (1, 0)):
                            rs = slice(64 * s, 64 * s + 64)
                            kb.fence_pe()
                            first_s = {2: True, 3: True}
                            for h in range(4):
                                pso = PS[4 + h // 2]
                                for half in range(2):
                                    c0 = ((h % 2) * 2 + half) * P + 64 * s
                                    fo = first_o[4 + h // 2]
                                    first_o[4 + h // 2] = False
                                    kb.op("pe", lambda e, pso=pso, c0=c0, h=h, half=half, rs=rs, s=s: e.matmul(
                                        pso[:, c0:c0 + 64], vtm[rs, h * 256 + half * P:h * 256 + (half + 1) * P], att4[rs, h, 64 * s:64 * s + 64], start=True, stop=False),
                                        reads=[vtm, att4], writes=[pso] if fo else (), pwrites=() if fo else [pso])
                                    kb.op("pe", lambda e, pso=pso, c0=c0, h=h, half=half, s=s: e.matmul(
                                        pso[:, c0:c0 + 64], S4[:, h, half * P:(half + 1) * P], qb4[:, h, 64 * s:64 * s + 64], start=False, stop=True),
                                        reads=[S4, qb4], pwrites=[pso])
                                pss = PS[2 + h // 2]
                                fs = first_s[2 + h // 2]
                                first_s[2 + h // 2] = False
                                kb.op("pe", lambda e, pss=pss, h=h, rs=rs: e.matmul(pss[:, (h % 2) * 256:(h % 2 + 1) * 256], kend[rs, h * P:(h + 1) * P], vtm[rs, h * 256:(h + 1) * 256], start=True, stop=True),
                                      reads=[kend, vtm], writes=[pss] if fs else (), pwrites=() if fs else [pss])
                            kb.op("dve", lambda e, s=s: e.tensor_tensor(S4[:], S4[:], dec4[:, :, s:s + 1].to_broadcast([P, 4, 256]), ALU.mult), reads=[S4, dec4], writes=[S4])
                            kb.op("dve", lambda e: e.tensor_tensor(S4[:], S4[:], PSA[:, 2:4, :].rearrange("p b (h v) -> p (b h) v", h=2), ALU.add), reads=[S4, PS[2], PS[3]], writes=[S4])
                        osrc = PSA[:, 4:6, :].rearrange("p b (c t) -> p (b c) t", c=4)
                        if d == 0:
                            kb.op("act", lambda e: e.activation(out=o8[:], in_=osrc, func=AF.Copy), reads=[PS[4], PS[5]], writes=[o8])
                            kb.dma("sp", OFG[:, t0:t0 + P].rearrange("(c p) t -> p c t", p=P), o8[:], reads=[o8], pwrites=[OFG])
                        else:
                            kb.op("dve", lambda e, of_=of_: e.tensor_tensor(o8[:], osrc, of_[:], ALU.add), reads=[PS[4], PS[5], of_], writes=[o8])
                            kb.op("pool", lambda e: e.tensor_tensor(sq8[:], o8[:], o8[:], ALU.mult), reads=[o8], writes=[sq8])
                            for h in range(4):
                                for half in range(2):
                                    kb.op("pe", lambda e, h=h, half=half: e.matmul(PS[0][:, h * P:(h + 1) * P], ones_f[:, :], sq8[:, 2 * h + half, :], start=(half == 0), stop=(half == 1)),
                                          reads=[ones_f, sq8], writes=[PS[0]] if (h == 0 and half == 0) else (), pwrites=() if (h == 0 and half == 0) else [PS[0]])
                            kb.op("dve", lambda e: e.tensor_scalar(rs4[:], PS[0][:, :].rearrange("p (h t) -> p h t", h=4), 1.0 / 256.0, 1e-6, ALU.mult, ALU.add), reads=[PS[0]], writes=[rs4])
                            kb.op("act", lambda e: e.activation(out=rs4[:], in_=rs4[:], func=AF.Sqrt), reads=[rs4], writes=[rs4])
                            kb.op("dve", lambda e: e.reciprocal(rs4[:], rs4[:]), reads=[rs4], writes=[rs4])
                            o84 = o8[:].rearrange("p (h f) t -> p h f t", f=2)
                            kb.op("dve", lambda e: e.tensor_tensor(o84, o84, bc(rs4[:], [P, 4, 2, P], [2]), ALU.mult), reads=[o8, rs4], writes=[o8])
                            kb.op("pool", lambda e: e.tensor_tensor(o84, o84, bc(nw[:], [P, 4, 2, P], [1, 3]), ALU.mult), reads=[o8, nw], writes=[o8])
                            kb.op("act", lambda e, og_=og_: e.activation(out=sg8[:], in_=og_[:], func=AF.Silu), reads=[og_], writes=[sg8])
                            ob_ = ob8[ui % 2]
                            kb.op("dve", lambda e, ob_=ob_: e.tensor_tensor(ob_[:], o8[:], sg8[:], ALU.mult), reads=[o8, sg8], writes=[ob_])
                            kb.dma("sp", OT[0:1024, t0:t0 + P].rearrange("(c p) t -> p c t", p=P), ob_[:], reads=[ob_], pwrites=[OT])
                kb.barrier()

        def emit_solve(zq, b0):
            nq = len(zq)
            bk = [[PS[b0[i] + h] for h in range(4)] for i in range(nq)]
            for step in range(6):
                for i in range(nq):
                    z = zq[i]
                    for h in range(4):
                        if step == 0:
                            kb.op("pe", lambda e, z=z, h=h, i=i: e.matmul(bk[i][h][:, 128:256], z[:, h, 256:384], z[:, h, 128:256], start=True, stop=True), reads=[z], writes=[bk[i][h]])
                        elif step < 5:
                            kb.op("pe", lambda e, z=z, h=h, i=i: e.matmul(bk[i][h][:, 0:256], z[:, h, 256:384], z[:, h, 0:256], start=True, stop=True), reads=[z], writes=[bk[i][h]])
                        else:
                            kb.op("pe", lambda e, z=z, h=h, i=i: e.matmul(bk[i][h][:, 0:128], z[:, h, 256:384], z[:, h, 0:128], start=True, stop=True), reads=[z], writes=[bk[i][h]])
                        if step < 5:
                            kb.op("pe", lambda e, z=z, h=h, i=i: e.matmul(bk[i][h][:, 256:384], z[:, h, 128:256], z[:, h, 256:384], start=True, stop=True), reads=[z], pwrites=[bk[i][h]])
                for i in range(nq):
                    z = zq[i]
                    bb = b0[i]
                    if step > 0:
                        kb.op("dve", lambda e, z=z, bb=bb: e.tensor_tensor(z[:, :, 0:128], z[:, :, 0:128], PSA[:, bb:bb + 4, 0:128], ALU.add), reads=[z] + bk[i], writes=[z])
                    if step < 5:
                        kb.op("act", lambda e, z=z, bb=bb: e.activation(out=z[:, :, 128:384], in_=PSA[:, bb:bb + 4, 128:384], func=AF.Copy), reads=bk[i],
                              writes=[z] if step == 0 else (), pwrites=() if step == 0 else [z])

        if "gdn" in phases:
            g0 = cfg.gdn_off
            with ExitStack() as ph:
                GW = cfg.grid_w
                RB = 8
                xi_ = [kb.sb(ph, "c_xi%d" % i, [P, RB + 2, GW], F32) for i in range(2)]
                yc = [kb.sb(ph, "c_y%d" % i, [P, RB, GW], F32) for i in range(2)]
                ysq = kb.sb(ph, "c_ysq", [P, RB * GW], F32)
                rsd = kb.sb(ph, "c_rsd", [P, RB * GW], F32)
                wc = kb.sb(ph, "c_wc", [P, 24, 9], F32)
                kb.dma("sp", wc[:], gdn_conv_wT[l, :, :, :].rearrange("c p k -> p c k"), reads=[gdn_conv_wT], writes=[wc])
                blocks = [("ctx", 0, cfg.n_ctx)] + [("lat", r0, min(RB, cfg.n_lat // GW - r0)) for r0 in range(0, cfg.n_lat // GW, RB)]
                bi = 0
                for cc in range(24):
                    for kind, b_0, b_n in blocks:
                        x_, y_ = xi_[bi % 2], yc[bi % 2]
                        bi += 1
                        prow = g0 + cc * P
                        if kind == "ctx":
                            nt = b_n
                            xf = x_[:].rearrange("p r w -> p (r w)")
                            yf = y_[:].rearrange("p r w -> p (r w)")
                            kb.op("pool", lambda e, xf=xf: e.memset(xf[:, 0:nt + 2], 0.0), writes=[x_])
                            kb.dma("sp", xf[:, 1:nt + 1], PT[prow:prow + P, 0:nt], reads=[PT], pwrites=[x_])
                            kb.op("dve", lambda e, xf=xf, yf=yf, cc=cc: e.tensor_scalar(yf[:, 0:nt], xf[:, 1:nt + 1], wc[:, cc, 4:5], None, ALU.mult), reads=[x_, wc], writes=[y_])
                            for dx in (0, 2):
                                kb.op("dve", lambda e, xf=xf, yf=yf, cc=cc, dx=dx: e.scalar_tensor_tensor(out=yf[:, 0:nt], in0=xf[:, dx:dx + nt], scalar=wc[:, cc, 3 + dx:4 + dx], in1=yf[:, 0:nt], op0=ALU.mult, op1=ALU.add),
                                      reads=[x_, wc, y_], writes=[y_])
                            ntok, tok0 = nt, 0
                            yv = yf[:, 0:nt]
                        else:
                            r0, nr = b_0, b_n
                            nrows = cfg.n_lat // GW
                            lo, hi = max(r0 - 1, 0), min(r0 + nr + 1, nrows)
                            if lo > r0 - 1 or hi < r0 + nr + 1:
                                kb.op("pool", lambda e, x_=x_: e.memset(x_[:], 0.0), writes=[x_])
                                wr = dict(pwrites=[x_])
                            else:
                                wr = dict(writes=[x_])
                            kb.dma("sp", x_[:, lo - (r0 - 1):hi - (r0 - 1), :],
                                   PT[prow:prow + P, cfg.n_ctx + lo * GW:cfg.n_ctx + hi * GW].rearrange("p (r w) -> p r w", w=GW), reads=[PT], **wr)
                            kb.op("dve", lambda e, x_=x_, y_=y_, cc=cc, nr=nr: e.tensor_scalar(y_[:, 0:nr, :], x_[:, 1:nr + 1, :], wc[:, cc, 4:5], None, ALU.mult), reads=[x_, wc], writes=[y_])
                            for dy in range(3):
                                for dx in range(3):
                                    if dy == 1 and dx == 1:
                                        continue
                                    c_lo, c_hi = (1, GW) if dx == 0 else ((0, GW) if dx == 1 else (0, GW - 1))
                                    kb.op("dve", lambda e, x_=x_, y_=y_, cc=cc, nr=nr, dy=dy, dx=dx, c_lo=c_lo, c_hi=c_hi: e.scalar_tensor_tensor(
                                        out=y_[:, 0:nr, c_lo:c_hi], in0=x_[:, dy:dy + nr, c_lo + dx - 1:c_hi + dx - 1], scalar=wc[:, cc, dy * 3 + dx:dy * 3 + dx + 1],
                                        in1=y_[:, 0:nr, c_lo:c_hi], op0=ALU.mult, op1=ALU.add), reads=[x_, wc, y_], writes=[y_])
                            ntok, tok0 = nr * GW, cfg.n_ctx + r0 * GW
                            yv = y_[:, 0:nr, :].rearrange("p r w -> p (r w)")
                        kb.op("act", lambda e, yv=yv: e.activation(out=yv, in_=yv, func=AF.Silu), reads=[y_], writes=[y_])
                        if cc < 16:
                            kb.op("pool", lambda e, yv=yv: e.tensor_tensor(ysq[:, 0:ntok], yv, yv, ALU.mult), reads=[y_], writes=[ysq])
                            kb.op("pe", lambda e: e.matmul(PS[0][:, 0:ntok], ones_f[:, :], ysq[:, 0:ntok], start=True, stop=True), reads=[ones_f, ysq], writes=[PS[0]])
                            kb.op("dve", lambda e: e.tensor_scalar(rsd[:, 0:ntok], PS[0][:, 0:ntok], 1e-6, None, ALU.add), reads=[PS[0]], writes=[rsd])
                            kb.op("act", lambda e: e.activation(out=rsd[:, 0:ntok], in_=rsd[:, 0:ntok], func=AF.Sqrt), reads=[rsd], writes=[rsd])
                            kb.op("dve", lambda e: e.reciprocal(rsd[:, 0:ntok], rsd[:, 0:ntok]), reads=[rsd], writes=[rsd])
                            qs = (128.0 ** -0.5) if cc < 8 else 1.0
                            kb.op("dve", lambda e, yv=yv, qs=qs: e.scalar_tensor_tensor(out=yv, in0=yv, scalar=qs, in1=rsd[:, 0:ntok], op0=ALU.mult, op1=ALU.mult), reads=[y_, rsd], writes=[y_])
                        kb.dma("sp", GC[cc * P:(cc + 1) * P, tok0:tok0 + ntok], yv, reads=[y_], pwrites=[GC])
                kb.barrier()

            with ExitStack() as ph:
              if not getattr(cfg, 'gdn_only_pre', False):
                  mk = {k_: cmask(ph, k_) for k_ in ("BD", "LINC0", "LINC1", "NEG0", "NEG1", "NEGI0", "NEGI1")}
                  bdc = kb.sb(ph, "d_bdc", [P, 2], F32)
                  kb.dma("sp", bdc[:], c_bdcol[:, :], reads=[c_bdcol], writes=[bdc])
                  negones = kb.sb(ph, "d_negones", [P, P], F32)
                  kb.op("dve", lambda e: e.memset(negones[:], -1.0), writes=[negones])
                  LABs = kb.sb(ph, "d_LABs", [P, NU, 32], F32)
                  par = kb.sb(ph, "d_par", [P, 32], F32)
                  nwg = kb.sb(ph, "d_nwg", [P, 1], F32)
                  kb.dma("sp", par[:, 0:16], gdn_a_log[l, :, :].partition_broadcast(P), reads=[gdn_a_log], writes=[par])
                  kb.dma("sp", par[:, 16:32], gdn_dt_bias[l, :, :].partition_broadcast(P), reads=[gdn_dt_bias], pwrites=[par])
                  kb.dma("sp", nwg[:], gdn_norm_wT[l, :, :], reads=[gdn_norm_wT], writes=[nwg])
                  kb.op("act", lambda e: e.activation(out=par[:, 0:16], in_=par[:, 0:16], func=AF.Exp), reads=[par], writes=[par])
                  kb.op("dve", lambda e: e.tensor_scalar(par[:, 0:16], par[:, 0:16], -1.0, None, ALU.mult), reads=[par], writes=[par])
                  abT = [kb.sb(ph, "d_abT%d" % i, [32, P], F32) for i in range(2)]
                  abx = kb.sb(ph, "d_abx", [P, 16], F32)
                  for u in range(NU):
                      a_ = abT[u % 2]
                      kb.dma("sp", a_[:], PT[g0 + 4096:g0 + 4128, u * P:(u + 1) * P], reads=[PT], writes=[a_])
                      kb.op("pe", lambda e, a_=a_: e.transpose(PS[0][:, 0:32], a_[:, :], ident_f[0:32, 0:32]), reads=[a_, ident_f], writes=[PS[0]])
                      kb.op("dve", lambda e: e.tensor_tensor(abx[:], PS[0][:, 0:16], par[:, 16:32], ALU.add), reads=[PS[0], par], writes=[abx])
                      kb.op("act", lambda e: e.activation(out=abx[:], in_=abx[:], func=AF.Exp), reads=[abx], writes=[abx])
                      kb.op("act", lambda e: e.activation(out=abx[:], in_=abx[:], func=AF.Ln, bias=ones_f[:, 0:1], scale=1.0), reads=[abx, ones_f], writes=[abx])
                      kb.op("dve", lambda e, u=u: e.tensor_tensor(LABs[:, u, 0:16], abx[:], par[:, 0:16], ALU.mult), reads=[abx, par], pwrites=[LABs])
                      kb.op("act", lambda e, u=u: e.activation(out=LABs[:, u, 16:32], in_=PS[0][:, 16:32], func=AF.Sigmoid), reads=[PS[0]], pwrites=[LABs])
                  ld = [[kb.sb(ph, "d_%s%d" % (nm, i), [P, 8, P], F32) for i in range(2)] for nm in ("qT", "kT", "vT")]
                  bk8 = kb.sb(ph, "d_bk8", [P, 8, P], F32)
                  bek8 = kb.sb(ph, "d_bek8", [P, 8, P], F32)
                  kend8 = kb.sb(ph, "d_kend8", [P, 8, P], F32)
                  bv8 = kb.sb(ph, "d_bv8", [P, 8, P], F32)
                  LAL8 = kb.sb(ph, "d_LAL8", [P, 8, P], F32)
                  F0 = kb.sb(ph, "d_F0", [P, 8, P], F32)
                  F0T = kb.sb(ph, "d_F0T", [P, 8, P], F32)
                  F0Ti = kb.sb(ph, "d_F0Ti", [P, 8, P], F32)
                  bkT8 = kb.sb(ph, "d_bkT8", [P, 8, P], F32)
                  zq = [kb.sb(ph, "d_zq%d" % i, [P, 4, 384], F32) for i in range(2)]
                  qkT8 = kb.sb(ph, "d_qkT8", [P, 8, P], F32)
                  u8 = kb.sb(ph, "d_u8", [P, 8, P], F32)
                  wT8 = kb.sb(ph, "d_wT8", [P, 8, P], F32)
                  S8 = kb.sb(ph, "d_S8", [P, 8, P], F32)
                  dl8 = kb.sb(ph, "d_dl8", [P, 8, P], F32)
                  o18 = kb.sb(ph, "d_o18", [P, 8, P], F32)
                  O8 = kb.sb(ph, "d_O8", [P, 8, P], F32)
                  sm8 = kb.sb(ph, "d_sm8", [P, 64], F32)
                  decb = kb.sb(ph, "d_decb", [P, 2, 8], F32)
                  zT8 = [kb.sb(ph, "d_zT%d" % i, [P, 8, P], F32) for i in range(2)]
                  ofd = [kb.sb(ph, "d_of%d" % i, [P, 8, P], F32) for i in range(2)]
                  sq_ = kb.sb(ph, "d_sq", [P, 8, P], F32)
                  ob8 = [kb.sb(ph, "d_ob%d" % i, [P, 8, P], BF16) for i in range(2)]
                  b8 = lambda t_: [PS[i] for i in t_]
                  for d in range(2):
                      LINCd, NEGd, NEGId, NEGt = mk["LINC%d" % d], mk["NEG%d" % d], mk["NEGI%d" % d], mk["NEG%d" % (1 - d)]
                      kb.op("dve", lambda e: e.memset(S8[:], 0.0), writes=[S8])
                      for ui, u in enumerate(unit_order(d)):
                          t0 = u * P
                          qT8, kT8, vT8 = ld[0][ui % 2], ld[1][ui % 2], ld[2][ui % 2]
                          for j_, t_ in enumerate((qT8, kT8, vT8)):
                              kb.dma("sp", t_[:], GC[j_ * 1024:(j_ + 1) * 1024, t0:t0 + P].rearrange("(h p) t -> p h t", p=P), reads=[GC], writes=[t_])
                          if d == 1:
                              z_, of_ = zT8[ui % 2], ofd[ui % 2]
                              kb.dma("sp", z_[:], PT[g0 + 3072:g0 + 4096, t0:t0 + P].rearrange("(h p) t -> p h t", p=P), reads=[PT], writes=[z_])
                              kb.dma("sp", of_[:], OFD[t0:t0 + P, :].rearrange("t (h v) -> t h v", h=8), reads=[OFD], writes=[of_])
                          la8 = LABs[:, u, 8 * d:8 * d + 8]
                          be8 = LABs[:, u, 16 + 8 * d:24 + 8 * d]
                          kb.op("pe", lambda e: e.matmul(PS[0][:, 0:8], LINCd[:, :], la8, start=True, stop=True), reads=[LINCd, LABs], writes=[PS[0]])
                          kb.op("pe", lambda e: e.matmul(PS[0][:, 8:16], mk["BD"][:, :], la8, start=True, stop=True), reads=[mk["BD"], LABs], pwrites=[PS[0]])
                          kb.op("act", lambda e: e.activation(out=sm8[:, 0:8], in_=PS[0][:, 0:8], func=AF.Copy), reads=[PS[0]], writes=[sm8])
                          kb.op("act", lambda e: e.activation(out=sm8[:, 8:16], in_=PS[0][:, 0:8], func=AF.Exp), reads=[PS[0]], pwrites=[sm8])
                          kb.op("dve", lambda e: e.tensor_tensor(sm8[:, 24:32], PS[0][:, 8:16], sm8[:, 0:8], ALU.subtract), reads=[PS[0], sm8], pwrites=[sm8])
                          kb.op("act", lambda e: e.activation(out=sm8[:, 16:24], in_=sm8[:, 24:32], func=AF.Exp), reads=[sm8], pwrites=[sm8])
                          for s in range(2):
                              kb.op("dve", lambda e, s=s: e.tensor_scalar(sm8[:, 32 + 8 * s:40 + 8 * s], la8, bdc[:, s:s + 1], None, ALU.mult), reads=[LABs, bdc], pwrites=[sm8])
                          kb.op("pe", lambda e: e.matmul(PS[0][:, 16:32], ones_f[:, :], sm8[:, 32:48], start=True, stop=True), reads=[ones_f, sm8], pwrites=[PS[0]])
                          kb.op("act", lambda e: e.activation(out=decb[:].rearrange("p s h -> p (s h)"), in_=PS[0][:, 16:32], func=AF.Exp), reads=[PS[0]], writes=[decb])
                          for h in range(8):
                              kb.op("pe", lambda e, h=h: e.transpose(PS[2 + h // 4][:, (h % 4) * P:(h % 4 + 1) * P], kT8[:, h, :], ident_f[:]), reads=[kT8, ident_f],
                                    writes=[PS[2 + h // 4]] if h % 4 == 0 else (), pwrites=() if h % 4 == 0 else [PS[2 + h // 4]])
                              kb.op("pe", lambda e, h=h: e.transpose(PS[4 + h // 4][:, (h % 4) * P:(h % 4 + 1) * P], vT8[:, h, :], ident_f[:]), reads=[vT8, ident_f],
                                    writes=[PS[4 + h // 4]] if h % 4 == 0 else (), pwrites=() if h % 4 == 0 else [PS[4 + h // 4]])
                          kps = PSA[:, 2:4, :].rearrange("p b (h t) -> p (b h) t", h=4)
                          vps = PSA[:, 4:6, :].rearrange("p b (h t) -> p (b h) t", h=4)
                          kb.op("dve", lambda e: e.tensor_tensor(bk8[:], kps, bc(be8, [P, 8, P], [2]), ALU.mult), reads=b8((2, 3)) + [LABs], writes=[bk8])
                          kb.op("dve", lambda e: e.tensor_tensor(kend8[:], kps, bc(sm8[:, 16:24], [P, 8, P], [2]), ALU.mult), reads=b8((2, 3)) + [sm8], writes=[kend8])
                          kb.op("dve", lambda e: e.tensor_tensor(bv8[:], vps, bc(be8, [P, 8, P], [2]), ALU.mult), reads=b8((4, 5)) + [LABs], writes=[bv8])
                          kb.op("pool", lambda e: e.tensor_tensor(bek8[:], bk8[:], bc(sm8[:, 8:16], [P, 8, P], [2]), ALU.mult), reads=[bk8, sm8], writes=[bek8])
                          kb.op("pool", lambda e: e.tensor_tensor(LAL8[:], bc(LINCd[:], [P, 8, P], [1]), bc(la8, [P, 8, P], [2]), ALU.mult), reads=[LINCd, LABs], writes=[LAL8])
                          for h in range(8):
                              pr = PS[6 + h // 4]
                              kb.op("pe", lambda e, h=h, pr=pr: e.matmul(pr[:, (h % 4) * P:(h % 4 + 1) * P], LAL8[:, h, :], ones_f[:, :], start=True, stop=False), reads=[LAL8, ones_f],
                                    writes=[pr] if h % 4 == 0 else (), pwrites=() if h % 4 == 0 else [pr])
                              kb.op("pe", lambda e, h=h, pr=pr: e.matmul(pr[:, (h % 4) * P:(h % 4 + 1) * P], negones[:, :], LAL8[:, h, :], start=False, stop=True), reads=[LAL8, negones], pwrites=[pr])
                          rps = PSA[:, 6:8, :].rearrange("p b (h t) -> p (b h) t", h=4)
                          kb.op("dve", lambda e: e.tensor_tensor(F0[:], rps, bc(NEGt[:], [P, 8, P], [1]), ALU.add), reads=b8((6, 7)) + [NEGt], writes=[F0])
                          kb.op("act", lambda e: e.activation(out=F0[:], in_=F0[:], func=AF.Exp), reads=[F0], writes=[F0])
                          kb.op("dve", lambda e: e.scalar_tensor_tensor(out=F0T[:], in0=rps, scalar=-1.0, in1=bc(NEGd[:], [P, 8, P], [1]), op0=ALU.mult, op1=ALU.add), reads=b8((6, 7)) + [NEGd], writes=[F0T])
                          kb.op("act", lambda e: e.activation(out=F0T[:], in_=F0T[:], func=AF.Exp), reads=[F0T], writes=[F0T])
                          kb.op("dve", lambda e: e.scalar_tensor_tensor(out=F0Ti[:], in0=rps, scalar=-1.0, in1=bc(NEGId[:], [P, 8, P], [1]), op0=ALU.mult, op1=ALU.add), reads=b8((6, 7)) + [NEGId], writes=[F0Ti])
                          kb.op("act", lambda e: e.activation(out=F0Ti[:], in_=F0Ti[:], func=AF.Exp), reads=[F0Ti], writes=[F0Ti])
                          for h in range(8):
                              kb.op("pe", lambda e, h=h: e.transpose(PS[2 + h // 4][:, (h % 4) * P:(h % 4 + 1) * P], bk8[:, h, :], ident_f[:]), reads=[bk8, ident_f],
                                    writes=[PS[2 + h // 4]] if h % 4 == 0 else (), pwrites=() if h % 4 == 0 else [PS[2 + h // 4]])
                          kb.op("act", lambda e: e.activation(out=bkT8[:], in_=kps, func=AF.Copy), reads=b8((2, 3)), writes=[bkT8])
                          for qd in range(2):
                              z = zq[qd]
                              for hh in range(4):
                                  h = qd * 4 + hh
                                  kb.op("pe", lambda e, h=h, hh=hh: e.matmul(PS[4][:, hh * P:(hh + 1) * P], bkT8[:, h, :], kT8[:, h, :], start=True, stop=True), reads=[bkT8, kT8],
                                        writes=[PS[4]] if hh == 0 else (), pwrites=() if hh == 0 else [PS[4]])
                                  kb.op("pe", lambda e, h=h, hh=hh: e.matmul(PS[5][:, hh * P:(hh + 1) * P], kT8[:, h, :], bkT8[:, h, :], start=True, stop=True), reads=[bkT8, kT8],
                                        writes=[PS[5]] if hh == 0 else (), pwrites=() if hh == 0 else [PS[5]])
                                  kb.op("pe", lambda e, h=h, hh=hh: e.matmul(PS[6][:, hh * P:(hh + 1) * P], kT8[:, h, :], qT8[:, h, :], start=True, stop=True), reads=[qT8, kT8],
                                        writes=[PS[6]] if hh == 0 else (), pwrites=() if hh == 0 else [PS[6]])
                              v4 = lambda b_: PS[b_][:, :].rearrange("p (h t) -> p h t", h=4)
                              kb.op("dve", lambda e, z=z, qd=qd: e.tensor_tensor(z[:, :, 256:384], v4(4), F0[:, qd * 4:qd * 4 + 4, :], ALU.mult), reads=[PS[4], F0], writes=[z])
                              kb.op("dve", lambda e, z=z, qd=qd: e.tensor_tensor(z[:, :, 128:256], v4(5), F0T[:, qd * 4:qd * 4 + 4, :], ALU.mult), reads=[PS[5], F0T], pwrites=[z])
                              kb.op("dve", lambda e, qd=qd: e.tensor_tensor(qkT8[:, qd * 4:qd * 4 + 4, :], v4(6), F0Ti[:, qd * 4:qd * 4 + 4, :], ALU.mult), reads=[PS[6], F0Ti],
                                    writes=[qkT8] if qd == 0 else (), pwrites=() if qd == 0 else [qkT8])
                              kb.op("pool", lambda e, z=z: e.tensor_tensor(z[:, :, 0:128], bc(ident_f[:], [P, 4, P], [1]), z[:, :, 128:256], ALU.subtract), reads=[ident_f, z], pwrites=[z])
                          emit_solve(zq, [0, 4])
                          for h in range(8):
                              z = zq[h // 4]
                              kb.op("pe", lambda e, h=h, z=z: e.matmul(PS[0 + h // 4][:, (h % 4) * P:(h % 4 + 1) * P], z[:, h % 4, 0:128], bv8[:, h, :], start=True, stop=True), reads=[z, bv8],
                                    writes=[PS[h // 4]] if h % 4 == 0 else (), pwrites=() if h % 4 == 0 else [PS[h // 4]])
                              kb.op("pe", lambda e, h=h, z=z: e.matmul(PS[2 + h // 4][:, (h % 4) * P:(h % 4 + 1) * P], bek8[:, h, :], z[:, h % 4, 0:128], start=True, stop=True), reads=[z, bek8],
                                    writes=[PS[2 + h // 4]] if h % 4 == 0 else (), pwrites=() if h % 4 == 0 else [PS[2 + h // 4]])
                          kb.op("act", lambda e: e.activation(out=u8[:], in_=PSA[:, 0:2, :].rearrange("p b (h t) -> p (b h) t", h=4), func=AF.Copy), reads=b8((0, 1)), writes=[u8])
                          kb.op("dve", lambda e: e.tensor_copy(wT8[:], kps), reads=b8((2, 3)), writes=[wT8])
                          for s in ((0, 1) if d == 0 else (1, 0)):
                              rs = slice(64 * s, 64 * s + 64)
                              for h in range(8):
                                  kb.op("pe", lambda e, h=h: e.matmul(PS[0 + h // 4][:, (h % 4) * P:(h % 4 + 1) * P], wT8[:, h, :], S8[:, h, :], start=True, stop=True), reads=[wT8, S8],
                                        writes=[PS[h // 4]] if h % 4 == 0 else (), pwrites=() if h % 4 == 0 else [PS[h // 4]])
                                  kb.op("pe", lambda e, h=h: e.matmul(PS[2 + h // 4][:, (h % 4) * P:(h % 4 + 1) * P], qT8[:, h, :], S8[:, h, :], start=True, stop=True), reads=[qT8, S8],
                                        writes=[PS[2 + h // 4]] if h % 4 == 0 else (), pwrites=() if h % 4 == 0 else [PS[2 + h // 4]])
                              kb.op("dve", lambda e, rs=rs: e.tensor_tensor(dl8[rs, :, :], u8[rs, :, :], PSA[rs, 0:2, :].rearrange("p b (h t) -> p (b h) t", h=4), ALU.subtract),
                                    reads=[u8] + b8((0, 1)), writes=[dl8])
                              kb.op("dve", lambda e, rs=rs: e.tensor_tensor(o18[rs, :, :], PSA[rs, 2:4, :].rearrange("p b (h t) -> p (b h) t", h=4), bc(sm8[rs, 8:16], [64, 8, P], [2]), ALU.mult),
                                    reads=[sm8] + b8((2, 3)), writes=[o18])
                              for h in range(8):
                                  kb.op("pe", lambda e, h=h, rs=rs: e.matmul(PS[4 + h // 4][:, (h % 4) * P:(h % 4 + 1) * P], qkT8[rs, h, :], dl8[rs, h, :], start=True, stop=True), reads=[qkT8, dl8],
                                        writes=[PS[4 + h // 4]] if h % 4 == 0 else (), pwrites=() if h % 4 == 0 else [PS[4 + h // 4]])
                                  kb.op("pe", lambda e, h=h, rs=rs: e.matmul(PS[6 + h // 4][:, (h % 4) * P:(h % 4 + 1) * P], kend8[rs, h, :], dl8[rs, h, :], start=True, stop=True), reads=[kend8, dl8],
                                        writes=[PS[6 + h // 4]] if h % 4 == 0 else (), pwrites=() if h % 4 == 0 else [PS[6 + h // 4]])
                              kb.op("dve", lambda e, rs=rs: e.tensor_tensor(O8[rs, :, :], o18[rs, :, :], PSA[rs, 4:6, :].rearrange("p b (h t) -> p (b h) t", h=4), ALU.add),
                                    reads=[o18] + b8((4, 5)), pwrites=[O8])
                              kb.op("pool", lambda e, s=s: e.tensor_tensor(S8[:], S8[:], bc(decb[:, s, :], [P, 8, P], [2]), ALU.mult), reads=[S8, decb], writes=[S8])
                              kb.op("dve", lambda e: e.tensor_tensor(S8[:], S8[:], rps, ALU.add), reads=[S8] + b8((6, 7)), writes=[S8])
                          if d == 0:
                              kb.dma("sp", OFD[t0:t0 + P, :].rearrange("t (h v) -> t h v", h=8), O8[:], reads=[O8], pwrites=[OFD])
                          else:
                              kb.op("dve", lambda e, of_=of_: e.tensor_tensor(O8[:], O8[:], of_[:], ALU.add), reads=[O8, of_], writes=[O8])
                              kb.op("pool", lambda e: e.tensor_tensor(sq_[:], O8[:], O8[:], ALU.mult), reads=[O8], writes=[sq_])
                              kb.op("dve", lambda e: e.tensor_reduce(out=sm8[:, 48:56], in_=sq_[:], axis=mybir.AxisListType.X, op=ALU.add), reads=[sq_], pwrites=[sm8])
                              kb.op("dve", lambda e: e.tensor_scalar(sm8[:, 48:56], sm8[:, 48:56], 1.0 / 128.0, 1e-6, ALU.mult, ALU.add), reads=[sm8], pwrites=[sm8])
                              kb.op("act", lambda e: e.activation(out=sm8[:, 48:56], in_=sm8[:, 48:56], func=AF.Sqrt), reads=[sm8], pwrites=[sm8])
                              kb.op("dve", lambda e: e.reciprocal(sm8[:, 48:56], sm8[:, 48:56]), reads=[sm8], pwrites=[sm8])
                              kb.op("dve", lambda e: e.tensor_tensor(O8[:], O8[:], bc(sm8[:, 48:56], [P, 8, P], [2]), ALU.mult), reads=[O8, sm8], writes=[O8])
                              for h in range(8):
                                  kb.op("pe", lambda e, h=h: e.transpose(PS[h // 4][:, (h % 4) * P:(h % 4 + 1) * P], O8[:, h, :], ident_f[:]), reads=[O8, ident_f],
                                        writes=[PS[h // 4]] if h % 4 == 0 else (), pwrites=() if h % 4 == 0 else [PS[h // 4]])
                              kb.op("act", lambda e, z_=z_: e.activation(out=z_[:], in_=z_[:], func=AF.Silu), reads=[z_], writes=[z_])
                              kb.op("dve", lambda e, z_=z_: e.scalar_tensor_tensor(out=sq_[:], in0=PSA[:, 0:2, :].rearrange("p b (h t) -> p (b h) t", h=4), scalar=nwg[:, 0:1], in1=z_[:], op0=ALU.mult, op1=ALU.mult),
                                    reads=b8((0, 1)) + [nwg, z_], writes=[sq_])
                              ob_ = ob8[ui % 2]
                              kb.op("act", lambda e, ob_=ob_: e.activation(out=ob_[:], in_=sq_[:], func=AF.Copy), reads=[sq_], writes=[ob_])
                              kb.dma("sp", OT[2048:3072, t0:t0 + P].rearrange("(h p) t -> p h t", p=P), ob_[:], reads=[ob_], pwrites=[OT])
                  kb.barrier()

        if "rwkv" in phases:
            r0_ = cfg.rwkv_off
            with ExitStack() as ph:
                PP = kb.sb(ph, "r_PP", [P, 27, 512], F32)
                xh = [kb.sb(ph, "r_xh%d" % i, [P, 514], F32) for i in range(2)]
                ssum = [kb.sb(ph, "r_ss%d" % i, [P, 512], F32) for i in range(2)]
                mu = kb.sb(ph, "r_mu", [P, 27, 2], F32)
                W2t = kb.sb(ph, "r_W2t", [P, 1024], F32)
                A2t = kb.sb(ph, "r_A2t", [P, 1024], F32)
                G2t = kb.sb(ph, "r_G2t", [P, 1024], F32)
                pr8 = kb.sb(ph, "r_pr8", [P, 7, 8], F32)
                w0a0 = kb.sb(ph, "r_w0a0", [P, 2, 2, 8], F32)
                bo64 = cmask(ph, "BD")
                tl = kb.sb(ph, "r_tl", [P, 512], F32)
                wk = [kb.sb(ph, "r_wk%d" % i, [P, 512], F32) for i in range(6)]
                kkc = kb.sb(ph, "r_kkc", [P, 8, 512], F32)
                mu0 = kb.sb(ph, "r_mu0", [P, 27], F32)
                kb.dma("sp", mu0[:], rwkv_muT[l, :, :], reads=[rwkv_muT], writes=[mu0])
                kb.op("dve", lambda e: e.tensor_scalar(mu[:, :, 1], mu0[:], 0.5, None, ALU.mult), reads=[mu0], writes=[mu])
                kb.op("dve", lambda e: e.tensor_scalar(mu[:, :, 0], mu0[:], -1.0, 1.0, ALU.mult, ALU.add), reads=[mu0], pwrites=[mu])
                kb.dma("sp", W2t[:], rwkv_w2[l, :, :, :].rearrange("d r c -> (d r) c"), reads=[rwkv_w2], writes=[W2t])
                kb.dma("sp", A2t[:], rwkv_a2[l, :, :, :].rearrange("d r c -> (d r) c"), reads=[rwkv_a2], writes=[A2t])
                kb.dma("sp", G2t[:], rwkv_g2[l, :, :], reads=[rwkv_g2], writes=[G2t])
                kb.dma("sp", pr8[:, 0:4, :], rwkv_p4T[l, :, :, :], reads=[rwkv_p4T], writes=[pr8])
                kb.dma("sp", w0a0[:], rwkv_w0a0T[l, :, :, :, :], reads=[rwkv_w0a0T], writes=[w0a0])
                kb.op("dve", lambda e: e.tensor_scalar(pr8[:, 3, :], pr8[:, 1, :], -1.0, 1.0, ALU.mult, ALU.add), reads=[pr8], pwrites=[pr8])
                seqs = [(0, cfg.n_ctx), (cfg.n_ctx, n)]
                blocks = []
                for (s0, s1) in seqs:
                    for t0 in range(s0, s1, 512):
                        blocks.append((t0, min(512, s1 - t0), s0, s1))
                xi = 0
                for (t0, tw, s0, s1) in blocks:
                    for c in range(27):
                        x_ = xh[xi % 2]
                        s_ = ssum[xi % 2]
                        xi += 1
                        lo, hi = max(t0 - 1, s0), min(t0 + tw + 1, s1)
                        if lo > t0 - 1 or hi < t0 + tw + 1:
                            kb.op("pool", lambda e, x_=x_: e.memset(x_[:], 0.0), writes=[x_])
                            wr = dict(pwrites=[x_])
                        else:
                            wr = dict(writes=[x_])
                        kb.dma("sp", x_[:, lo - (t0 - 1):hi - (t0 - 1)], PT[r0_ + c * P:r0_ + (c + 1) * P, lo:hi], reads=[PT], **wr)
                        kb.op("pool", lambda e, x_=x_, s_=s_: e.tensor_tensor(s_[:, 0:tw], x_[:, 0:tw], x_[:, 2:tw + 2], ALU.add), reads=[x_], writes=[s_])
                        kb.op("dve", lambda e, x_=x_, c=c: e.tensor_scalar(PP[:, c, 0:tw], x_[:, 1:tw + 1], mu[:, c, 0:1], None, ALU.mult), reads=[x_, mu], pwrites=[PP])
                        kb.op("dve", lambda e, s_=s_, c=c: e.scalar_tensor_tensor(out=PP[:, c, 0:tw], in0=s_[:, 0:tw], scalar=mu[:, c, 1:2], in1=PP[:, c, 0:tw], op0=ALU.mult, op1=ALU.add),
                              reads=[s_, mu, PP], pwrites=[PP])
                    sto = lambda dst, c, src_t: kb.dma("sp", dst[c * P:(c + 1) * P, t0:t0 + tw], src_t, reads=[], pwrites=[dst])
                    for c in range(8):
                        kb.dma("sp", RW["R"][c * P:(c + 1) * P, t0:t0 + tw], PP[:, c, 0:tw], reads=[PP], pwrites=[RW["R"]])
                        kb.dma("sp", RW["V"][c * P:(c + 1) * P, t0:t0 + tw], PP[:, 16 + c, 0:tw], reads=[PP], pwrites=[RW["V"]])
                    wi_ = 0
                    for c in range(8):
                        a_, b_ = wk[wi_ % 6], wk[(wi_ + 1) % 6]
                        wi_ += 2
                        kb.op("dve", lambda e, a_=a_, c=c: e.tensor_scalar(a_[:, 0:tw], PP[:, 8 + c, 0:tw], pr8[:, 0, c:c + 1], None, ALU.mult), reads=[PP, pr8], writes=[a_])
                        kb.op("pool", lambda e, a_=a_, b_=b_: e.tensor_tensor(b_[:, 0:tw], a_[:, 0:tw], a_[:, 0:tw], ALU.mult), reads=[a_], writes=[b_])
                        kb.op("pe", lambda e, b_=b_: e.matmul(PS[0][:, 0:tw], bo64[:, :], b_[:, 0:tw], start=True, stop=True), reads=[bo64, b_], writes=[PS[0]])
                        kb.op("dve", lambda e, b_=b_: e.tensor_scalar(b_[:, 0:tw], PS[0][:, 0:tw], 1e-6, None, ALU.add), reads=[PS[0]], writes=[b_])
                        kb.op("act", lambda e, b_=b_: e.activation(out=b_[:, 0:tw], in_=b_[:, 0:tw], func=AF.Sqrt), reads=[b_], writes=[b_])
                        kb.op("dve", lambda e, b_=b_: e.reciprocal(b_[:, 0:tw], b_[:, 0:tw]), reads=[b_], writes=[b_])
                        kb.op("dve", lambda e, a_=a_, b_=b_, c=c: e.tensor_tensor(kkc[:, c, 0:tw], a_[:, 0:tw], b_[:, 0:tw], ALU.mult), reads=[a_, b_], pwrites=[kkc])
                        kb.dma("sp", RW["KK"][c * P:(c + 1) * P, t0:t0 + tw], kkc[:, c, 0:tw], reads=[kkc], pwrites=[RW["KK"]])
                    kb.op("act", lambda e: e.activation(out=tl[:, 0:tw], in_=PP[:, 26, 0:tw], func=AF.Sigmoid), reads=[PP], writes=[tl])
                    for c in range(8):
                        a_ = wk[wi_ % 6]
                        wi_ += 1
                        ps = PS[1 + c % 2]
                        kb.op("pe", lambda e, ps=ps, c=c: e.matmul(ps[:, 0:tw], G2t[:, c * P:(c + 1) * P], tl[:, 0:tw], start=True, stop=True), reads=[G2t, tl], writes=[ps])
                        kb.op("act", lambda e, ps=ps, a_=a_: e.activation(out=a_[:, 0:tw], in_=ps[:, 0:tw], func=AF.Copy), reads=[ps], writes=[a_])
                        kb.dma("sp", RW["G"][c * P:(c + 1) * P, t0:t0 + tw], a_[:, 0:tw], reads=[a_], pwrites=[RW["G"]])
                        b_ = wk[wi_ % 6]
                        wi_ += 1
                        kb.op("dve", lambda e, b_=b_, c=c: e.scalar_tensor_tensor(out=b_[:, 0:tw], in0=PP[:, c, 0:tw], scalar=pr8[:, 2, c:c + 1], in1=PP[:, 8 + c, 0:tw], op0=ALU.mult, op1=ALU.mult),
                              reads=[PP, pr8], writes=[b_])
                        ps2 = PS[3 + c % 2]
                        kb.op("pe", lambda e, ps2=ps2, b_=b_: e.matmul(ps2[:, 0:tw], bo64[:, :], b_[:, 0:tw], start=True, stop=True), reads=[bo64, b_], writes=[ps2])
                        kb.op("dve", lambda e, ps2=ps2, b_=b_, c=c: e.tensor_tensor(b_[:, 0:tw], ps2[:, 0:tw], PP[:, 16 + c, 0:tw], ALU.mult), reads=[ps2, PP], writes=[b_])
                        kb.dma("sp", RW["BON"][c * P:(c + 1) * P, t0:t0 + tw], b_[:, 0:tw], reads=[b_], pwrites=[RW["BON"]])
                    for d in range(2):
                        hs = slice(64 * d, 64 * d + 64)
                        kb.fence_pe()
                        kb.op("act", lambda e, hs=hs: e.activation(out=tl[hs, 0:tw], in_=PP[hs, 24, 0:tw], func=AF.Tanh), reads=[PP], writes=[tl])
                        for c in range(8):
                            a_, b_, c_ = wk[wi_ % 6], wk[(wi_ + 1) % 6], wk[(wi_ + 2) % 6]
                            wi_ += 3
                            ps = PS[1 + c % 2]
                            kb.op("pe", lambda e, ps=ps, c=c, hs=hs: e.matmul(ps[:, 0:tw], W2t[hs, c * P:(c + 1) * P], tl[hs, 0:tw], start=True, stop=True), reads=[W2t, tl], writes=[ps])
                            kb.op("act", lambda e, ps=ps, a_=a_, c=c, d=d: e.activation(out=a_[:, 0:tw], in_=ps[:, 0:tw], func=AF.Sigmoid, bias=w0a0[:, 0, d, c:c + 1], scale=1.0), reads=[ps, w0a0], writes=[a_])
                            kb.op("dve", lambda e, a_=a_: e.tensor_scalar(a_[:, 0:tw], a_[:, 0:tw], -0.6065306597126334, None, ALU.mult), reads=[a_], writes=[a_])
                            kb.dma("sp", RW["LW%d" % d][c * P:(c + 1) * P, t0:t0 + tw], a_[:, 0:tw], reads=[a_], pwrites=[RW["LW%d" % d]])
                            ps2 = PS[3 + c % 2]
                            kb.op("pe", lambda e, ps2=ps2, c=c, hs=hs: e.matmul(ps2[:, 0:tw], A2t[hs, c * P:(c + 1) * P], PP[hs, 25, 0:tw], start=True, stop=True), reads=[A2t, PP], writes=[ps2])
                            kb.op("act", lambda e, ps2=ps2, b_=b_, c=c, d=d: e.activation(out=b_[:, 0:tw], in_=ps2[:, 0:tw], func=AF.Sigmoid, bias=w0a0[:, 1, d, c:c + 1], scale=1.0), reads=[ps2, w0a0], writes=[b_])
                            kb.op("dve", lambda e, b_=b_, c_=c_, c=c: e.tensor_scalar(c_[:, 0:tw], b_[:, 0:tw], pr8[:, 1, c:c + 1], pr8[:, 3, c:c + 1], ALU.mult, ALU.add), reads=[b_, pr8], writes=[c_])
                            kb.op("pool", lambda e, c_=c_, c=c: e.tensor_tensor(c_[:, 0:tw], c_[:, 0:tw], PP[:, 8 + c, 0:tw], ALU.mult), reads=[c_, PP], writes=[c_])
                            kb.dma("sp", RW["KH%d" % d][c * P:(c + 1) * P, t0:t0 + tw], c_[:, 0:tw], reads=[c_], pwrites=[RW["KH%d" % d]])
                            kb.op("pool", lambda e, b_=b_, c=c: e.tensor_tensor(b_[:, 0:tw], b_[:, 0:tw], kkc[:, c, 0:tw], ALU.mult), reads=[b_, kkc], writes=[b_])
                            kb.dma("sp", RW["BB%d" % d][c * P:(c + 1) * P, t0:t0 + tw], b_[:, 0:tw], reads=[b_], pwrites=[RW["BB%d" % d]])
                kb.barrier()

            with ExitStack() as ph:
              if not getattr(cfg, 'rwkv_only_pre', False):
               try:
                CUT = getattr(cfg, 'rwkv_cut', 0)
                def cut(k_):
                    if CUT == k_:
                        raise StopIteration
                mk = {k_: cmask(ph, k_) for k_ in ("BD", "LINC0", "LINC1", "LSTR0", "LSTR1")}
                CATR = [kb.sb(ph, "w_cat%d" % d, [P, 386], F32) for d in range(2)]
                for d in range(2):
                    kb.dma("sp", CATR[d][:, 0:128], c_masks[MI["LINC%d" % d], :, :], reads=[c_masks], writes=[CATR[d]])
                    kb.dma("sp", CATR[d][:, 128:256], c_masks[MI["LSTR%d" % d], :, :], reads=[c_masks], pwrites=[CATR[d]])
                    kb.dma("sp", CATR[d][:, 256:384], c_masks[MI["MEND%d" % d], :, :], reads=[c_masks], pwrites=[CATR[d]])
                    kb.dma("sp", CATR[d][:, 384:386], c_bdcol[:, :], reads=[c_bdcol], pwrites=[CATR[d]])
                lnp = kb.sb(ph, "w_lnp", [P, 2, 8], F32)
                kb.dma("sp", lnp[:], rwkv_lnT[l, :, :, :], reads=[rwkv_lnT], writes=[lnp])
                ldn = ("R", "V", "KK", "LW", "KH", "BB")
                ld = {nm: kb.sb(ph, "w_%s" % nm, [P, 8, P], F32) for nm in ldn}
                lwtm = kb.sb(ph, "w_lwtm", [P, 8, P], F32)
                Wg = [kb.sb(ph, "w_Wg%d" % i, [P, 4, P], F32) for i in range(4)]
                dec8 = kb.sb(ph, "w_dec8", [P, 8, 2], F32)
                AR = kb.sb(ph, "w_AR", [P, 8, 256], F32)
                BtT = kb.sb(ph, "w_BtT", [P, 8, P], F32)
                KtT = kb.sb(ph, "w_KtT", [P, 8, P], F32)
                BtT2 = kb.sb(ph, "w_BtT2", [P, 8, 2, P], F32)
                KtT2 = kb.sb(ph, "w_KtT2", [P, 8, 2, P], F32)
                bdc2 = kb.sb(ph, "w_bdc2", [P, 2], F32)
                kb.dma("sp", bdc2[:], c_bdcol[:, :], reads=[c_bdcol], writes=[bdc2])
                BeT = kb.sb(ph, "w_BeT", [P, 8, P], F32)
                KeT = kb.sb(ph, "w_KeT", [P, 8, P], F32)
                vtm = kb.sb(ph, "w_vtm", [P, 8, P], F32)
                Atm = kb.sb(ph, "w_Atm", [P, 8, P], F32)
                Betm = kb.sb(ph, "w_Betm", [P, 8, P], F32)
                Ketm = kb.sb(ph, "w_Ketm", [P, 8, P], F32)
                zq = [kb.sb(ph, "w_zq%d" % i, [P, 4, 384], F32) for i in range(2)]
                Y16 = kb.sb(ph, "w_Y16", [P, 16, P], F32)
                PrbT = kb.sb(ph, "w_PrbT", [P, 16, P], F32)
                MakT = kb.sb(ph, "w_MakT", [P, 4, P], F32)
                PrkT = kb.sb(ph, "w_PrkT", [P, 4, P], F32)
                nmv = kb.sb(ph, "w_nmv", [P, 16, 64], F32)
                O0 = kb.sb(ph, "w_O0", [P, 16, 64], F32)
                U0 = kb.sb(ph, "w_U0", [P, 16, 64], F32)
                WmT8 = kb.sb(ph, "w_WmT8", [P, 8, P], F32)
                T8 = kb.sb(ph, "w_T8", [P, 8, P], F32)
                Uu = kb.sb(ph, "w_Uu", [P, 8, P], F32)
                tmp8 = kb.sb(ph, "w_tmp8", [P, 8, P], F32)
                Yo = kb.sb(ph, "w_Yo", [P, 16, 64], F32)
                ofr = kb.sb(ph, "w_ofr", [P, 16, 64], F32)
                bon = kb.sb(ph, "w_bon", [P, 8, P], F32)
                gg = kb.sb(ph, "w_gg", [P, 8, P], F32)
                st16 = kb.sb(ph, "w_st16", [P, 4, 16], F32)
                ob8 = [kb.sb(ph, "w_ob%d" % i, [P, 8, P], BF16) for i in range(2)]
                b8 = lambda t_: [PS[i] for i in t_]
                pv = lambda b_, nb: PSA[:, b_:b_ + nb, :].rearrange("p b (h t) -> p (b h) t", t=P)
                for d in range(2):
                    LINCd, LSTRd, LSTRt = mk["LINC%d" % d], mk["LSTR%d" % d], mk["LSTR%d" % (1 - d)]
                    kb.op("dve", lambda e: e.memset(T8[:], 0.0), writes=[T8])
                    for ui, u in enumerate(unit_order(d)):
                        t0 = u * P
                        for nm in ldn:
                            srcT = RW[nm] if nm in ("R", "V", "KK") else RW["%s%d" % (nm, d)]
                            kb.dma("sp", ld[nm][:], srcT[:, t0:t0 + P].rearrange("(c p) t -> p c t", p=P), reads=[srcT], writes=[ld[nm]])
                        if d == 1:
                            kb.dma("sp", ofr[:], OFR[t0:t0 + P, :].rearrange("t (h v) -> t h v", h=16), reads=[OFR], writes=[ofr])
                            kb.dma("sp", bon[:], RW["BON"][:, t0:t0 + P].rearrange("(c p) t -> p c t", p=P), reads=[RW["BON"]], writes=[bon])
                            kb.dma("sp", gg[:], RW["G"][:, t0:t0 + P].rearrange("(c p) t -> p c t", p=P), reads=[RW["G"]], writes=[gg])
                        for c in range(8):
                            kb.op("pe", lambda e, c=c: e.transpose(PS[c // 4][:, (c % 4) * P:(c % 4 + 1) * P], ld["LW"][:, c, :], ident_f[:]), reads=[ld["LW"], ident_f],
                                  writes=[PS[c // 4]] if c % 4 == 0 else (), pwrites=() if c % 4 == 0 else [PS[c // 4]])
                            kb.op("pe", lambda e, c=c: e.transpose(PS[2 + c // 4][:, (c % 4) * P:(c % 4 + 1) * P], ld["V"][:, c, :], ident_f[:]), reads=[ld["V"], ident_f],
                                  writes=[PS[2 + c // 4]] if c % 4 == 0 else (), pwrites=() if c % 4 == 0 else [PS[2 + c // 4]])
                        kb.op("act", lambda e: e.activation(out=lwtm[:], in_=pv(0, 2), func=AF.Copy), reads=b8((0, 1)), writes=[lwtm])
                        kb.op("act", lambda e: e.activation(out=vtm[:], in_=pv(2, 2), func=AF.Copy), reads=b8((2, 3)), writes=[vtm])
                        cut(1)
                        for g4 in range(2):
                            cs = slice(4 * g4, 4 * g4 + 4)
                            for cc in range(4):
                                c = 4 * g4 + cc
                                kb.op("pe", lambda e, c=c, cc=cc: e.matmul(PS[4 + cc][:, 0:386], lwtm[:, c, :], CATR[d][:, :], start=True, stop=True), reads=[lwtm, CATR[d]], writes=[PS[4 + cc]])
                            pb4 = b8((4, 5, 6, 7))
                            kb.op("act", lambda e: e.activation(out=Wg[0][:], in_=PSA[:, 4:8, 0:128], func=AF.Exp), reads=pb4, writes=[Wg[0]])
                            kb.op("act", lambda e: e.activation(out=Wg[1][:], in_=PSA[:, 4:8, 0:128], func=AF.Exp, scale=-1.0), reads=pb4, writes=[Wg[1]])
                            kb.op("act", lambda e: e.activation(out=Wg[2][:], in_=PSA[:, 4:8, 128:256], func=AF.Exp), reads=pb4, writes=[Wg[2]])
                            kb.op("act", lambda e: e.activation(out=Wg[3][:], in_=PSA[:, 4:8, 256:384], func=AF.Exp), reads=pb4, writes=[Wg[3]])
                            kb.op("act", lambda e, cs=cs: e.activation(out=dec8[:, cs, :], in_=PSA[:, 4:8, 384:386], func=AF.Exp), reads=pb4, pwrites=[dec8])
                            kb.op("dve", lambda e, cs=cs: e.tensor_tensor(AR[:, cs, 0:128], ld["KK"][:, cs, :], Wg[2][:], ALU.mult), reads=[ld["KK"], Wg[2]], pwrites=[AR])
                            kb.op("dve", lambda e, cs=cs: e.tensor_tensor(AR[:, cs, 128:256], ld["R"][:, cs, :], Wg[0][:], ALU.mult), reads=[ld["R"], Wg[0]], pwrites=[AR])
                            kb.op("pool", lambda e, cs=cs: e.tensor_tensor(BtT[:, cs, :], ld["BB"][:, cs, :], Wg[1][:], ALU.mult), reads=[ld["BB"], Wg[1]], pwrites=[BtT])
                            kb.op("pool", lambda e, cs=cs: e.tensor_tensor(KtT[:, cs, :], ld["KH"][:, cs, :], Wg[1][:], ALU.mult), reads=[ld["KH"], Wg[1]], pwrites=[KtT])
                            kb.op("dve", lambda e, cs=cs: e.tensor_tensor(BeT[:, cs, :], ld["BB"][:, cs, :], Wg[3][:], ALU.mult), reads=[ld["BB"], Wg[3]], pwrites=[BeT])
                            kb.op("pool", lambda e, cs=cs: e.tensor_tensor(KeT[:, cs, :], ld["KH"][:, cs, :], Wg[3][:], ALU.mult), reads=[ld["KH"], Wg[3]], pwrites=[KeT])
                        kb.op("dve", lambda e: e.tensor_tensor(BtT2[:], bc(BtT[:], [P, 8, 2, P], [2]), bc(bdc2[:], [P, 8, 2, P], [1, 3]), ALU.mult), reads=[BtT, bdc2], writes=[BtT2])
                        kb.op("pool", lambda e: e.tensor_tensor(KtT2[:], bc(KtT[:], [P, 8, 2, P], [2]), bc(bdc2[:], [P, 8, 2, P], [1, 3]), ALU.mult), reads=[KtT, bdc2], writes=[KtT2])
                        cut(2)
                        for srcT_, dst in ((AR, Atm), (BeT, Betm), (KeT, Ketm)):
                            for c in range(8):
                                in_ap = srcT_[:, c, 0:128] if srcT_ is AR else srcT_[:, c, :]
                                kb.op("pe", lambda e, c=c, in_ap=in_ap: e.transpose(PS[c // 4][:, (c % 4) * P:(c % 4 + 1) * P], in_ap, ident_f[:]), reads=[srcT_, ident_f],
                                      writes=[PS[c // 4]] if c % 4 == 0 else (), pwrites=() if c % 4 == 0 else [PS[c // 4]])
                            kb.op("act", lambda e, dst=dst: e.activation(out=dst[:], in_=pv(0, 2), func=AF.Copy), reads=b8((0, 1)), writes=[dst])
                        cut(3)
                        for qd in range(4):
                            z = zq[qd % 2]
                            for hh in range(4):
                                h = 4 * qd + hh
                                c, two = h // 2, h % 2
                                kb.op("pe", lambda e, c=c, two=two, hh=hh: e.matmul(PS[0][:, hh * P:(hh + 1) * P], AR[:, c, 0:128], BtT2[:, c, two, :], start=True, stop=True), reads=[AR, BtT2],
                                      writes=[PS[0]] if hh == 0 else (), pwrites=() if hh == 0 else [PS[0]])
                                pbb = PS[1 + hh // 2]
                                for x2 in range(2):
                                    kb.op("pe", lambda e, c=c, two=two, hh=hh, pbb=pbb, x2=x2: e.matmul(pbb[:, (hh % 2) * 256 + x2 * P:(hh % 2) * 256 + (x2 + 1) * P], BtT2[:, c, two, :], AR[:, c, x2 * P:(x2 + 1) * P], start=True, stop=True), reads=[AR, BtT2],
                                          writes=[pbb] if (hh % 2 == 0 and x2 == 0) else (), pwrites=() if (hh % 2 == 0 and x2 == 0) else [pbb])
                                pkk = PS[3 + hh // 2]
                                for x2 in range(2):
                                    kb.op("pe", lambda e, c=c, two=two, hh=hh, pkk=pkk, x2=x2: e.matmul(pkk[:, (hh % 2) * 256 + x2 * P:(hh % 2) * 256 + (x2 + 1) * P], KtT2[:, c, two, :], AR[:, c, x2 * P:(x2 + 1) * P], start=True, stop=True), reads=[AR, KtT2],
                                          writes=[pkk] if (hh % 2 == 0 and x2 == 0) else (), pwrites=() if (hh % 2 == 0 and x2 == 0) else [pkk])
                                h = 4 * qd + hh
                                c, hs = h // 2, slice(64 * (h % 2), 64 * (h % 2) + 64)
                                kb.op("pe", lambda e, c=c, hs=hs, hh=hh: e.matmul(PS[0][:, hh * P:(hh + 1) * P], AR[hs, c, 0:128], BtT[hs, c, :], start=True, stop=True), reads=[AR, BtT],
                                      writes=[PS[0]] if hh == 0 else (), pwrites=() if hh == 0 else [PS[0]])
                                pbb = PS[1 + hh // 2]
                                kb.op("pe", lambda e, c=c, hs=hs, hh=hh, pbb=pbb: e.matmul(pbb[:, (hh % 2) * 256:(hh % 2 + 1) * 256], BtT[hs, c, :], AR[hs, c, :], start=True, stop=True), reads=[AR, BtT],
                                      writes=[pbb] if hh % 2 == 0 else (), pwrites=() if hh % 2 == 0 else [pbb])
                                pkk = PS[3 + hh // 2]
                                kb.op("pe", lambda e, c=c, hs=hs, hh=hh, pkk=pkk: e.matmul(pkk[:, (hh % 2) * 256:(hh % 2 + 1) * 256], KtT[hs, c, :], AR[hs, c, :], start=True, stop=True), reads=[AR, KtT],
                                      writes=[pkk] if hh % 2 == 0 else (), pwrites=() if hh % 2 == 0 else [pkk])
                            kb.fence_pe()
                            pB = PSA[:, 1:3, :].rearrange("p b (h x) -> p (b h) x", x=256)
                            cut(41)
                            pK = PSA[:, 3:5, :].rearrange("p b (h x) -> p (b h) x", x=256)
                            kb.op("dve", lambda e, z=z: e.tensor_tensor(z[:, :, 256:384], PS[0][:, :].rearrange("p (h t) -> p h t", h=4), bc(LSTRt[:], [P, 4, P], [1]), ALU.mult), reads=[PS[0], LSTRt], writes=[z])
                            kb.op("dve", lambda e, z=z: e.tensor_tensor(z[:, :, 128:256], pB[:, :, 0:128], bc(LSTRd[:], [P, 4, P], [1]), ALU.mult), reads=b8((1, 2)) + [LSTRd], pwrites=[z])
                            kb.op("dve", lambda e, qd=qd: e.tensor_tensor(PrbT[:, 4 * qd:4 * qd + 4, :], pB[:, :, 128:256], bc(LINCd[:], [P, 4, P], [1]), ALU.mult), reads=b8((1, 2)) + [LINCd], pwrites=[PrbT])
                            kb.op("dve", lambda e: e.tensor_tensor(MakT[:], pK[:, :, 0:128], bc(LSTRd[:], [P, 4, P], [1]), ALU.mult), reads=b8((3, 4)) + [LSTRd], writes=[MakT])
                            kb.op("dve", lambda e: e.tensor_tensor(PrkT[:], pK[:, :, 128:256], bc(LINCd[:], [P, 4, P], [1]), ALU.mult), reads=b8((3, 4)) + [LINCd], writes=[PrkT])
                            kb.op("pool", lambda e, z=z: e.tensor_tensor(z[:, :, 0:128], bc(ident_f[:], [P, 4, P], [1]), z[:, :, 128:256], ALU.subtract), reads=[ident_f, z], pwrites=[z])
                            cut(42)
                            for hh in range(4):
                                h = 4 * qd + hh
                                kb.op("pe", lambda e, h=h, hh=hh: e.matmul(PS[5][:, hh * 64:(hh + 1) * 64], MakT[:, hh, :], vtm[:, h // 2, 64 * (h % 2):64 * (h % 2) + 64], start=True, stop=True), reads=[MakT, vtm],
                                      writes=[PS[5]] if hh == 0 else (), pwrites=() if hh == 0 else [PS[5]])
                                kb.op("pe", lambda e, h=h, hh=hh: e.matmul(PS[5][:, 256 + hh * 64:256 + (hh + 1) * 64], PrkT[:, hh, :], vtm[:, h // 2, 64 * (h % 2):64 * (h % 2) + 64], start=True, stop=True), reads=[PrkT, vtm],
                                      pwrites=[PS[5]])
                            cut(43)
                            if True:
                                kb.op("dve", lambda e, qd=qd: e.tensor_scalar(nmv[:, 4 * qd:4 * qd + 4, :], PS[5][:, 0:256].rearrange("p (h v) -> p h v", h=4), -1.0, None, ALU.mult), reads=[PS[5]], pwrites=[nmv])
                            if True:
                                kb.op("act", lambda e, qd=qd: e.activation(out=O0[:, 4 * qd:4 * qd + 4, :], in_=PS[5][:, 256:512].rearrange("p (h v) -> p h v", h=4), func=AF.Copy), reads=[PS[5]], pwrites=[O0])
                            cut(4)
                            if qd % 2 == 1:
                                emit_solve(zq, [0, 4])
                                for i2 in range(2):
                                    q2 = qd - 1 + i2
                                    kb.op("pool", lambda e, i2=i2, q2=q2: e.tensor_copy(Y16[:, 4 * q2:4 * q2 + 4, :], zq[i2][:, :, 0:128]), reads=[zq[i2]], pwrites=[Y16])
                                cut(5)
                        for h in range(16):
                            kb.op("pe", lambda e, h=h: e.matmul(PS[h // 8][:, (h % 8) * 64:(h % 8 + 1) * 64], Y16[:, h, :], nmv[:, h, :], start=True, stop=True), reads=[Y16, nmv],
                                  writes=[PS[h // 8]] if h % 8 == 0 else (), pwrites=() if h % 8 == 0 else [PS[h // 8]])
                            kb.op("pe", lambda e, h=h: e.matmul(PS[2 + h // 4][:, (h % 4) * P:(h % 4 + 1) * P], Atm[:, h // 2, :], Y16[:, h, :], start=True, stop=True), reads=[Y16, Atm],
                                  writes=[PS[2 + h // 4]] if h % 4 == 0 else (), pwrites=() if h % 4 == 0 else [PS[2 + h // 4]])
                        kb.op("act", lambda e: e.activation(out=U0[:], in_=PSA[:, 0:2, :].rearrange("p b (h v) -> p (b h) v", v=64), func=AF.Copy), reads=b8((0, 1)), writes=[U0])
                        wsrc = PSA[:, 2:6, :].rearrange("p b (h t) -> p (b h) t", t=P)
                        wsrc2 = wsrc.rearrange("p (c two) t -> p c two t", two=2)
                        kb.op("dve", lambda e: e.tensor_copy(WmT8[0:64, :, :], wsrc2[0:64, :, 0, :]), reads=b8((2, 3, 4, 5)), writes=[WmT8])
                        kb.op("dve", lambda e: e.tensor_copy(WmT8[64:128, :, :], wsrc2[64:128, :, 1, :]), reads=b8((2, 3, 4, 5)), pwrites=[WmT8])
                        cut(6)
                        U0p = U0[:].rearrange("p (c two) v -> p c (two v)", two=2)
                        O0p = O0[:].rearrange("p (c two) v -> p c (two v)", two=2)
                        Yop = Yo[:].rearrange("p (c two) v -> p c (two v)", two=2)
                        for s in ((0, 1) if d == 0 else (1, 0)):
                            rs = slice(64 * s, 64 * s + 64)
                            kb.fence_pe()
                            for c in range(8):
                                kb.op("pe", lambda e, c=c: e.matmul(PS[c // 4][:, (c % 4) * P:(c % 4 + 1) * P], WmT8[:, c, :], T8[:, c, :], start=True, stop=True), reads=[WmT8, T8],
                                      writes=[PS[c // 4]] if c % 4 == 0 else (), pwrites=() if c % 4 == 0 else [PS[c // 4]])
                            kb.op("dve", lambda e, rs=rs: e.tensor_tensor(Uu[rs, :, :], U0p[rs, :, :], PSA[rs, 0:2, :].rearrange("p b (h t) -> p (b h) t", t=P), ALU.subtract), reads=[U0] + b8((0, 1)), writes=[Uu])
                            for c in range(8):
                                po = PS[2 + c // 4]
                                for two in range(2):
                                    h = 2 * c + two
                                    first = (c % 4 == 0 and two == 0)
                                    kb.op("pe", lambda e, c=c, po=po, two=two: e.matmul(po[:, (c % 4) * P + two * 64:(c % 4) * P + two * 64 + 64], AR[:, c, 128:256], T8[:, c, two * 64:two * 64 + 64], start=True, stop=False), reads=[AR, T8],
                                          writes=[po] if first else (), pwrites=() if first else [po])
                                    kb.op("pe", lambda e, c=c, po=po, h=h, two=two, rs=rs: e.matmul(po[:, (c % 4) * P + two * 64:(c % 4) * P + two * 64 + 64], PrbT[rs, h, :], Uu[rs, c, two * 64:two * 64 + 64], start=False, stop=True),
                                          reads=[PrbT, Uu], pwrites=[po])
                            kb.op("dve", lambda e, rs=rs: e.tensor_tensor(Yop[rs, :, :], O0p[rs, :, :], PSA[rs, 2:4, :].rearrange("p b (h t) -> p (b h) t", t=P), ALU.add), reads=[O0] + b8((2, 3)), pwrites=[Yo])
                            for c in range(8):
                                pt_ = PS[4 + c // 4]
                                kb.op("pe", lambda e, c=c, pt_=pt_, rs=rs: e.matmul(pt_[:, (c % 4) * P:(c % 4 + 1) * P], Betm[rs, c, :], Uu[rs, c, :], start=True, stop=False), reads=[Betm, Uu],
                                      writes=[pt_] if c % 4 == 0 else (), pwrites=() if c % 4 == 0 else [pt_])
                                kb.op("pe", lambda e, c=c, pt_=pt_, rs=rs: e.matmul(pt_[:, (c % 4) * P:(c % 4 + 1) * P], Ketm[rs, c, :], vtm[rs, c, :], start=False, stop=True), reads=[Ketm, vtm], pwrites=[pt_])
                            kb.op("dve", lambda e: e.tensor_tensor(tmp8[:], pv(4, 2), bc(mk["BD"][:], [P, 8, P], [1]), ALU.mult), reads=b8((4, 5)) + [mk["BD"]], writes=[tmp8])
                            kb.op("pool", lambda e, s=s: e.tensor_tensor(T8[:], T8[:], bc(dec8[:, :, s], [P, 8, P], [2]), ALU.mult), reads=[T8, dec8], writes=[T8])
                            kb.op("dve", lambda e: e.tensor_tensor(T8[:], T8[:], tmp8[:], ALU.add), reads=[T8, tmp8], writes=[T8])
                        if d == 0:
                            kb.dma("sp", OFR[t0:t0 + P, :].rearrange("t (h v) -> t h v", h=16), Yo[:], reads=[Yo], pwrites=[OFR])
                        else:
                            kb.op("dve", lambda e: e.tensor_tensor(Yo[:], Yo[:], ofr[:], ALU.add), reads=[Yo, ofr], writes=[Yo])
                            kb.op("dve", lambda e: e.tensor_reduce(out=st16[:, 0, :], in_=Yo[:], axis=mybir.AxisListType.X, op=ALU.add), reads=[Yo], writes=[st16])
                            kb.op("pool", lambda e: e.tensor_tensor(ofr[:], Yo[:], Yo[:], ALU.mult), reads=[Yo], writes=[ofr])
                            kb.op("dve", lambda e: e.tensor_reduce(out=st16[:, 1, :], in_=ofr[:], axis=mybir.AxisListType.X, op=ALU.add), reads=[ofr], pwrites=[st16])
                            kb.op("dve", lambda e: e.tensor_scalar(st16[:, 0, :], st16[:, 0, :], 1.0 / 64.0, None, ALU.mult), reads=[st16], pwrites=[st16])
                            kb.op("dve", lambda e: e.tensor_tensor(st16[:, 2, :], st16[:, 0, :], st16[:, 0, :], ALU.mult), reads=[st16], pwrites=[st16])
                            kb.op("dve", lambda e: e.scalar_tensor_tensor(out=st16[:, 1, :], in0=st16[:, 1, :], scalar=1.0 / 64.0, in1=st16[:, 2, :], op0=ALU.mult, op1=ALU.subtract), reads=[st16], pwrites=[st16])
                            kb.op("dve", lambda e: e.tensor_scalar(st16[:, 1, :], st16[:, 1, :], 64e-5, None, ALU.add), reads=[st16], pwrites=[st16])
                            kb.op("act", lambda e: e.activation(out=st16[:, 1, :], in_=st16[:, 1, :], func=AF.Sqrt), reads=[st16], pwrites=[st16])
                            kb.op("dve", lambda e: e.reciprocal(st16[:, 1, :], st16[:, 1, :]), reads=[st16], pwrites=[st16])
                            kb.op("dve", lambda e: e.tensor_tensor(Yo[:], Yo[:], bc(st16[:, 0, :], [P, 16, 64], [2]), ALU.subtract), reads=[Yo, st16], writes=[Yo])
                            kb.op("dve", lambda e: e.tensor_tensor(Yo[:], Yo[:], bc(st16[:, 1, :], [P, 16, 64], [2]), ALU.mult), reads=[Yo, st16], writes=[Yo])
                            for c in range(8):
                                kb.op("pe", lambda e, c=c: e.transpose(PS[c // 4][:, (c % 4) * P:(c % 4 + 1) * P], Yop[:, c, :], ident_f[:]), reads=[Yo, ident_f],
                                      writes=[PS[c // 4]] if c % 4 == 0 else (), pwrites=() if c % 4 == 0 else [PS[c // 4]])
                            kb.op("dve", lambda e: e.tensor_tensor(tmp8[:], pv(0, 2), bc(lnp[:, 0, :], [P, 8, P], [2]), ALU.mult), reads=b8((0, 1)) + [lnp], writes=[tmp8])
                            kb.op("pool", lambda e: e.tensor_tensor(tmp8[:], tmp8[:], bc(lnp[:, 1, :], [P, 8, P], [2]), ALU.add), reads=[tmp8, lnp], writes=[tmp8])
                            kb.op("pool", lambda e: e.tensor_tensor(tmp8[:], tmp8[:], bon[:], ALU.add), reads=[tmp8, bon], writes=[tmp8])
                            ob_ = ob8[ui % 2]
                            kb.op("dve", lambda e, ob_=ob_: e.tensor_tensor(ob_[:], tmp8[:], gg[:], ALU.mult), reads=[tmp8, gg], writes=[ob_])
                            kb.dma("sp", OT[1024:2048, t0:t0 + P].rearrange("(c p) t -> p c t", p=P), ob_[:], reads=[ob_], pwrites=[OT])
               except StopIteration:
                pass
                kb.barrier()

        if "merge" in phases:
            with ExitStack() as ph:
                otb = [kb.sb(ph, "otb%d" % i, [P, 24, 512], BF16) for i in range(2)]
                wbr = [kb.sb(ph, "wbr%d" % i, [P, 24, 512], BF16) for i in range(2)]
                gp = [kb.sb(ph, "gp%d" % i, [P, 512], F32) for i in range(3)]
                sg = [kb.sb(ph, "sg%d" % i, [P, 512], F32) for i in range(3)]
                acc = [kb.sb(ph, "acc%d" % i, [P, 512], F32) for i in range(2)]
                tmp = [kb.sb(ph, "tmpm%d" % i, [P, 512], F32) for i in range(2)]
                accb = [kb.sb(ph, "accb%d" % i, [P, 512], BF16) for i in range(2)]
                bi = wi = gi = ai = pi = 0
                for t0 in range(0, n, 512):
                    tw = min(512, n - t0)
                    ot = otb[bi % 2]
                    bi += 1
                    kb.dma("sp", ot[:, :, 0:tw], OT[:, t0:t0 + tw].rearrange("(c p) t -> p c t", p=P),
                           reads=[OT], writes=[ot])
                    for fg in range(4):
                        w = wbr[wi % 2]
                        wi += 1
                        for m in range(3):
                            kb.dma("pool", w[:, m * 8:(m + 1) * 8, :],
                                   w_branch[l, m, :, fg * 512:(fg + 1) * 512].rearrange("(k p) c -> p k c", p=P),
                                   reads=[w_branch], writes=[w] if m == 0 else (), pwrites=() if m == 0 else [w])
                        for f in range(4):
                            fo = fg * 4 + f
                            a_t, a_b = acc[ai % 2], accb[ai % 2]
                            ai += 1
                            for m in range(3):
                                ps = PS[pi % 4]
                                pi += 1
                                g_t, s_t = gp[gi % 3], sg[gi % 3]
                                gi += 1
                                r0 = cfg.gate_off + m * D + fo * P
                                kb.dma("sp", g_t[:, 0:tw], PT[r0:r0 + P, t0:t0 + tw], reads=[PT], writes=[g_t])
                                kb.op("act", lambda e, g_t=g_t, s_t=s_t: e.activation(out=s_t[:, 0:tw], in_=g_t[:, 0:tw], func=AF.Sigmoid),
                                      reads=[g_t], writes=[s_t])
                                for k in range(8):
                                    kb.op("pe", lambda e, ps=ps, w=w, ot=ot, m=m, k=k, f=f: e.matmul(
                                        ps[:, 0:tw], w[:, m * 8 + k, f * P:(f + 1) * P], ot[:, m * 8 + k, 0:tw],
                                        start=(k == 0), stop=(k == 7)),
                                        reads=[w, ot], writes=[ps] if k == 0 else (), pwrites=() if k == 0 else [ps])
                                if m == 0:
                                    kb.op("dve", lambda e, a_t=a_t, s_t=s_t, ps=ps: e.tensor_tensor(a_t[:, 0:tw], s_t[:, 0:tw], ps[:, 0:tw], ALU.mult),
                                          reads=[s_t, ps], writes=[a_t])
                                else:
                                    t_t = tmp[m % 2]
                                    kb.op("dve", lambda e, t_t=t_t, s_t=s_t, ps=ps: e.tensor_tensor(t_t[:, 0:tw], s_t[:, 0:tw], ps[:, 0:tw], ALU.mult),
                                          reads=[s_t, ps], writes=[t_t])
                                    if m == 1:
                                        kb.op("pool", lambda e, a_t=a_t, t_t=t_t: e.tensor_tensor(a_t[:, 0:tw], a_t[:, 0:tw], t_t[:, 0:tw], ALU.add),
                                              reads=[a_t, t_t], writes=[a_t])
                                    else:
                                        kb.op("pool", lambda e, a_t=a_t, a_b=a_b, t_t=t_t: e.tensor_tensor(a_b[:, 0:tw], a_t[:, 0:tw], t_t[:, 0:tw], ALU.add),
                                              reads=[a_t, t_t], writes=[a_b])
                            kb.dma("sp", ACC[fo * P:(fo + 1) * P, t0:t0 + tw], a_b[:, 0:tw], reads=[a_b], pwrites=[ACC])
                kb.barrier()

        if "merge" in phases:
            xsrc = xin if l == 0 else XR
            with ExitStack() as ph:
                accs = [kb.sb(ph, "accs%d" % i, [P, 16, 512], BF16) for i in range(2)]
                wob = [kb.sb(ph, "wob%d" % i, [P, 16, 512], BF16) for i in range(2)]
                xts = [kb.sb(ph, "xts%d" % i, [P, D], F32) for i in range(4)]
                g1b = [kb.sb(ph, "g1b%d" % r, [P, D], F32) for r in range(2)]
                s2b = [kb.sb(ph, "s2b%d" % r, [P, D], F32) for r in range(2)]
                sh2b = [kb.sb(ph, "sh2b%d" % r, [P, D], F32) for r in range(2)]
                lng = kb.sb(ph, "lng", [P, D], F32)
                lnb = kb.sb(ph, "lnb", [P, D], F32)
                tm2 = [kb.sb(ph, "tm2%d" % i, [P, 512], F32) for i in range(2)]
                hh = [kb.sb(ph, "hh%d" % i, [P, D], F32) for i in range(2)]
                stt = [kb.sb(ph, "stt%d" % i, [P, 4, 6], F32) for i in range(2)]
                mvt = [kb.sb(ph, "mvt%d" % i, [P, 4], F32) for i in range(2)]
                for r in range(2):
                    kb.dma("sp", g1b[r][:], MODD[r:r + 1, 2 * D:3 * D].partition_broadcast(P), reads=[MODD], writes=[g1b[r]])
                    kb.dma("sp", s2b[r][:], MODD[r:r + 1, 4 * D:5 * D].partition_broadcast(P), reads=[MODD], writes=[s2b[r]])
                    kb.dma("sp", sh2b[r][:], MODD[r:r + 1, 3 * D:4 * D].partition_broadcast(P), reads=[MODD], writes=[sh2b[r]])
                kb.dma("sp", lng[:], ln1_g[l, :, :].partition_broadcast(P), reads=[ln1_g], writes=[lng])
                kb.dma("sp", lnb[:], ln1_b[l, :, :].partition_broadcast(P), reads=[ln1_b], writes=[lnb])
                bi = wi = pi = ti2 = hi = 0
                for t0 in range(0, n, 512):
                    tw = min(512, n - t0)
                    ntl = tw // P
                    a_s = accs[bi % 2]
                    bi += 1
                    kb.dma("sp", a_s[:, :, 0:tw], ACC[:, t0:t0 + tw].rearrange("(k p) t -> p k t", p=P), reads=[ACC], writes=[a_s])
                    for tt in range(ntl):
                        gt = t0 // P + tt
                        kb.dma("sp", xts[tt][:], xsrc[gt * P:(gt + 1) * P, :], reads=[xsrc], writes=[xts[tt]])
                    for og in range(4):
                        wo = wob[wi % 2]
                        wi += 1
                        kb.dma("pool", wo[:], w_out[l, :, og * 512:(og + 1) * 512].rearrange("(k p) c -> p k c", p=P),
                               reads=[w_out], writes=[wo])
                        for tt in range(ntl):
                            gt = t0 // P + tt
                            r = 1 if tile_is_ctx(gt) else 0
                            ps = PS[pi % 4]
                            pi += 1
                            for k in range(16):
                                kb.op("pe", lambda e, ps=ps, a_s=a_s, wo=wo, k=k, tt=tt: e.matmul(
                                    ps[:, :], a_s[:, k, tt * P:(tt + 1) * P], wo[:, k, :], start=(k == 0), stop=(k == 15)),
                                    reads=[a_s, wo], writes=[ps] if k == 0 else (), pwrites=() if k == 0 else [ps])
                            t_t = tm2[ti2 % 2]
                            ti2 += 1
                            x_t = xts[tt]
                            kb.op("dve", lambda e, t_t=t_t, ps=ps, r=r, og=og: e.tensor_tensor(t_t[:], ps[:, :], g1b[r][:, og * 512:(og + 1) * 512], ALU.mult),
                                  reads=[ps, g1b[r]], writes=[t_t])
                            kb.op("dve", lambda e, t_t=t_t, x_t=x_t, og=og: e.scalar_tensor_tensor(
                                out=x_t[:, og * 512:(og + 1) * 512], in0=x_t[:, og * 512:(og + 1) * 512], scalar=cfg_alpha,
                                in1=t_t[:], op0=ALU.mult, op1=ALU.add), reads=[t_t, x_t], writes=[x_t])
                    for tt in range(ntl):
                        gt = t0 // P + tt
                        r = 1 if tile_is_ctx(gt) else 0
                        x_t = xts[tt]
                        h_t = hh[hi % 2]
                        st_t, mv_t = stt[hi % 2], mvt[hi % 2]
                        hi += 1
                        layer_norm(kb, x_t, st_t, mv_t, lng, lnb, 1e-5)
                        kb.dma("sp", XM[gt * P:(gt + 1) * P, :], x_t[:], reads=[x_t], pwrites=[XM])
                        kb.op("pool", lambda e, h_t=h_t, x_t=x_t, r=r: e.tensor_tensor(h_t[:], x_t[:], s2b[r][:], ALU.mult),
                              reads=[x_t, s2b[r]], writes=[h_t])
                        kb.op("pool", lambda e, h_t=h_t, r=r: e.tensor_tensor(h_t[:], h_t[:], sh2b[r][:], ALU.add),
                              reads=[h_t, sh2b[r]], writes=[h_t])
                        kb.dma("sp", H2[gt * P:(gt + 1) * P, :], h_t[:], reads=[h_t], pwrites=[H2])
                kb.barrier()


        if "moe" in phases:
            NT = n // P
            B = cfg.dblk
            de = cfg.d_expert
            nfc = de // P
            with ExitStack() as ph:
                h2t = [kb.sb(ph, "h2t%d" % i, [P, D], F32) for i in range(2)]
                h2T = [kb.sb(ph, "h2T%d" % i, [P, 16, P], F32) for i in range(2)]
                rw = kb.sb(ph, "rw", [P, 16, 16], F32)
                rbb = kb.sb(ph, "rbb", [P, 16], F32)
                E12 = kb.sb(ph, "E12", [P, NT, 32], F32)
                W12 = kb.sb(ph, "W12", [P, NT, 2], F32)
                ET = kb.sb(ph, "ET", [16, n], F32)
                RK = kb.sb(ph, "RK", [16, n], F32)
                on16 = kb.sb(ph, "on16", [16, 2048], F32)
                sm = [kb.sb(ph, "sm%d" % i, [P, 160], F32) for i in range(2)]
                cst = kb.sb(ph, "cst", [16, 8], F32)
                ut16 = kb.sb(ph, "ut16", [16, 16], F32)
                bbrow = kb.sb(ph, "bbrow", [16, cfg.n_blk], F32)
                ge = kb.sb(ph, "ge", [16, cfg.n_blk], F32)
                on16p = kb.sb(ph, "on16p", [16, P], F32)
                eoff = kb.sb(ph, "eoff", [P, cfg.n_blk], F32)
                det = kb.sb(ph, "det", [P, 16], F32)
                slf = [kb.sb(ph, "slf%d" % i, [P, 20], F32) for i in range(2)]
                sli = [[kb.sb(ph, "sli%d_%d" % (i, j), [P, 1], I32) for j in range(2)] for i in range(2)]
                kb.dma("sp", rw[:], router_w[:, :].rearrange("(k p) e -> p k e", p=P), reads=[router_w], writes=[rw])
                kb.dma("sp", rbb[:], router_bias[:, :].partition_broadcast(P), reads=[router_bias], writes=[rbb])
                kb.dma("sp", ut16[:], c_ut16[:, :], reads=[c_ut16], writes=[ut16])
                kb.dma("sp", bbrow[:], c_bbrow[:, :], reads=[c_bbrow], writes=[bbrow])
                kb.op("dve", lambda e: e.memset(on16[:], 1.0), writes=[on16])
                kb.op("dve", lambda e: e.memset(on16p[:], 1.0), writes=[on16p])
                for tt in range(NT):
                    x_t, xT = h2t[tt % 2], h2T[tt % 2]
                    s = sm[tt % 2]
                    kb.dma("sp", x_t[:], H2[tt * P:(tt + 1) * P, :], reads=[H2], writes=[x_t])
                    for q4 in range(4):
                        ps = PS[4 + q4 % 2]
                        for kk in range(4):
                            k = q4 * 4 + kk
                            kb.op("pe", lambda e, ps=ps, x_t=x_t, k=k, kk=kk: e.transpose(ps[:, kk * P:(kk + 1) * P], x_t[:, k * P:(k + 1) * P], ident_f[:]),
                                  reads=[x_t, ident_f], writes=[ps] if kk == 0 else (), pwrites=() if kk == 0 else [ps])
                        kb.op("act", lambda e, ps=ps, xT=xT, q4=q4: e.activation(out=xT[:, q4 * 4:(q4 + 1) * 4, :], in_=ps[:, :].rearrange("p (k t) -> p k t", k=4), func=AF.Copy),
                              reads=[ps], writes=[xT] if q4 == 0 else (), pwrites=() if q4 == 0 else [xT])
                    pl = PS[6]
                    for k in range(16):
                        kb.op("pe", lambda e, pl=pl, xT=xT, k=k: e.matmul(pl[:, 0:16], xT[:, k, :], rw[:, k, :], start=(k == 0), stop=(k == 15)),
                              reads=[xT, rw], writes=[pl] if k == 0 else (), pwrites=() if k == 0 else [pl])
                    E1 = E12[:, tt, 0:16]
                    E2 = E12[:, tt, 16:32]
                    kb.op("act", lambda e, s=s, pl=pl: e.activation(out=s[:, 0:16], in_=pl[:, 0:16], func=AF.Sigmoid), reads=[pl], writes=[s])
                    kb.op("dve", lambda e, s=s: e.tensor_tensor(s[:, 16:32], s[:, 0:16], rbb[:], ALU.add), reads=[s, rbb], writes=[s])
                    kb.op("dve", lambda e, s=s: e.tensor_reduce(out=s[:, 64:68], in_=s[:, 16:32].rearrange("p (g e) -> p g e", g=4), axis=mybir.AxisListType.X, op=ALU.max),
                          reads=[s], writes=[s])
                    for g4 in range(4):
                        kb.op("dve", lambda e, s=s, g4=g4: e.tensor_scalar(s[:, 32 + 4 * g4:36 + 4 * g4], s[:, 16 + 4 * g4:20 + 4 * g4], s[:, 64 + g4:65 + g4], None, ALU.is_equal),
                              reads=[s], writes=[s])
                    kb.op("dve", lambda e, s=s: e.scalar_tensor_tensor(out=s[:, 48:64], in0=s[:, 32:48], scalar=-1.0e9, in1=s[:, 16:32], op0=ALU.mult, op1=ALU.add),
                          reads=[s], writes=[s])
                    kb.op("dve", lambda e, s=s: e.tensor_reduce(out=s[:, 68:72], in_=s[:, 48:64].rearrange("p (g e) -> p g e", g=4), axis=mybir.AxisListType.X, op=ALU.max),
                          reads=[s], writes=[s])
                    kb.op("dve", lambda e, s=s: e.tensor_tensor(s[:, 72:76], s[:, 64:68], s[:, 68:72], ALU.add), reads=[s], writes=[s])
                    kb.op("dve", lambda e, s=s: e.tensor_reduce(out=s[:, 76:77], in_=s[:, 72:76], axis=mybir.AxisListType.X, op=ALU.max), reads=[s], writes=[s])
                    kb.op("dve", lambda e, s=s: e.tensor_scalar(s[:, 80:84], s[:, 72:76], s[:, 76:77], None, ALU.is_ge), reads=[s], writes=[s])
                    for g4 in range(4):
                        kb.op("dve", lambda e, s=s, g4=g4, E1=E1: e.tensor_scalar(E1[:, 4 * g4:4 * g4 + 4], s[:, 32 + 4 * g4:36 + 4 * g4], s[:, 80 + g4:81 + g4], None, ALU.mult),
                              reads=[s], pwrites=[E12])
                        kb.op("dve", lambda e, s=s, g4=g4, E2=E2: e.tensor_scalar(E2[:, 4 * g4:4 * g4 + 4], s[:, 48 + 4 * g4:52 + 4 * g4], s[:, 68 + g4:69 + g4], s[:, 80 + g4:81 + g4], ALU.is_equal, ALU.mult),
                              reads=[s], pwrites=[E12])
                    kb.op("dve", lambda e, s=s, E1=E1: e.tensor_tensor(s[:, 96:112], E1, s[:, 0:16], ALU.mult), reads=[s, E12], writes=[s])
                    kb.op("dve", lambda e, s=s: e.tensor_reduce(out=s[:, 112:113], in_=s[:, 96:112], axis=mybir.AxisListType.X, op=ALU.add), reads=[s], writes=[s])
                    kb.op("dve", lambda e, s=s, E2=E2: e.tensor_tensor(s[:, 96:112], E2, s[:, 0:16], ALU.mult), reads=[s, E12], writes=[s])
                    kb.op("dve", lambda e, s=s: e.tensor_reduce(out=s[:, 113:114], in_=s[:, 96:112], axis=mybir.AxisListType.X, op=ALU.add), reads=[s], writes=[s])
                    kb.op("dve", lambda e, s=s: e.tensor_tensor(s[:, 114:115], s[:, 112:113], s[:, 113:114], ALU.add), reads=[s], writes=[s])
                    kb.op("dve", lambda e, s=s: e.reciprocal(s[:, 115:116], s[:, 114:115]), reads=[s], writes=[s])
                    kb.op("dve", lambda e, s=s, tt=tt: e.tensor_scalar(W12[:, tt, 0:2], s[:, 112:114], s[:, 115:116], None, ALU.mult), reads=[s], pwrites=[W12])
                    kb.op("dve", lambda e, s=s, E1=E1, E2=E2: e.tensor_tensor(s[:, 96:112], E1, E2, ALU.add), reads=[s, E12], writes=[s])
                    pt_ = PS[7]
                    kb.op("pe", lambda e, pt_=pt_, s=s: e.transpose(pt_[0:16, 0:P], s[:, 96:112], ident_f[:]), reads=[s, ident_f], writes=[pt_])
                    kb.op("act", lambda e, pt_=pt_, tt=tt: e.activation(out=ET[:, tt * P:(tt + 1) * P], in_=pt_[0:16, 0:P], func=AF.Copy), reads=[pt_], pwrites=[ET])
                for s0 in range(0, n, 2048):
                    sw = min(2048, n - s0)
                    init = 0.0 if s0 == 0 else RK[:, s0 - 1:s0]
                    kb.op("dve", lambda e, s0=s0, sw=sw, init=init: e.tensor_tensor_scan(out=RK[:, s0:s0 + sw], data0=on16[:, 0:sw], data1=ET[:, s0:s0 + sw], initial=init, op0=ALU.mult, op1=ALU.add),
                          reads=[on16, ET, RK], pwrites=[RK])
                kb.op("dve", lambda e: e.tensor_scalar(ge[:], bbrow[:], RK[:, n - 1:n], None, ALU.is_lt), reads=[bbrow, RK], writes=[ge])
                kb.op("dve", lambda e: e.tensor_reduce(out=cst[:, 1:2], in_=ge[:], axis=mybir.AxisListType.X, op=ALU.add), reads=[ge], writes=[cst])
                kb.op("dve", lambda e: e.tensor_scalar(cst[:, 3:4], cst[:, 1:2], float(B), None, ALU.mult), reads=[cst], writes=[cst])
                pq = PS[6]
                kb.op("pe", lambda e: e.matmul(pq[0:16, 0:1], ut16[:], cst[:, 3:4], start=True, stop=True), reads=[ut16, cst], writes=[pq])
                kb.op("dve", lambda e: e.tensor_copy(cst[:, 4:5], pq[0:16, 0:1]), reads=[pq], writes=[cst])
                kb.op("dve", lambda e: e.tensor_tensor(cst[:, 5:6], cst[:, 4:5], cst[:, 3:4], ALU.add), reads=[cst], writes=[cst])
                kb.op("dve", lambda e: e.tensor_scalar(RK[:], RK[:], cst[:, 4:5], -1.0, ALU.add, ALU.add), reads=[RK, cst], writes=[RK])
                kb.op("dve", lambda e: e.tensor_scalar(ge[:], bbrow[:], cst[:, 5:6], None, ALU.is_ge), reads=[bbrow, cst], writes=[ge])
                kb.op("pe", lambda e: e.matmul(pq[:, 0:cfg.n_blk], on16p[:], ge[:], start=True, stop=True), reads=[on16p, ge], writes=[pq])
                kb.op("dve", lambda e: e.tensor_scalar(eoff[:], pq[:, 0:cfg.n_blk], 15.0, None, ALU.min), reads=[pq], writes=[eoff])
                kb.dma("sp", EOFF[:, :], eoff[:], reads=[eoff], writes=[EOFF])
                for tt in range(NT):
                    pt_ = PS[7]
                    kb.op("pe", lambda e, pt_=pt_, tt=tt: e.transpose(pt_[:, 0:16], RK[:, tt * P:(tt + 1) * P], ident_f[0:16, 0:16]), reads=[RK, ident_f], writes=[pt_])
                    sf = slf[tt % 2]
                    kb.op("dve", lambda e, pt_=pt_: e.tensor_copy(det[:], pt_[:, 0:16]), reads=[pt_], writes=[det])
                    for j in range(2):
                        kb.op("dve", lambda e, sf=sf, j=j, tt=tt: e.tensor_tensor(sf[:, 0:16], det[:], E12[:, tt, 16 * j:16 * j + 16], ALU.mult), reads=[det, E12], writes=[sf])
                        kb.op("dve", lambda e, sf=sf, j=j: e.tensor_reduce(out=sf[:, 16 + j:17 + j], in_=sf[:, 0:16], axis=mybir.AxisListType.X, op=ALU.add), reads=[sf], writes=[sf])
                        si = sli[tt % 2][j]
                        kb.op("dve", lambda e, sf=sf, j=j, si=si: e.tensor_copy(si[:], sf[:, 16 + j:17 + j]), reads=[sf], writes=[si])
                        kb.dma("sp", SLOT[tt, j, :, :], si[:], reads=[si], pwrites=[SLOT])
                kb.dma("sp", W12D[:, :, :], W12[:], reads=[W12], writes=[W12D])
                kb.barrier()

            with ExitStack() as ph:
                h2t = [kb.sb(ph, "h2u%d" % i, [P, D], F32) for i in range(2)]
                h2b = [kb.sb(ph, "h2b%d" % i, [P, D], BF16) for i in range(2)]
                sli = [[kb.sb(ph, "slj%d_%d" % (i, j), [P, 1], I32) for j in range(2)] for i in range(2)]
                for tt in range(NT):
                    x_t, x_b = h2t[tt % 2], h2b[tt % 2]
                    kb.dma("sp", x_t[:], H2[tt * P:(tt + 1) * P, :], reads=[H2], writes=[x_t])
                    kb.op("act", lambda e, x_t=x_t, x_b=x_b: e.activation(out=x_b[:], in_=x_t[:], func=AF.Copy), reads=[x_t], writes=[x_b])
                    for j in range(2):
                        si = sli[tt % 2][j]
                        kb.dma("sp", si[:], SLOT[tt, j, :, :], reads=[SLOT], writes=[si])
                        kb.idma(XS[:, :], bass.IndirectOffsetOnAxis(ap=si[:, :], axis=0), x_b[:, :], None, cfg.n_slot,
                                reads=[x_b, si], pwrites=[XS])
                kb.barrier()

            with ExitStack() as ph:
                eo = kb.sb(ph, "eo", [P, cfg.n_blk], F32)
                eo1 = kb.sb(ph, "eo1", [P, cfg.n_blk], F32)
                eo2 = kb.sb(ph, "eo2", [P, cfg.n_blk], F32)
                idb = kb.sb(ph, "idb", [P, 32], F32)
                idf = [kb.sb(ph, "idf%d" % i, [P, 16 + 4 * nfc], F32) for i in range(2)]
                idi = [[kb.sb(ph, "idi%d_%d" % (i, j), [P, 1], I32) for j in range(16 + 4 * nfc)] for i in range(2)]
                xs = [kb.sb(ph, "xs%d" % i, [P, D], BF16) for i in range(2)]
                xT = kb.sb(ph, "xTb", [P, 16, B], BF16)
                wA = kb.sb(ph, "wA", [P, 16, de], BF16)
                wB = kb.sb(ph, "wB", [P, 16, de], BF16)
                w2t = [kb.sb(ph, "w2t%d" % i, [P, nfc, 512], BF16) for i in range(2)]
                s1T = kb.sb(ph, "s1T", [P, nfc, B], BF16)
                actT = kb.sb(ph, "actT", [P, nfc, B], BF16)
                yo = [kb.sb(ph, "yo%d" % i, [P, 512], F32) for i in range(2)]
                kb.dma("sp", eo[:], EOFF[:, :], reads=[EOFF], writes=[eo])
                kb.dma("sp", idb[:], c_idb[:, :], reads=[c_idb], writes=[idb])
                kb.op("dve", lambda e: e.tensor_scalar(eo1[:], eo[:], float(D), float(l * 16 * D), ALU.mult, ALU.add), reads=[eo], writes=[eo1])
                kb.op("dve", lambda e: e.tensor_scalar(eo2[:], eo[:], float(4 * de), float(l * 16 * de * 4), ALU.mult, ALU.add), reads=[eo], writes=[eo2])
                xi = w2i = yi = pi = 0
                for b in range(cfg.n_blk):
                    ii = b % 2
                    kb.op("dve", lambda e, b=b, ii=ii: e.tensor_scalar(idf[ii][:, 0:16], idb[:, 0:16], eo1[:, b:b + 1], None, ALU.add),
                          reads=[eo1, idb], writes=[idf[ii]])
                    for og in range(4):
                        kb.op("dve", lambda e, b=b, ii=ii, og=og: e.tensor_scalar(idf[ii][:, 16 + og * nfc:16 + (og + 1) * nfc], idb[:, 0:nfc], 4.0, eo2[:, b:b + 1], ALU.mult, ALU.add),
                              reads=[eo2, idb], pwrites=[idf[ii]])
                        if og:
                            kb.op("dve", lambda e, ii=ii, og=og: e.tensor_scalar(idf[ii][:, 16 + og * nfc:16 + (og + 1) * nfc], idf[ii][:, 16 + og * nfc:16 + (og + 1) * nfc], float(og), None, ALU.add),
                                  reads=[idf[ii]], pwrites=[idf[ii]])
                    for j in range(16 + 4 * nfc):
                        kb.op("dve", lambda e, ii=ii, j=j: e.tensor_copy(idi[ii][j][:], idf[ii][:, j:j + 1]), reads=[idf[ii]], writes=[idi[ii][j]])
                    for st_ in range(B // P):
                        x_s = xs[xi % 2]
                        xi += 1
                        kb.dma("sp", x_s[:], XS[b * B + st_ * P:b * B + (st_ + 1) * P, :], reads=[XS], writes=[x_s])
                        for q4 in range(4):
                            ps = PS[4 + q4 % 2]
                            for kk in range(4):
                                k = q4 * 4 + kk
                                o = ps[:].bitcast(BF16)[:, kk * P:(kk + 1) * P]
                                kb.op("pe", lambda e, o=o, x_s=x_s, k=k: e.transpose(o, x_s[:, k * P:(k + 1) * P], ident_b[:]),
                                      reads=[x_s, ident_b], writes=[ps] if kk == 0 else (), pwrites=() if kk == 0 else [ps])
                            src_ = ps[:].bitcast(BF16)[:, 0:4 * P].rearrange("p (k t) -> p k t", k=4)
                            kb.op("act", lambda e, src_=src_, q4=q4, st_=st_: e.activation(out=xT[:, q4 * 4:(q4 + 1) * 4, st_ * P:(st_ + 1) * P], in_=src_, func=AF.Copy),
                                  reads=[ps], pwrites=[xT])
                    for wsel, (w, wt_) in enumerate(((wA, moe_w1), (wB, moe_w3))):
                        for k in range(16):
                            kb.idma(w[:, k, :], None, wt_[:, :], bass.IndirectOffsetOnAxis(ap=idi[ii][k][:, :], axis=0), L * 16 * D,
                                    reads=[wt_, idi[ii][k]], writes=[w] if k == 0 else (), pwrites=() if k == 0 else [w])
                        for fc in range(nfc):
                            ps = PS[pi % 4]
                            pi += 1
                            for k in range(16):
                                kb.op("pe", lambda e, ps=ps, w=w, k=k, fc=fc: e.matmul(ps[:, 0:B], w[:, k, fc * P:(fc + 1) * P], xT[:, k, :], start=(k == 0), stop=(k == 15)),
                                      reads=[w, xT], writes=[ps] if k == 0 else (), pwrites=() if k == 0 else [ps])
                            if wsel == 0:
                                kb.op("act", lambda e, ps=ps, fc=fc: e.activation(out=s1T[:, fc, :], in_=ps[:, 0:B], func=AF.Silu), reads=[ps], pwrites=[s1T])
                            else:
                                kb.op("dve", lambda e, ps=ps, fc=fc: e.tensor_tensor(actT[:, fc, :], s1T[:, fc, :], ps[:, 0:B], ALU.mult), reads=[s1T, ps], pwrites=[actT])
                    for og in range(4):
                        w2_ = w2t[w2i % 2]
                        w2i += 1
                        for fc in range(nfc):
                            ix = idi[ii][16 + og * nfc + fc]
                            kb.idma(w2_[:, fc, :], None, moe_w2[:, :], bass.IndirectOffsetOnAxis(ap=ix[:, :], axis=0), L * 16 * de * 4,
                                    reads=[moe_w2, ix], writes=[w2_] if fc == 0 else (), pwrites=() if fc == 0 else [w2_])
                        for st_ in range(B // P):
                            ps = PS[6 + pi % 2]
                            pi += 1
                            for fc in range(nfc):
                                kb.op("pe", lambda e, ps=ps, w2_=w2_, fc=fc, st_=st_: e.matmul(ps[:, :], actT[:, fc, st_ * P:(st_ + 1) * P], w2_[:, fc, :], start=(fc == 0), stop=(fc == nfc - 1)),
                                      reads=[actT, w2_], writes=[ps] if fc == 0 else (), pwrites=() if fc == 0 else [ps])
                            y_t = yo[yi % 2]
                            yi += 1
                            kb.op("act", lambda e, y_t=y_t, ps=ps: e.activation(out=y_t[:], in_=ps[:, :], func=AF.Copy), reads=[ps], writes=[y_t])
                            kb.dma("sp", YS[b * B + st_ * P:b * B + (st_ + 1) * P, og * 512:(og + 1) * 512], y_t[:], reads=[y_t], pwrites=[YS])
                kb.barrier()

            with ExitStack() as ph:
                y1 = [kb.sb(ph, "y1_%d" % i, [P, D], F32) for i in range(2)]
                y2 = [kb.sb(ph, "y2_%d" % i, [P, D], F32) for i in range(2)]
                xm = [kb.sb(ph, "xm_%d" % i, [P, D], F32) for i in range(2)]
                g2b = [kb.sb(ph, "g2b%d" % r, [P, D], F32) for r in range(2)]
                lng = kb.sb(ph, "lng2", [P, D], F32)
                lnb = kb.sb(ph, "lnb2", [P, D], F32)
                w12 = kb.sb(ph, "w12", [P, NT, 2], F32)
                sli = [[kb.sb(ph, "slk%d_%d" % (i, j), [P, 1], I32) for j in range(2)] for i in range(2)]
                stt = [kb.sb(ph, "stu%d" % i, [P, 4, 6], F32) for i in range(2)]
                mvt = [kb.sb(ph, "mvu%d" % i, [P, 4], F32) for i in range(2)]
                for r in range(2):
                    kb.dma("sp", g2b[r][:], MODD[r:r + 1, 5 * D:6 * D].partition_broadcast(P), reads=[MODD], writes=[g2b[r]])
                kb.dma("sp", lng[:], ln2_g[l, :, :].partition_broadcast(P), reads=[ln2_g], writes=[lng])
                kb.dma("sp", lnb[:], ln2_b[l, :, :].partition_broadcast(P), reads=[ln2_b], writes=[lnb])
                kb.dma("sp", w12[:], W12D[:, :, :], reads=[W12D], writes=[w12])
                for tt in range(NT):
                    r = 1 if tile_is_ctx(tt) else 0
                    a1, a2, x_m = y1[tt % 2], y2[tt % 2], xm[tt % 2]
                    for j, dst in ((0, a1), (1, a2)):
                        si = sli[tt % 2][j]
                        kb.dma("sp", si[:], SLOT[tt, j, :, :], reads=[SLOT], writes=[si])
                        kb.idma(dst[:, :], None, YS[:, :], bass.IndirectOffsetOnAxis(ap=si[:, :], axis=0), cfg.n_slot, reads=[YS, si], writes=[dst])
                    kb.dma("sp", x_m[:], XM[tt * P:(tt + 1) * P, :], reads=[XM], writes=[x_m])
                    kb.op("dve", lambda e, a1=a1, tt=tt: e.tensor_scalar(a1[:], a1[:], w12[:, tt, 0:1], None, ALU.mult), reads=[a1, w12], writes=[a1])
                    kb.op("dve", lambda e, a1=a1, a2=a2, tt=tt: e.scalar_tensor_tensor(out=a1[:], in0=a2[:], scalar=w12[:, tt, 1:2], in1=a1[:], op0=ALU.mult, op1=ALU.add),
                          reads=[a1, a2, w12], writes=[a1])
                    kb.op("pool", lambda e, a1=a1, r=r: e.tensor_tensor(a1[:], a1[:], g2b[r][:], ALU.mult), reads=[a1, g2b[r]], writes=[a1])
                    kb.op("dve", lambda e, a1=a1, x_m=x_m: e.scalar_tensor_tensor(out=x_m[:], in0=x_m[:], scalar=cfg_alpha, in1=a1[:], op0=ALU.mult, op1=ALU.add),
                          reads=[a1, x_m], writes=[x_m])
                    layer_norm(kb, x_m, stt[tt % 2], mvt[tt % 2], lng, lnb, 1e-5)
                    if l == L - 1 and not per_layer:
                        if tt * P >= cfg.n_ctx:
                            kb.dma("sp", yout[tt * P - cfg.n_ctx:(tt + 1) * P - cfg.n_ctx, :], x_m[:], reads=[x_m], pwrites=[yout])
                    else:
                        kb.dma("sp", XR[tt * P:(tt + 1) * P, :], x_m[:], reads=[x_m], pwrites=[XR])
                kb.barrier()

    kb.finish()
    es.close()
    return nc, kb


ALL_PHASES = ("mod", "inproj", "gla", "gdn", "rwkv", "merge", "moe")
_PROG = {}


def _layer_inputs(inp, l, consts):
    f32 = lambda a: np.ascontiguousarray(np.asarray(a, dtype=np.float32))
    D = 2048
    d = {}
    d["w_ada"] = f32(inp["w_ada"][l:l + 1])
    d["b_ada"] = f32(inp["b_ada"][l:l + 1]).reshape(1, 1, 6 * D)
    d["w_in"] = f32(inp["w_in"][l:l + 1])
    b_pad = np.zeros((132 * P,), np.float32)
    b_pad[:inp["b_in"].shape[1]] = np.asarray(inp["b_in"][l], np.float32)
    d["b_inT"] = np.ascontiguousarray(b_pad.reshape(1, 132, P).transpose(0, 2, 1))
    d["gla_dec_w"] = f32(inp["gla_dec_w"][l:l + 1])
    d["gla_dec_b"] = f32(inp["gla_dec_b"][l:l + 1]).reshape(1, 2, 1, 512)
    d["gla_norm_wT"] = np.ascontiguousarray(f32(inp["gla_norm_w"][l:l + 1]).reshape(1, 2, P).transpose(0, 2, 1))
    prm = dict(mu=inp["rwkv_mu"][l:l + 1], w2=inp["rwkv_w2"][l:l + 1], w0=inp["rwkv_w0"][l:l + 1], a2=inp["rwkv_a2"][l:l + 1],
               a0=inp["rwkv_a0"][l:l + 1], g2=inp["rwkv_g2"][l:l + 1], kk=inp["rwkv_kk"][l:l + 1], ka=inp["rwkv_ka"][l:l + 1],
               rk=inp["rwkv_rk"][l:l + 1], ln_w=inp["rwkv_ln_w"][l:l + 1], ln_b=inp["rwkv_ln_b"][l:l + 1])
    prm = {k: f32(v) for k, v in prm.items()}
    d.update(rwkv_layout(prm))
    d["gdn_conv_wT"] = np.ascontiguousarray(f32(inp["gdn_conv_w"][l:l + 1]).reshape(1, 9, 24, P).transpose(0, 2, 3, 1))
    d["gdn_a_log"] = f32(inp["gdn_a_log"][l:l + 1]).reshape(1, 1, 16)
    d["gdn_dt_bias"] = f32(inp["gdn_dt_bias"][l:l + 1]).reshape(1, 1, 16)
    d["gdn_norm_wT"] = f32(inp["gdn_norm_w"][l:l + 1]).reshape(1, P, 1)
    d["w_branch"] = f32(inp["w_branch"][l:l + 1])
    d["w_out"] = f32(inp["w_out"][l:l + 1])
    for k in ("ln1_g", "ln1_b", "ln2_g", "ln2_b"):
        d[k] = f32(inp[k][l:l + 1]).reshape(1, 1, D)
    d["router_w"] = f32(inp["router_w"])
    d["router_bias"] = f32(inp["router_bias"]).reshape(1, 16)
    de = inp["moe_w1"].shape[-1]
    d["moe_w1"] = f32(inp["moe_w1"][l]).reshape(16 * D, de)
    d["moe_w3"] = f32(inp["moe_w3"][l]).reshape(16 * D, de)
    d["moe_w2"] = f32(inp["moe_w2"][l]).reshape(16 * de * 4, 512)
    d.update(consts)
    return d


def kernel(**inputs):
    x = np.asarray(inputs["x"], np.float32)
    ctx = np.asarray(inputs["ctx"], np.float32)
    n_lat, n_ctx = x.shape[1], ctx.shape[1]
    depth = inputs["w_ada"].shape[0]
    de = inputs["moe_w1"].shape[-1]
    key = (n_ctx, n_lat, de)
    if key not in _PROG:
        cfg = Cfg(n_ctx=n_ctx, n_lat=n_lat, depth=1, d_expert=de)
        cfg.per_layer = True
        nc, kb = build_program(cfg, phases=ALL_PHASES, dbg=("XR",))
        _PROG[key] = (cfg, nc)
    cfg, nc = _PROG[key]
    consts = make_consts(cfg)
    X = np.ascontiguousarray(np.concatenate([ctx[0], x[0]], axis=0))
    ccT = np.ascontiguousarray(np.stack([np.asarray(inputs["c"], np.float32)[0], np.asarray(inputs["c_ctx"], np.float32)], axis=1))
    for l in range(depth):
        ins = _layer_inputs(inputs, l, consts)
        ins["xin"] = X
        ins["ccT"] = ccT
        res = run_bass_kernel_spmd(nc, [ins], core_ids=[0])
        X = np.ascontiguousarray(np.asarray(res.results[0]["XR"], np.float32))
    return X[n_ctx:].reshape(1, n_lat, -1).astype(np.float32)


def make_consts(cfg):
    c = {}
    c["identD"] = np.eye(P, dtype=np.float32)
    c["c_ut16"] = np.triu(np.ones((16, 16), np.float32), 1)
    c["c_bbrow"] = np.tile((np.arange(cfg.n_blk, dtype=np.float32) * cfg.dblk)[None, :], (16, 1))
    c["c_idb"] = (np.arange(32, dtype=np.float32)[None, :] * P + np.arange(P, dtype=np.float32)[:, None]).astype(np.float32)
    idx = np.arange(P)
    sub = idx // 64
    BD = (sub[:, None] == sub[None, :]).astype(np.float32)
    m = {"BD": BD}
    for d in range(2):
        le = (idx[:, None] <= idx[None, :]) if d == 0 else (idx[:, None] >= idx[None, :])
        lt = (idx[:, None] < idx[None, :]) if d == 0 else (idx[:, None] > idx[None, :])
        LINC = BD * le
        LSTR = BD * lt
        mid = sub * 64 + (31 if d == 0 else 32)
        m["LINC%d" % d] = LINC
        m["LSTR%d" % d] = LSTR
        m["MREL%d" % d] = LINC - LINC[:, mid]
        m["MEND%d" % d] = BD - LINC
        m["NEG%d" % d] = (LSTR - 1.0) * 3.0e4
        m["NEGI%d" % d] = (LINC - 1.0) * 3.0e4
    c["c_masks"] = np.stack([m[k] for k in MASKS]).astype(np.float32)
    c["c_bdcol"] = np.stack([(sub == 0), (sub == 1)], 1).astype(np.float32)
    return c


def rwkv_layout(prm):
    L = prm["mu"].shape[0]
    t8 = lambda a: np.ascontiguousarray(a.reshape(L, 8, P).transpose(0, 2, 1))
    out = {}
    out["rwkv_muT"] = np.ascontiguousarray(prm["mu"].reshape(L, 27, P).transpose(0, 2, 1))
    out["rwkv_w2"] = np.ascontiguousarray(prm["w2"])
    out["rwkv_a2"] = np.ascontiguousarray(prm["a2"])
    out["rwkv_g2"] = np.ascontiguousarray(prm["g2"])
    z8 = np.zeros((L, P, 8), np.float32)
    out["rwkv_p4T"] = np.ascontiguousarray(np.stack([t8(prm["kk"]), t8(prm["ka"]), t8(prm["rk"].reshape(L, 1024)), z8], 2))
    w0 = np.stack([t8(prm["w0"][:, d]) for d in range(2)], 2)
    a0 = np.stack([t8(prm["a0"][:, d]) for d in range(2)], 2)
    out["rwkv_w0a0T"] = np.ascontiguousarray(np.stack([w0, a0], 2))
    out["rwkv_lnT"] = np.ascontiguousarray(np.stack([t8(prm["ln_w"]), t8(prm["ln_b"])], 2))
    return out
```

```python
import numpy as np
from contextlib import ExitStack
import concourse.bass as bass
import concourse.mybir as mybir
from concourse.bass_utils import run_bass_kernel_spmd

F32 = mybir.dt.float32
BF16 = mybir.dt.bfloat16
I32 = mybir.dt.int32
AF = mybir.ActivationFunctionType
ALU = mybir.AluOpType

P = 128
MASKS = ['BD', 'LINC0', 'LINC1', 'LSTR0', 'LSTR1', 'MREL0', 'MREL1', 'MEND0', 'MEND1', 'NEG0', 'NEG1', 'NEGI0', 'NEGI1']
NMASK = len(MASKS)
MI = {k: i for i, k in enumerate(MASKS)}


class Cfg:
    def __init__(self, n_ctx=256, n_lat=8192, depth=4, d_expert=1408, grid_w=64, dblk=512):
        self.D = 2048
        self.n_ctx, self.n_lat, self.depth = n_ctx, n_lat, depth
        self.n_tok = n_ctx + n_lat
        self.grid_w = grid_w
        self.MIX = 1024
        self.d_expert = d_expert
        self.n_exp = 16
        self.dblk = dblk
        self.gla_off = 0
        self.rwkv_off = 3104
        self.gdn_off = 3104 + 3456
        self.gate_off = 3104 + 3456 + 4128
        self.D_IN = self.gate_off + 3 * self.D
        self.n_asg = 2 * self.n_tok
        self.n_blk = -(-self.n_asg // dblk) + self.n_exp
        self.n_slot = self.n_blk * dblk


class Buf:
    def __init__(self, name):
        self.name = name
        self.w = {}
        self.wf = {}
        self.r = {}


class T:
    def __init__(self, t, name):
        self.t = t
        self.b = Buf(name)

    def __getitem__(self, idx):
        return self.t[idx]


class BankT(T):
    def __init__(self, t, bank, name):
        self.t = t
        self.bank = bank
        self.b = Buf(name)

    def __getitem__(self, idx):
        if not isinstance(idx, tuple):
            idx = (idx, slice(None))
        return self.t[(idx[0], self.bank) + tuple(idx[1:])]


class SegT:
    def __init__(self, segs, name):
        self.segs = segs
        self.b = Buf(name)

    def __getitem__(self, idx):
        rs, cs = idx
        for (r0, r1, t) in self.segs:
            if rs.start >= r0 and rs.stop <= r1:
                return t[rs.start - r0:rs.stop - r0, cs]
        raise ValueError("row range %s straddles DRAM segments" % (rs,))

    def pieces(self, a, b_):
        out = []
        for (r0, r1, t) in self.segs:
            lo, hi = max(a, r0), min(b_, r1)
            if lo < hi:
                out.append((lo, hi))
        return out


def _merge(d, s):
    for k, v in s.items():
        if d.get(k, 0) < v:
            d[k] = v


class KB:
    EPOCH = 30000
    RING = 8

    def __init__(self, nc, es):
        self.nc, self.es = nc, es
        self.eng = {"pe": nc.tensor, "act": nc.scalar, "dve": nc.vector, "pool": nc.gpsimd, "sp": nc.sync}
        self.sems = {}
        self.cur = {}
        self.seen = {e: {} for e in self.eng}
        self.nsem = 0
        self.ring = {}
        self.ring_i = {}
        self.ninst = 0
        self.bc_regs = {}
        for q in ("sp", "pool", "act"):
            self.ring[q] = [[self._newsem("dq_%s" % q), 0] for _ in range(self.RING)]
            self.ring_i[q] = 0
        for e in ("pe", "act", "dve", "pool"):
            self.cur[e] = [self._newsem("pg_%s" % e), 0]

    def _newsem(self, base):
        key = "%s_%d" % (base, self.nsem)
        self.nsem += 1
        self.sems[key] = self.es.enter_context(self.nc.semaphore(key))
        return key

    def _wait(self, e, deps):
        seen = self.seen[e]
        h = self.eng[e]
        for k, v in deps.items():
            if v <= 0:
                continue
            if e == "pe" and k.startswith("pg_pe"):
                continue
            if seen.get(k, 0) < v:
                h.wait_ge(self.sems[k], v)
                seen[k] = v
                self.ninst += 1

    def _deps(self, reads, writes, pwrites):
        deps = {}
        for b in reads:
            _merge(deps, b.b.w)
            if isinstance(b, BankT):
                _merge(deps, b.b.r)
        for b in writes:
            _merge(deps, b.b.w)
            _merge(deps, b.b.r)
        for b in pwrites:
            _merge(deps, b.b.r)
            _merge(deps, b.b.wf)
        return deps

    def _post(self, key, val, reads, writes, pwrites):
        for b in reads:
            if b.b.r.get(key, 0) < val:
                b.b.r[key] = val
        for b in writes:
            b.b.w = {key: val}
            b.b.wf = {key: val}
            b.b.r = {}
        for b in pwrites:
            if b.b.w.get(key, 0) < val:
                b.b.w[key] = val

    def op(self, e, fn, reads=(), writes=(), pwrites=()):
        deps = self._deps(reads, writes, pwrites)
        self._wait(e, deps)
        c = self.cur[e]
        if c[1] >= self.EPOCH:
            c = self.cur[e] = [self._newsem("pg_%s" % e), 0]
        ins = fn(self.eng[e])
        c[1] += 1
        ins.then_inc(self.sems[c[0]], 1)
        self.ninst += 1
        self._post(c[0], c[1], reads, writes, pwrites)

    def dma(self, q, out, in_, reads=(), writes=(), pwrites=(), **kw):
        deps = self._deps(reads, writes, pwrites)
        i = self.ring_i[q]
        self.ring_i[q] = (i + 1) % self.RING
        slot = self.ring[q][i]
        deps2 = dict(deps)
        if deps2.get(slot[0], 0) < slot[1]:
            deps2[slot[0]] = slot[1]
        self._wait(q, deps2)
        ins = self.eng[q].dma_start(out=out, in_=in_, **kw)
        slot[1] += 16
        ins.then_inc(self.sems[slot[0]], 16)
        self.ninst += 1
        self._post(slot[0], slot[1], reads, writes, pwrites)

    def fence_pe(self):
        c = self.cur["pe"]
        if c[1] > 0:
            self.eng["pe"].wait_ge(self.sems[c[0]], c[1])
            self.ninst += 1

    def idma(self, out, out_off, in_, in_off, nrows, reads=(), writes=(), pwrites=()):
        q = "pool"
        deps = self._deps(reads, writes, pwrites)
        i = self.ring_i[q]
        self.ring_i[q] = (i + 1) % self.RING
        slot = self.ring[q][i]
        if deps.get(slot[0], 0) < slot[1]:
            deps[slot[0]] = slot[1]
        self._wait(q, deps)
        if nrows not in self.bc_regs:
            regs = self.nc.alloc_registers("bc%d" % nrows, engines=[mybir.EngineType.Pool])
            self.nc.regs_mov(regs, nrows - 1)
            self.bc_regs[nrows] = regs[mybir.EngineType.Pool]
        ins = self.nc.gpsimd.indirect_dma_start(out=out, out_offset=out_off, in_=in_, in_offset=in_off,
                                                bounds_check=self.bc_regs[nrows], oob_is_err=False)
        slot[1] += 16
        ins.then_inc(self.sems[slot[0]], 16)
        self.ninst += 1
        self._post(slot[0], slot[1], reads, writes, pwrites)

    def barrier(self):
        deps = {}
        for e, c in self.cur.items():
            deps[c[0]] = c[1]
        for q, r in self.ring.items():
            for s in r:
                deps[s[0]] = s[1]
        for e in self.eng:
            self._wait(e, deps)

    def finish(self, engines=("sp", "pool", "act")):
        deps = {}
        for q, r in self.ring.items():
            for s in r:
                deps[s[0]] = s[1]
        for e in engines:
            self._wait(e, deps)

    def sb(self, st, name, shape, dt=F32):
        self.uid = getattr(self, "uid", 0) + 1
        name = "%s_u%d" % (name, self.uid)
        return T(st.enter_context(self.nc.sbuf_tensor(name, list(shape), dt)), name)

    def dram(self, name, shape, dt=F32, kind="Internal"):
        return T(self.nc.dram_tensor(name, list(shape), dt, kind=kind), name)


def layer_norm(kb, x_t, st_t, mv_t, gam, bet, eps):
    for c in range(4):
        kb.op("dve", lambda e, c=c: e.bn_stats(st_t[:, c, :], x_t[:, c * 512:(c + 1) * 512]),
              reads=[x_t], writes=[st_t] if c == 0 else (), pwrites=() if c == 0 else [st_t])
    kb.op("dve", lambda e: e.bn_aggr(mv_t[:, 0:2], st_t[:].rearrange("p c s -> p (c s)")), reads=[st_t], writes=[mv_t])
    kb.op("dve", lambda e: e.tensor_scalar(mv_t[:, 2:3], mv_t[:, 1:2], eps, None, ALU.add), reads=[mv_t], pwrites=[mv_t])
    kb.op("act", lambda e: e.activation(out=mv_t[:, 3:4], in_=mv_t[:, 2:3], func=AF.Sqrt), reads=[mv_t], pwrites=[mv_t])
    kb.op("dve", lambda e: e.reciprocal(mv_t[:, 2:3], mv_t[:, 3:4]), reads=[mv_t], pwrites=[mv_t])
    kb.op("dve", lambda e: e.tensor_scalar(x_t[:], x_t[:], mv_t[:, 0:1], mv_t[:, 2:3], ALU.subtract, ALU.mult),
          reads=[x_t, mv_t], writes=[x_t])
    kb.op("pool", lambda e: e.tensor_tensor(x_t[:], x_t[:], gam[:], ALU.mult), reads=[x_t, gam], writes=[x_t])
    kb.op("pool", lambda e: e.tensor_tensor(x_t[:], x_t[:], bet[:], ALU.add), reads=[x_t, bet], writes=[x_t])

def build_program(cfg, phases=("mod", "inproj"), dbg=(), ext_in=()):
    nc = bass.Bass("TRN2", target_bir_lowering=False)
    es = ExitStack()
    kb = KB(nc, es)
    D, n = cfg.D, cfg.n_tok
    L = cfg.depth

    def din(name, shape, dt=F32, ph=None):
        if ph is not None and not any(p_ in phases for p_ in ph):
            return None
        return T(nc.dram_tensor(name, list(shape), dt, kind="ExternalInput"), name)

    def dscr(name, shape, dt=F32):
        kind = "ExternalOutput" if name in dbg else ("ExternalInput" if name in ext_in else "Internal")
        return T(nc.dram_tensor(name, list(shape), dt, kind=kind), name)

    xin = din("xin", [n, D], ph=("inproj", "merge"))
    ccT = din("ccT", [D, 2], ph=("mod",))
    w_ada = din("w_ada", [L, D, 6 * D], ph=("mod",))
    b_ada = din("b_ada", [L, 1, 6 * D], ph=("mod",))
    w_in = din("w_in", [L, D, cfg.D_IN], ph=("inproj",))
    b_inT = din("b_inT", [L, P, 132], ph=("inproj",))

    w_branch = din("w_branch", [L, 3, cfg.MIX, D], ph=("merge",))
    w_out = din("w_out", [L, D, D], ph=("merge",))
    ln1_g = din("ln1_g", [L, 1, D], ph=("merge",))
    ln1_b = din("ln1_b", [L, 1, D], ph=("merge",))
    ln2_g = din("ln2_g", [L, 1, D], ph=("moe",))
    ln2_b = din("ln2_b", [L, 1, D], ph=("moe",))
    cfg_alpha = float((2.0 * 4) ** 0.25)
    router_w = din("router_w", [D, 16], ph=("moe",))
    router_bias = din("router_bias", [1, 16], ph=("moe",))
    moe_w1 = din("moe_w1", [L * 16 * D, cfg.d_expert], ph=("moe",))
    moe_w3 = din("moe_w3", [L * 16 * D, cfg.d_expert], ph=("moe",))
    moe_w2 = din("moe_w2", [L * 16 * cfg.d_expert * 4, 512], ph=("moe",))
    c_ut16 = din("c_ut16", [16, 16], ph=("moe",))
    c_bbrow = din("c_bbrow", [16, cfg.n_blk], ph=("moe",))
    c_idb = din("c_idb", [P, 32], ph=("moe",))
    per_layer = getattr(cfg, "per_layer", False)
    yout = T(nc.dram_tensor("yout", [cfg.n_lat, D], F32, kind="ExternalOutput"), "yout") if ("moe" in phases and not per_layer) else None
    MIXP = ("gla", "gdn", "rwkv")
    c_masks = din("c_masks", [NMASK, P, P], ph=MIXP)
    c_bdcol = din("c_bdcol", [P, 2], ph=MIXP)
    gla_dec_w = din("gla_dec_w", [L, 2, 16, 512], ph=("gla",))
    gla_dec_b = din("gla_dec_b", [L, 2, 1, 512], ph=("gla",))
    gla_norm_wT = din("gla_norm_wT", [L, P, 2], ph=("gla",))
    rwkv_muT = din("rwkv_muT", [L, P, 27], ph=("rwkv",))
    rwkv_w2 = din("rwkv_w2", [L, 2, 64, 1024], ph=("rwkv",))
    rwkv_a2 = din("rwkv_a2", [L, 2, 64, 1024], ph=("rwkv",))
    rwkv_g2 = din("rwkv_g2", [L, P, 1024], ph=("rwkv",))
    rwkv_p4T = din("rwkv_p4T", [L, P, 4, 8], ph=("rwkv",))
    rwkv_w0a0T = din("rwkv_w0a0T", [L, P, 2, 2, 8], ph=("rwkv",))
    rwkv_lnT = din("rwkv_lnT", [L, P, 2, 8], ph=("rwkv",))
    gdn_conv_wT = din("gdn_conv_wT", [L, 24, P, 9], ph=("gdn",))
    gdn_a_log = din("gdn_a_log", [L, 1, 16], ph=("gdn",))
    gdn_dt_bias = din("gdn_dt_bias", [L, 1, 16], ph=("gdn",))
    gdn_norm_wT = din("gdn_norm_wT", [L, P, 1], ph=("gdn",))
    MODD = dscr("MODD", [2, 6 * D])
    if "PT" in dbg or "PT" in ext_in:
        PT = dscr("PT", [cfg.D_IN, n])
    else:
        bnd = [0, cfg.gdn_off, cfg.gate_off, cfg.D_IN]
        PT = SegT([(bnd[i], bnd[i + 1], nc.dram_tensor("PT%d" % i, [bnd[i + 1] - bnd[i], n], F32, kind="Internal")) for i in range(3)], "PT")
    OT = dscr("OT", [3 * cfg.MIX, n], BF16)
    ACC = dscr("ACC", [D, n], BF16)
    OFG = dscr("OFG", [cfg.MIX, n])
    GC = dscr("GC", [3 * cfg.MIX, n])
    RW = {k_: dscr("RW_" + k_, [cfg.MIX, n]) for k_ in ("R", "V", "KK", "G", "BON", "LW0", "LW1", "KH0", "KH1", "BB0", "BB1")}
    OFR = dscr("OFR", [n, cfg.MIX])
    OFD = dscr("OFD", [n, cfg.MIX])
    XR = dscr("XR", [n, D])
    XM = dscr("XM", [n, D])
    H2 = dscr("H2", [n, D])
    EOFF = dscr("EOFF", [P, cfg.n_blk])
    SLOT = dscr("SLOT", [n // P, 2, P, 1], I32)
    W12D = dscr("W12D", [P, n // P, 2])
    XS = dscr("XS", [cfg.n_slot, D], BF16)
    YS = dscr("YS", [cfg.n_slot, D])

    g = ExitStack()
    es.enter_context(g)
    PSA = g.enter_context(nc.psum_tensor("psa", [P, 8, 512], F32))
    PS = [BankT(PSA, i, "ps%d" % i) for i in range(8)]
    ident_f = kb.sb(g, "ident_f", [P, P], F32)
    ident_b = kb.sb(g, "ident_b", [P, P], BF16)
    ones_f = kb.sb(g, "ones_f", [P, P], F32)
    identD = din("identD", [P, P])
    kb.dma("sp", ident_f[:], identD[:, :], reads=[identD], writes=[ident_f])
    kb.op("dve", lambda e: e.tensor_copy(ident_b[:], ident_f[:]), reads=[ident_f], writes=[ident_b])
    kb.op("dve", lambda e: e.memset(ones_f[:], 1.0), writes=[ones_f])

    def tile_is_ctx(tt):
        return tt * P < cfg.n_ctx

    for l in range(L):
        if "mod" in phases:
            with ExitStack() as ph:
                sc = kb.sb(ph, "sc", [P, 16, 2], F32)
                scs = kb.sb(ph, "scs", [P, 16, 2], F32)
                one2 = kb.sb(ph, "one2", [1, 2], F32)
                brow = kb.sb(ph, "brow", [1, 6 * D], F32)
                wa = [kb.sb(ph, "wa%d" % i, [P, 16, 512], F32) for i in range(2)]
                mo = [kb.sb(ph, "mo%d" % i, [2, 512], F32) for i in range(2)]
                kb.dma("sp", sc[:], ccT[:, :].rearrange("(k p) r -> p k r", p=P), reads=[ccT], writes=[sc])
                kb.op("act", lambda e: e.activation(out=scs[:], in_=sc[:], func=AF.Silu), reads=[sc], writes=[scs])
                kb.op("dve", lambda e: e.memset(one2[:], 1.0), writes=[one2])
                kb.dma("sp", brow[:], b_ada[l, :, :], reads=[b_ada], writes=[brow])
                for cg in range(24):
                    w = wa[cg % 2]
                    kb.dma("sp", w[:], w_ada[l, :, cg * 512:(cg + 1) * 512].rearrange("(k p) c -> p k c", p=P),
                           reads=[w_ada], writes=[w])
                    ps = PS[cg % 2]
                    for k in range(16):
                        kb.op("pe", lambda e, k=k, w=w, ps=ps: e.matmul(ps[0:2, :], scs[:, k, :], w[:, k, :],
                                                                         start=(k == 0), stop=False),
                              reads=[scs, w], writes=[ps] if k == 0 else (), pwrites=() if k == 0 else [ps])
                    kb.op("pe", lambda e, ps=ps, cg=cg: e.matmul(ps[0:2, :], one2[:], brow[:, cg * 512:(cg + 1) * 512],
                                                                  start=False, stop=True),
                          reads=[one2, brow], pwrites=[ps])
                    m = mo[cg % 2]
                    addc = 1.0 if (cg // 4) in (1, 4) else 0.0
                    kb.op("dve", lambda e, m=m, ps=ps, addc=addc: e.tensor_scalar(m[:], ps[0:2, :], addc, None, ALU.add),
                          reads=[ps], writes=[m])
                    kb.dma("pool", MODD[:, cg * 512:(cg + 1) * 512], m[:], reads=[m], pwrites=[MODD])
                kb.barrier()

        if "inproj" in phases:
            xsrc = xin if l == 0 else None
            with ExitStack() as ph:
                TB = 2048 if n >= 2048 else n
                hT = kb.sb(ph, "hT", [P, 16, TB], BF16)
                s1b = [kb.sb(ph, "s1b%d" % r, [P, D], F32) for r in range(2)]
                sh1b = [kb.sb(ph, "sh1b%d" % r, [P, D], F32) for r in range(2)]
                xt = [kb.sb(ph, "xt%d" % i, [P, D], F32) for i in range(2)]
                hb = [kb.sb(ph, "hb%d" % i, [P, D], BF16) for i in range(2)]
                wb = [kb.sb(ph, "wb%d" % i, [P, 16, 512], BF16) for i in range(2)]
                bt = kb.sb(ph, "bt", [P, 132], F32)
                po = [kb.sb(ph, "po%d" % i, [P, 512], F32) for i in range(3)]
                kb.dma("sp", bt[:], b_inT[l, :, :], reads=[b_inT], writes=[bt])
                for r in range(2):
                    kb.dma("sp", s1b[r][:], MODD[r:r + 1, D:2 * D].partition_broadcast(P), reads=[MODD], writes=[s1b[r]])
                    kb.dma("sp", sh1b[r][:], MODD[r:r + 1, 0:D].partition_broadcast(P), reads=[MODD], writes=[sh1b[r]])
                n_cg = -(-cfg.D_IN // 512)
                ti = 0
                wi = 0
                oi = 0
                for t0 in range(0, n, TB):
                    tb = min(TB, n - t0)
                    for tt in range(tb // P):
                        gt = (t0 // P) + tt
                        r = 1 if tile_is_ctx(gt) else 0
                        x_t, h_b = xt[ti % 2], hb[ti % 2]
                        ti += 1
                        kb.dma("sp", x_t[:], xsrc[gt * P:(gt + 1) * P, :], reads=[xsrc], writes=[x_t])
                        kb.op("dve", lambda e, x_t=x_t, r=r: e.tensor_tensor(x_t[:], x_t[:], s1b[r][:], ALU.mult),
                              reads=[x_t, s1b[r]], writes=[x_t])
                        kb.op("pool", lambda e, x_t=x_t, h_b=h_b, r=r: e.tensor_tensor(h_b[:], x_t[:], sh1b[r][:], ALU.add),
                              reads=[x_t, sh1b[r]], writes=[h_b])
                        for half in range(2):
                            pa, pb_ = PS[4 + 2 * half], PS[5 + 2 * half]
                            for kk in range(8):
                                k = half * 8 + kk
                                ps = pa if kk < 4 else pb_
                                o = ps[:].bitcast(BF16)[:, (kk % 4) * P:(kk % 4 + 1) * P]
                                kb.op("pe", lambda e, o=o, h_b=h_b, k=k: e.transpose(o, h_b[:, k * P:(k + 1) * P], ident_b[:]),
                                      reads=[h_b, ident_b], writes=[ps] if kk % 4 == 0 else (),
                                      pwrites=() if kk % 4 == 0 else [ps])
                            for q4, ps in enumerate((pa, pb_)):
                                k0 = half * 8 + q4 * 4
                                src = ps[:].bitcast(BF16)[:, 0:4 * P].rearrange("p (k t) -> p k t", k=4)
                                kb.op("act", lambda e, src=src, k0=k0, tt=tt: e.activation(
                                    out=hT[:, k0:k0 + 4, tt * P:(tt + 1) * P], in_=src, func=AF.Copy),
                                    reads=[ps], pwrites=[hT])
                    for cg in range(n_cg):
                        c0 = cg * 512
                        cw = min(512, cfg.D_IN - c0)
                        w = wb[wi % 2]
                        wi += 1
                        kb.dma("pool", w[:, :, 0:cw], w_in[l, :, c0:c0 + cw].rearrange("(k p) c -> p k c", p=P),
                               reads=[w_in], writes=[w])
                        for jj in range(-(-cw // P)):
                            jw = min(P, cw - jj * P)
                            j = cg * 4 + jj
                            for ts in range(0, tb, 512):
                                tw = min(512, tb - ts)
                                ps = PS[oi % 4]
                                o_t = po[oi % 3]
                                oi += 1
                                for k in range(16):
                                    kb.op("pe", lambda e, ps=ps, w=w, k=k, jj=jj, jw=jw, ts=ts, tw=tw: e.matmul(
                                        ps[0:jw, 0:tw], w[:, k, jj * P:jj * P + jw], hT[:, k, ts:ts + tw],
                                        start=(k == 0), stop=(k == 15)),
                                        reads=[w, hT], writes=[ps] if k == 0 else (), pwrites=() if k == 0 else [ps])
                                kb.op("act", lambda e, ps=ps, o_t=o_t, j=j, jw=jw, tw=tw: e.activation(
                                    out=o_t[0:jw, 0:tw], in_=ps[0:jw, 0:tw], func=AF.Identity, bias=bt[0:jw, j:j + 1], scale=1.0),
                                    reads=[ps, bt], writes=[o_t])
                                pcs = PT.pieces(j * P, j * P + jw) if isinstance(PT, SegT) else [(j * P, j * P + jw)]
                                for (lo_, hi_) in pcs:
                                    kb.dma("sp", PT[lo_:hi_, t0 + ts:t0 + ts + tw], o_t[lo_ - j * P:hi_ - j * P, 0:tw],
                                           reads=[o_t], pwrites=[PT])
                kb.barrier()


        NU = n // P
        NUc = cfg.n_ctx // P

        def unit_order(d):
            if d == 0:
                return list(range(NU))
            return list(range(NUc - 1, -1, -1)) + list(range(NU - 1, NUc - 1, -1))

        def cmask(ph, name):
            t_ = kb.sb(ph, "cm_" + name, [P, P], F32)
            kb.dma("sp", t_[:], c_masks[MI[name], :, :], reads=[c_masks], writes=[t_])
            return t_

        def bc(ap, shape, at):
            for a in at:
                ap = ap.unsqueeze(a)
            return ap.to_broadcast(list(shape))

        if "gla" in phases:
            with ExitStack() as ph:
                QS = 128.0 ** -0.5
                CAT = [kb.sb(ph, "cat%d" % d, [P, 258], F32) for d in range(2)]
                for d in range(2):
                    kb.dma("sp", CAT[d][:, 0:128], c_masks[MI["LINC%d" % d], :, :], reads=[c_masks], writes=[CAT[d]])
                    kb.dma("sp", CAT[d][:, 128:256], c_masks[MI["MREL%d" % d], :, :], reads=[c_masks], pwrites=[CAT[d]])
                    kb.dma("sp", CAT[d][:, 256:258], c_bdcol[:, :], reads=[c_bdcol], pwrites=[CAT[d]])
                MEND = [cmask(ph, "MEND0"), cmask(ph, "MEND1")]
                LINC = [cmask(ph, "LINC0"), cmask(ph, "LINC1")]
                decw = kb.sb(ph, "decw", [16, 2, 512], F32)
                decb = kb.sb(ph, "decb", [1, 2, 512], F32)
                nw = kb.sb(ph, "nw", [P, 2], F32)
                one1 = kb.sb(ph, "one1", [1, P], F32)
                kb.dma("sp", decw[:], gla_dec_w[l, :, :, :].rearrange("d r c -> r d c"), reads=[gla_dec_w], writes=[decw])
                kb.dma("sp", decb[:], gla_dec_b[l, :, :, :].rearrange("d o c -> o d c"), reads=[gla_dec_b], writes=[decb])
                kb.dma("sp", nw[:], gla_norm_wT[l, :, :], reads=[gla_norm_wT], writes=[nw])
                kb.op("dve", lambda e: e.memset(one1[:], 1.0), writes=[one1])
                qT4 = [kb.sb(ph, "g_qT%d" % i, [P, 4, P], F32) for i in range(2)]
                kT4 = [kb.sb(ph, "g_kT%d" % i, [P, 4, P], F32) for i in range(2)]
                vT8 = [kb.sb(ph, "g_vT%d" % i, [P, 8, P], F32) for i in range(2)]
                lrT = [kb.sb(ph, "g_lr%d" % i, [16, P], F32) for i in range(2)]
                ofl = [kb.sb(ph, "g_of%d" % i, [P, 8, P], F32) for i in range(2)]
                ogT = [kb.sb(ph, "g_og%d" % i, [P, 8, P], F32) for i in range(2)]
                gt = kb.sb(ph, "g_gt", [P, 512], F32)
                vtm = kb.sb(ph, "g_vtm", [P, 1024], F32)
                eend = kb.sb(ph, "g_eend", [P, 512], F32)
                kend = kb.sb(ph, "g_kend", [P, 512], F32)
                E12 = kb.sb(ph, "g_E12", [P, 4, 256], F32)
                E3 = kb.sb(ph, "g_E3", [P, 4, P], F32)
                dec4 = kb.sb(ph, "g_dec", [P, 4, 2], F32)
                qb4 = kb.sb(ph, "g_qb", [P, 4, P], F32)
                qr4 = kb.sb(ph, "g_qr", [P, 4, P], F32)
                kr4 = kb.sb(ph, "g_kr", [P, 4, P], F32)
                att4 = kb.sb(ph, "g_att", [P, 4, P], F32)
                S4 = kb.sb(ph, "g_S", [P, 4, 256], F32)
                o8 = kb.sb(ph, "g_o8", [P, 8, P], F32)
                sq8 = kb.sb(ph, "g_sq8", [P, 8, P], F32)
                rs4 = kb.sb(ph, "g_rs4", [P, 4, P], F32)
                sg8 = kb.sb(ph, "g_sg8", [P, 8, P], F32)
                ob8 = [kb.sb(ph, "g_ob%d" % i, [P, 8, P], BF16) for i in range(2)]
                g0 = cfg.gla_off
                for d in range(2):
                    kb.op("dve", lambda e: e.memset(S4[:], 0.0), writes=[S4])
                    for ui, u in enumerate(unit_order(d)):
                        t0 = u * P
                        q_, k_, v_, lr_ = qT4[ui % 2], kT4[ui % 2], vT8[ui % 2], lrT[ui % 2]
                        kb.dma("sp", q_[:], PT[g0:g0 + 512, t0:t0 + P].rearrange("(h p) t -> p h t", p=P), reads=[PT], writes=[q_])
                        kb.dma("sp", k_[:], PT[g0 + 512:g0 + 1024, t0:t0 + P].rearrange("(h p) t -> p h t", p=P), reads=[PT], writes=[k_])
                        kb.dma("sp", v_[:], PT[g0 + 1024:g0 + 2048, t0:t0 + P].rearrange("(h p) t -> p h t", p=P), reads=[PT], writes=[v_])
                        kb.dma("sp", lr_[:], PT[g0 + 3072 + 16 * d:g0 + 3088 + 16 * d, t0:t0 + P], reads=[PT], writes=[lr_])
                        if d == 1:
                            of_, og_ = ofl[ui % 2], ogT[ui % 2]
                            kb.dma("sp", of_[:], OFG[:, t0:t0 + P].rearrange("(c p) t -> p c t", p=P), reads=[OFG], writes=[of_])
                            kb.dma("sp", og_[:], PT[g0 + 2048:g0 + 3072, t0:t0 + P].rearrange("(c p) t -> p c t", p=P), reads=[PT], writes=[og_])
                        kb.op("pe", lambda e, lr_=lr_: e.matmul(PS[0][:, :], lr_[:, :], decw[:, d, :], start=True, stop=False), reads=[lr_, decw], writes=[PS[0]])
                        kb.op("pe", lambda e: e.matmul(PS[0][:, :], one1[:, :], decb[:, d, :], start=False, stop=True), reads=[one1, decb], pwrites=[PS[0]])
                        kb.op("act", lambda e: e.activation(out=gt[:], in_=PS[0][:, :], func=AF.Exp, scale=-1.0), reads=[PS[0]], writes=[gt])
                        kb.op("act", lambda e: e.activation(out=gt[:], in_=gt[:], func=AF.Ln, bias=ones_f[:, 0:1], scale=1.0), reads=[gt, ones_f], writes=[gt])
                        kb.op("dve", lambda e: e.tensor_scalar(gt[:], gt[:], -1.0 / 16.0, None, ALU.mult), reads=[gt], writes=[gt])
                        for h in range(4):
                            kb.op("pe", lambda e, h=h, k_=k_: e.transpose(PS[1][:, h * P:(h + 1) * P], k_[:, h, :], ident_f[:]), reads=[k_, ident_f],
                                  writes=[PS[1]] if h == 0 else (), pwrites=() if h == 0 else [PS[1]])
                        for c in range(8):
                            pb_ = PS[2 + c // 4]
                            kb.op("pe", lambda e, c=c, v_=v_, pb_=pb_: e.transpose(pb_[:, (c % 4) * P:(c % 4 + 1) * P], v_[:, c, :], ident_f[:]), reads=[v_, ident_f],
                                  writes=[pb_] if c % 4 == 0 else (), pwrites=() if c % 4 == 0 else [pb_])
                        kb.op("act", lambda e: e.activation(out=vtm[:].rearrange("p (b c) -> p b c", b=2), in_=PSA[:, 2:4, :], func=AF.Copy), reads=[PS[2], PS[3]], writes=[vtm])
                        kb.op("pe", lambda e: e.matmul(PS[4][:, :], MEND[d][:, :], gt[:, :], start=True, stop=True), reads=[MEND[d], gt], writes=[PS[4]])
                        kb.op("act", lambda e: e.activation(out=eend[:], in_=PS[4][:, :], func=AF.Exp), reads=[PS[4]], writes=[eend])
                        kb.op("dve", lambda e: e.tensor_tensor(kend[:], PS[1][:, :], eend[:], ALU.mult), reads=[PS[1], eend], writes=[kend])
                        for h in range(4):
                            kb.op("pe", lambda e, h=h: e.matmul(PS[4 + h][:, 0:258], gt[:, h * P:(h + 1) * P], CAT[d][:, :], start=True, stop=True), reads=[gt, CAT[d]], writes=[PS[4 + h]])
                        pa4 = [PS[4], PS[5], PS[6], PS[7]]
                        kb.op("act", lambda e: e.activation(out=E12[:], in_=PSA[:, 4:8, 0:256], func=AF.Exp), reads=pa4, writes=[E12])
                        kb.op("act", lambda e: e.activation(out=E3[:], in_=PSA[:, 4:8, 128:256], func=AF.Exp, scale=-1.0), reads=pa4, writes=[E3])
                        kb.op("act", lambda e: e.activation(out=dec4[:], in_=PSA[:, 4:8, 256:258], func=AF.Exp), reads=pa4, writes=[dec4])
                        kb.op("dve", lambda e, q_=q_: e.scalar_tensor_tensor(out=qb4[:], in0=q_[:], scalar=QS, in1=E12[:, :, 0:128], op0=ALU.mult, op1=ALU.mult), reads=[q_, E12], writes=[qb4])
                        kb.op("dve", lambda e, q_=q_: e.scalar_tensor_tensor(out=qr4[:], in0=q_[:], scalar=QS, in1=E12[:, :, 128:256], op0=ALU.mult, op1=ALU.mult), reads=[q_, E12], writes=[qr4])
                        kb.op("pool", lambda e, k_=k_: e.tensor_tensor(kr4[:], k_[:], E3[:], ALU.mult), reads=[k_, E3], writes=[kr4])
                        for h in range(4):
                            kb.op("pe", lambda e, h=h: e.matmul(PS[0][:, h * P:(h + 1) * P], kr4[:, h, :], qr4[:, h, :], start=True, stop=True), reads=[kr4, qr4],
                                  writes=[PS[0]] if h == 0 else (), pwrites=() if h == 0 else [PS[0]])
                        kb.op("dve", lambda e: e.tensor_tensor(att4[:], PS[0][:, :].rearrange("p (h t) -> p h t", h=4), bc(LINC[d][:], [P, 4, P], [1]), ALU.mult),
                              reads=[PS[0], LINC[d]], writes=[att4])
                        first_o = {4: True, 5: True}
                        for s in ((0, 1) if d == 0 else (1, 0)):
                            rs = slice(64 * s, 64 * s + 64)
                            kb.fence_pe()
                            first_s = {2: True, 3: True}
                            for h in range(4):
                                pso = PS[4 + h // 2]
                                for half in range(2):
                                    c0 = ((h % 2) * 2 + half) * P + 64 * s
                                    fo = first_o[4 + h // 2]
                                    first_o[4 + h // 2] = False
                                    kb.op("pe", lambda e, pso=pso, c0=c0, h=h, half=half, rs=rs, s=s: e.matmul(
                                        pso[:, c0:c0 + 64], vtm[rs, h * 256 + half * P:h * 256 + (half + 1) * P], att4[rs, h, 64 * s:64 * s + 64], start=True, stop=False),
                                        reads=[vtm, att4], writes=[pso] if fo else (), pwrites=() if fo else [pso])
                                    kb.op("pe", lambda e, pso=pso, c0=c0, h=h, half=half, s=s: e.matmul(
                                        pso[:, c0:c0 + 64], S4[:, h, half * P:(half + 1) * P], qb4[:, h, 64 * s:64 * s + 64], start=False, stop=True),
                                        reads=[S4, qb4], pwrites=[pso])
                                pss = PS[2 + h // 2]
                                fs = first_s[2 + h // 2]
                                first_s[2 + h // 2] = False
                                kb.op("pe", lambda e, pss=pss, h=h, rs=rs: e.matmul(pss[:, (h % 2) * 256:(h % 2 + 1) * 256], kend[rs, h * P:(h + 1) * P], vtm[rs, h * 256:(h + 1) * 256], start=True, stop=True),
                                      reads=[kend, vtm], writes=[pss] if fs else (), pwrites=() if fs else [pss])
                            kb.op("dve", lambda e, s=s: e.tensor_tensor(S4[:], S4[:], dec4[:, :, s:s + 1].to_broadcast([P, 4, 256]), ALU.mult), reads=[S4, dec4], writes=[S4])
                            kb.op("dve", lambda e: e.tensor_tensor(S4[:], S4[:], PSA[:, 2:4, :].rearrange("p b (h v) -> p (b h) v", h=2), ALU.add), reads=[S4, PS[2], PS[3]], writes=[S4])
                        osrc = PSA[:, 4:6, :].rearrange("p b (c t) -> p (b c) t", c=4)
                        if d == 0:
                            kb.op("act", lambda e: e.activation(out=o8[:], in_=osrc, func=AF.Copy), reads=[PS[4], PS[5]], writes=[o8])
                            kb.dma("sp", OFG[:, t0:t0 + P].rearrange("(c p) t -> p c t", p=P), o8[:], reads=[o8], pwrites=[OFG])
                        else:
                            kb.op("dve", lambda e, of_=of_: e.tensor_tensor(o8[:], osrc, of_[:], ALU.add), reads=[PS[4], PS[5], of_], writes=[o8])
                            kb.op("pool", lambda e: e.tensor_tensor(sq8[:], o8[:], o8[:], ALU.mult), reads=[o8], writes=[sq8])
                            for h in range(4):
                                for half in range(2):
                                    kb.op("pe", lambda e, h=h, half=half: e.matmul(PS[0][:, h * P:(h + 1) * P], ones_f[:, :], sq8[:, 2 * h + half, :], start=(half == 0), stop=(half == 1)),
                                          reads=[ones_f, sq8], writes=[PS[0]] if (h == 0 and half == 0) else (), pwrites=() if (h == 0 and half == 0) else [PS[0]])
                            kb.op("dve", lambda e: e.tensor_scalar(rs4[:], PS[0][:, :].rearrange("p (h t) -> p h t", h=4), 1.0 / 256.0, 1e-6, ALU.mult, ALU.add), reads=[PS[0]], writes=[rs4])
                            kb.op("act", lambda e: e.activation(out=rs4[:], in_=rs4[:], func=AF.Sqrt), reads=[rs4], writes=[rs4])
                            kb.op("dve", lambda e: e.reciprocal(rs4[:], rs4[:]), reads=[rs4], writes=[rs4])
                            o84 = o8[:].rearrange("p (h f) t -> p h f t", f=2)
                            kb.op("dve", lambda e: e.tensor_tensor(o84, o84, bc(rs4[:], [P, 4, 2, P], [2]), ALU.mult), reads=[o8, rs4], writes=[o8])
                            kb.op("pool", lambda e: e.tensor_tensor(o84, o84, bc(nw[:], [P, 4, 2, P], [1, 3]), ALU.mult), reads=[o8, nw], writes=[o8])
                            kb.op("act", lambda e, og_=og_: e.activation(out=sg8[:], in_=og_[:], func=AF.Silu), reads=[og_], writes=[sg8])
                            ob_ = ob8[ui % 2]
                            kb.op("dve", lambda e, ob_=ob_: e.tensor_tensor(ob_[:], o8[:], sg8[:], ALU.mult), reads=[o8, sg8], writes=[ob_])
                            kb.dma("sp", OT[0:1024, t0:t0 + P].rearrange("(c p) t -> p c t", p=P), ob_[:], reads=[ob_], pwrites=[OT])
                kb.barrier()

        def emit_solve(zq, b0):
            nq = len(zq)
            bk = [[PS[b0[i] + h] for h in range(4)] for i in range(nq)]
            for step in range(6):
                for i in range(nq):
                    z = zq[i]
                    for h in range(4):
                        if step == 0:
                            kb.op("pe", lambda e, z=z, h=h, i=i: e.matmul(bk[i][h][:, 128:256], z[:, h, 256:384], z[:, h, 128:256], start=True, stop=True), reads=[z], writes=[bk[i][h]])
                        elif step < 5:
                            kb.op("pe", lambda e, z=z, h=h, i=i: e.matmul(bk[i][h][:, 0:256], z[:, h, 256:384], z[:, h, 0:256], start=True, stop=True), reads=[z], writes=[bk[i][h]])
                        else:
                            kb.op("pe", lambda e, z=z, h=h, i=i: e.matmul(bk[i][h][:, 0:128], z[:, h, 256:384], z[:, h, 0:128], start=True, stop=True), reads=[z], writes=[bk[i][h]])
                        if step < 5:
                            kb.op("pe", lambda e, z=z, h=h, i=i: e.matmul(bk[i][h][:, 256:384], z[:, h, 128:256], z[:, h, 256:384], start=True, stop=True), reads=[z], pwrites=[bk[i][h]])
                for i in range(nq):
                    z = zq[i]
                    bb = b0[i]
                    if step > 0:
                        kb.op("dve", lambda e, z=z, bb=bb: e.tensor_tensor(z[:, :, 0:128], z[:, :, 0:128], PSA[:, bb:bb + 4, 0:128], ALU.add), reads=[z] + bk[i], writes=[z])
                    if step < 5:
                        kb.op("act", lambda e, z=z, bb=bb: e.activation(out=z[:, :, 128:384], in_=PSA[:, bb:bb + 4, 128:384], func=AF.Copy), reads=bk[i],
                              writes=[z] if step == 0 else (), pwrites=() if step == 0 else [z])

        if "gdn" in phases:
            g0 = cfg.gdn_off
            with ExitStack() as ph:
                GW = cfg.grid_w
                RB = 8
                xi_ = [kb.sb(ph, "c_xi%d" % i, [P, RB + 2, GW], F32) for i in range(2)]
                yc = [kb.sb(ph, "c_y%d" % i, [P, RB, GW], F32) for i in range(2)]
                ysq = kb.sb(ph, "c_ysq", [P, RB * GW], F32)
                rsd = kb.sb(ph, "c_rsd", [P, RB * GW], F32)
                wc = kb.sb(ph, "c_wc", [P, 24, 9], F32)
                kb.dma("sp", wc[:], gdn_conv_wT[l, :, :, :].rearrange("c p k -> p c k"), reads=[gdn_conv_wT], writes=[wc])
                blocks = [("ctx", 0, cfg.n_ctx)] + [("lat", r0, min(RB, cfg.n_lat // GW - r0)) for r0 in range(0, cfg.n_lat // GW, RB)]
                bi = 0
                for cc in range(24):
                    for kind, b_0, b_n in blocks:
                        x_, y_ = xi_[bi % 2], yc[bi % 2]
                        bi += 1
                        prow = g0 + cc * P
                        if kind == "ctx":
                            nt = b_n
                            xf = x_[:].rearrange("p r w -> p (r w)")
                            yf = y_[:].rearrange("p r w -> p (r w)")
                            kb.op("pool", lambda e, xf=xf: e.memset(xf[:, 0:nt + 2], 0.0), writes=[x_])
                            kb.dma("sp", xf[:, 1:nt + 1], PT[prow:prow + P, 0:nt], reads=[PT], pwrites=[x_])
                            kb.op("dve", lambda e, xf=xf, yf=yf, cc=cc: e.tensor_scalar(yf[:, 0:nt], xf[:, 1:nt + 1], wc[:, cc, 4:5], None, ALU.mult), reads=[x_, wc], writes=[y_])
                            for dx in (0, 2):
                                kb.op("dve", lambda e, xf=xf, yf=yf, cc=cc, dx=dx: e.scalar_tensor_tensor(out=yf[:, 0:nt], in0=xf[:, dx:dx + nt], scalar=wc[:, cc, 3 + dx:4 + dx], in1=yf[:, 0:nt], op0=ALU.mult, op1=ALU.add),
                                      reads=[x_, wc, y_], writes=[y_])
                            ntok, tok0 = nt, 0
                            yv = yf[:, 0:nt]
                        else:
                            r0, nr = b_0, b_n
                            nrows = cfg.n_lat // GW
                            lo, hi = max(r0 - 1, 0), min(r0 + nr + 1, nrows)
                            if lo > r0 - 1 or hi < r0 + nr + 1:
                                kb.op("pool", lambda e, x_=x_: e.memset(x_[:], 0.0), writes=[x_])
                                wr = dict(pwrites=[x_])
                            else:
                                wr = dict(writes=[x_])
                            kb.dma("sp", x_[:, lo - (r0 - 1):hi - (r0 - 1), :],
                                   PT[prow:prow + P, cfg.n_ctx + lo * GW:cfg.n_ctx + hi * GW].rearrange("p (r w) -> p r w", w=GW), reads=[PT], **wr)
                            kb.op("dve", lambda e, x_=x_, y_=y_, cc=cc, nr=nr: e.tensor_scalar(y_[:, 0:nr, :], x_[:, 1:nr + 1, :], wc[:, cc, 4:5], None, ALU.mult), reads=[x_, wc], writes=[y_])
                            for dy in range(3):
                                for dx in range(3):
                                    if dy == 1 and dx == 1:
                                        continue
                                    c_lo, c_hi = (1, GW) if dx == 0 else ((0, GW) if dx == 1 else (0, GW - 1))
                                    kb.op("dve", lambda e, x_=x_, y_=y_, cc=cc, nr=nr, dy=dy, dx=dx, c_lo=c_lo, c_hi=c_hi: e.scalar_tensor_tensor(
                                        out=y_[:, 0:nr, c_lo:c_hi], in0=x_[:, dy:dy + nr, c_lo + dx - 1:c_hi + dx - 1], scalar=wc[:, cc, dy * 3 + dx:dy * 3 + dx + 1],
                                        in1=y_[:, 0:nr, c_lo:c_hi], op0=ALU.mult, op1=ALU.add), reads=[x_, wc, y_], writes=[y_])
                            ntok, tok0 = nr * GW, cfg.n_ctx + r0 * GW
                            yv = y_[:, 0:nr, :].rearrange("p r w -> p (r w)")
                        kb.op("act", lambda e, yv=yv: e.activation(out=yv, in_=yv, func=AF.Silu), reads=[y_], writes=[y_])
                        if cc < 16:
                            kb.op("pool", lambda e, yv=yv: e.tensor_tensor(ysq[:, 0:ntok], yv, yv, ALU.mult), reads=[y_], writes=[ysq])
                            kb.op("pe", lambda e: e.matmul(PS[0][:, 0:ntok], ones_f[:, :], ysq[:, 0:ntok], start=True, stop=True), reads=[ones_f, ysq], writes=[PS[0]])
                            kb.op("dve", lambda e: e.tensor_scalar(rsd[:, 0:ntok], PS[0][:, 0:ntok], 1e-6, None, ALU.add), reads=[PS[0]], writes=[rsd])
                            kb.op("act", lambda e: e.activation(out=rsd[:, 0:ntok], in_=rsd[:, 0:ntok], func=AF.Sqrt), reads=[rsd], writes=[rsd])
                            kb.op("dve", lambda e: e.reciprocal(rsd[:, 0:ntok], rsd[:, 0:ntok]), reads=[rsd], writes=[rsd])
                            qs = (128.0 ** -0.5) if cc < 8 else 1.0
                            kb.op("dve", lambda e, yv=yv, qs=qs: e.scalar_tensor_tensor(out=yv, in0=yv, scalar=qs, in1=rsd[:, 0:ntok], op0=ALU.mult, op1=ALU.mult), reads=[y_, rsd], writes=[y_])
                        kb.dma("sp", GC[cc * P:(cc + 1) * P, tok0:tok0 + ntok], yv, reads=[y_], pwrites=[GC])
                kb.barrier()

            with ExitStack() as ph:
              if not getattr(cfg, 'gdn_only_pre', False):
                  mk = {k_: cmask(ph, k_) for k_ in ("BD", "LINC0", "LINC1", "NEG0", "NEG1", "NEGI0", "NEGI1")}
                  bdc = kb.sb(ph, "d_bdc", [P, 2], F32)
                  kb.dma("sp", bdc[:], c_bdcol[:, :], reads=[c_bdcol], writes=[bdc])
                  negones = kb.sb(ph, "d_negones", [P, P], F32)
                  kb.op("dve", lambda e: e.memset(negones[:], -1.0), writes=[negones])
                  LABs = kb.sb(ph, "d_LABs", [P, NU, 32], F32)
                  par = kb.sb(ph, "d_par", [P, 32], F32)
                  nwg = kb.sb(ph, "d_nwg", [P, 1], F32)
                  kb.dma("sp", par[:, 0:16], gdn_a_log[l, :, :].partition_broadcast(P), reads=[gdn_a_log], writes=[par])
                  kb.dma("sp", par[:, 16:32], gdn_dt_bias[l, :, :].partition_broadcast(P), reads=[gdn_dt_bias], pwrites=[par])
                  kb.dma("sp", nwg[:], gdn_norm_wT[l, :, :], reads=[gdn_norm_wT], writes=[nwg])
                  kb.op("act", lambda e: e.activation(out=par[:, 0:16], in_=par[:, 0:16], func=AF.Exp), reads=[par], writes=[par])
                  kb.op("dve", lambda e: e.tensor_scalar(par[:, 0:16], par[:, 0:16], -1.0, None, ALU.mult), reads=[par], writes=[par])
                  abT = [kb.sb(ph, "d_abT%d" % i, [32, P], F32) for i in range(2)]
                  abx = kb.sb(ph, "d_abx", [P, 16], F32)
                  for u in range(NU):
                      a_ = abT[u % 2]
                      kb.dma("sp", a_[:], PT[g0 + 4096:g0 + 4128, u * P:(u + 1) * P], reads=[PT], writes=[a_])
                      kb.op("pe", lambda e, a_=a_: e.transpose(PS[0][:, 0:32], a_[:, :], ident_f[0:32, 0:32]), reads=[a_, ident_f], writes=[PS[0]])
                      kb.op("dve", lambda e: e.tensor_tensor(abx[:], PS[0][:, 0:16], par[:, 16:32], ALU.add), reads=[PS[0], par], writes=[abx])
                      kb.op("act", lambda e: e.activation(out=abx[:], in_=abx[:], func=AF.Exp), reads=[abx], writes=[abx])
                      kb.op("act", lambda e: e.activation(out=abx[:], in_=abx[:], func=AF.Ln, bias=ones_f[:, 0:1], scale=1.0), reads=[abx, ones_f], writes=[abx])
                      kb.op("dve", lambda e, u=u: e.tensor_tensor(LABs[:, u, 0:16], abx[:], par[:, 0:16], ALU.mult), reads=[abx, par], pwrites=[LABs])
                      kb.op("act", lambda e, u=u: e.activation(out=LABs[:, u, 16:32], in_=PS[0][:, 16:32], func=AF.Sigmoid), reads=[PS[0]], pwrites=[LABs])
                  ld = [[kb.sb(ph, "d_%s%d" % (nm, i), [P, 8, P], F32) for i in range(2)] for nm in ("qT", "kT", "vT")]
                  bk8 = kb.sb(ph, "d_bk8", [P, 8, P], F32)
                  bek8 = kb.sb(ph, "d_bek8", [P, 8, P], F32)
                  kend8 = kb.sb(ph, "d_kend8", [P, 8, P], F32)
                  bv8 = kb.sb(ph, "d_bv8", [P, 8, P], F32)
                  LAL8 = kb.sb(ph, "d_LAL8", [P, 8, P], F32)
                  F0 = kb.sb(ph, "d_F0", [P, 8, P], F32)
                  F0T = kb.sb(ph, "d_F0T", [P, 8, P], F32)
                  F0Ti = kb.sb(ph, "d_F0Ti", [P, 8, P], F32)
                  bkT8 = kb.sb(ph, "d_bkT8", [P, 8, P], F32)
                  zq = [kb.sb(ph, "d_zq%d" % i, [P, 4, 384], BF16) for i in range(2)]
                  Yg = kb.sb(ph, "d_Yg", [P, 8, P], F32)
                  qkT8 = kb.sb(ph, "d_qkT8", [P, 8, P], F32)
                  u8 = kb.sb(ph, "d_u8", [P, 8, P], F32)
                  wT8 = kb.sb(ph, "d_wT8", [P, 8, P], F32)
                  S8 = kb.sb(ph, "d_S8", [P, 8, P], F32)
                  dl8 = kb.sb(ph, "d_dl8", [P, 8, P], F32)
                  o18 = kb.sb(ph, "d_o18", [P, 8, P], F32)
                  O8 = kb.sb(ph, "d_O8", [P, 8, P], F32)
                  sm8 = kb.sb(ph, "d_sm8", [P, 64], F32)
                  decb = kb.sb(ph, "d_decb", [P, 2, 8], F32)
                  zT8 = [kb.sb(ph, "d_zT%d" % i, [P, 8, P], F32) for i in range(2)]
                  ofd = [kb.sb(ph, "d_of%d" % i, [P, 8, P], F32) for i in range(2)]
                  sq_ = kb.sb(ph, "d_sq", [P, 8, P], F32)
                  ob8 = [kb.sb(ph, "d_ob%d" % i, [P, 8, P], BF16) for i in range(2)]
                  b8 = lambda t_: [PS[i] for i in t_]
                  for d in range(2):
                      LINCd, NEGd, NEGId, NEGt = mk["LINC%d" % d], mk["NEG%d" % d], mk["NEGI%d" % d], mk["NEG%d" % (1 - d)]
                      kb.op("dve", lambda e: e.memset(S8[:], 0.0), writes=[S8])
                      for ui, u in enumerate(unit_order(d)):
                          t0 = u * P
                          qT8, kT8, vT8 = ld[0][ui % 2], ld[1][ui % 2], ld[2][ui % 2]
                          for j_, t_ in enumerate((qT8, kT8, vT8)):
                              kb.dma("sp", t_[:], GC[j_ * 1024:(j_ + 1) * 1024, t0:t0 + P].rearrange("(h p) t -> p h t", p=P), reads=[GC], writes=[t_])
                          if d == 1:
                              z_, of_ = zT8[ui % 2], ofd[ui % 2]
                              kb.dma("sp", z_[:], PT[g0 + 3072:g0 + 4096, t0:t0 + P].rearrange("(h p) t -> p h t", p=P), reads=[PT], writes=[z_])
                              kb.dma("sp", of_[:], OFD[t0:t0 + P, :].rearrange("t (h v) -> t h v", h=8), reads=[OFD], writes=[of_])
                          la8 = LABs[:, u, 8 * d:8 * d + 8]
                          be8 = LABs[:, u, 16 + 8 * d:24 + 8 * d]
                          kb.op("pe", lambda e: e.matmul(PS[0][:, 0:8], LINCd[:, :], la8, start=True, stop=True), reads=[LINCd, LABs], writes=[PS[0]])
                          kb.op("pe", lambda e: e.matmul(PS[0][:, 8:16], mk["BD"][:, :], la8, start=True, stop=True), reads=[mk["BD"], LABs], pwrites=[PS[0]])
                          kb.op("act", lambda e: e.activation(out=sm8[:, 0:8], in_=PS[0][:, 0:8], func=AF.Copy), reads=[PS[0]], writes=[sm8])
                          kb.op("act", lambda e: e.activation(out=sm8[:, 8:16], in_=PS[0][:, 0:8], func=AF.Exp), reads=[PS[0]], pwrites=[sm8])
                          kb.op("dve", lambda e: e.tensor_tensor(sm8[:, 24:32], PS[0][:, 8:16], sm8[:, 0:8], ALU.subtract), reads=[PS[0], sm8], pwrites=[sm8])
                          kb.op("act", lambda e: e.activation(out=sm8[:, 16:24], in_=sm8[:, 24:32], func=AF.Exp), reads=[sm8], pwrites=[sm8])
                          for s in range(2):
                              kb.op("dve", lambda e, s=s: e.tensor_scalar(sm8[:, 32 + 8 * s:40 + 8 * s], la8, bdc[:, s:s + 1], None, ALU.mult), reads=[LABs, bdc], pwrites=[sm8])
                          kb.op("pe", lambda e: e.matmul(PS[0][:, 16:32], ones_f[:, :], sm8[:, 32:48], start=True, stop=True), reads=[ones_f, sm8], pwrites=[PS[0]])
                          kb.op("act", lambda e: e.activation(out=decb[:].rearrange("p s h -> p (s h)"), in_=PS[0][:, 16:32], func=AF.Exp), reads=[PS[0]], writes=[decb])
                          for h in range(8):
                              kb.op("pe", lambda e, h=h: e.transpose(PS[2 + h // 4][:, (h % 4) * P:(h % 4 + 1) * P], kT8[:, h, :], ident_f[:]), reads=[kT8, ident_f],
                                    writes=[PS[2 + h // 4]] if h % 4 == 0 else (), pwrites=() if h % 4 == 0 else [PS[2 + h // 4]])
                              kb.op("pe", lambda e, h=h: e.transpose(PS[4 + h // 4][:, (h % 4) * P:(h % 4 + 1) * P], vT8[:, h, :], ident_f[:]), reads=[vT8, ident_f],
                                    writes=[PS[4 + h // 4]] if h % 4 == 0 else (), pwrites=() if h % 4 == 0 else [PS[4 + h // 4]])
                          kps = PSA[:, 2:4, :].rearrange("p b (h t) -> p (b h) t", h=4)
                          vps = PSA[:, 4:6, :].rearrange("p b (h t) -> p (b h) t", h=4)
                          kb.op("dve", lambda e: e.tensor_tensor(bk8[:], kps, bc(be8, [P, 8, P], [2]), ALU.mult), reads=b8((2, 3)) + [LABs], writes=[bk8])
                          kb.op("dve", lambda e: e.tensor_tensor(kend8[:], kps, bc(sm8[:, 16:24], [P, 8, P], [2]), ALU.mult), reads=b8((2, 3)) + [sm8], writes=[kend8])
                          kb.op("dve", lambda e: e.tensor_tensor(bv8[:], vps, bc(be8, [P, 8, P], [2]), ALU.mult), reads=b8((4, 5)) + [LABs], writes=[bv8])
                          kb.op("pool", lambda e: e.tensor_tensor(bek8[:], bk8[:], bc(sm8[:, 8:16], [P, 8, P], [2]), ALU.mult), reads=[bk8, sm8], writes=[bek8])
                          kb.op("pool", lambda e: e.tensor_tensor(LAL8[:], bc(LINCd[:], [P, 8, P], [1]), bc(la8, [P, 8, P], [2]), ALU.mult), reads=[LINCd, LABs], writes=[LAL8])
                          for h in range(8):
                              pr = PS[6 + h // 4]
                              kb.op("pe", lambda e, h=h, pr=pr: e.matmul(pr[:, (h % 4) * P:(h % 4 + 1) * P], LAL8[:, h, :], ones_f[:, :], start=True, stop=False), reads=[LAL8, ones_f],
                                    writes=[pr] if h % 4 == 0 else (), pwrites=() if h % 4 == 0 else [pr])
                              kb.op("pe", lambda e, h=h, pr=pr: e.matmul(pr[:, (h % 4) * P:(h % 4 + 1) * P], negones[:, :], LAL8[:, h, :], start=False, stop=True), reads=[LAL8, negones], pwrites=[pr])
                          rps = PSA[:, 6:8, :].rearrange("p b (h t) -> p (b h) t", h=4)
                          kb.op("dve", lambda e: e.tensor_tensor(F0[:], rps, bc(NEGt[:], [P, 8, P], [1]), ALU.add), reads=b8((6, 7)) + [NEGt], writes=[F0])
                          kb.op("act", lambda e: e.activation(out=F0[:], in_=F0[:], func=AF.Exp), reads=[F0], writes=[F0])
                          kb.op("dve", lambda e: e.scalar_tensor_tensor(out=F0T[:], in0=rps, scalar=-1.0, in1=bc(NEGd[:], [P, 8, P], [1]), op0=ALU.mult, op1=ALU.add), reads=b8((6, 7)) + [NEGd], writes=[F0T])
                          kb.op("act", lambda e: e.activation(out=F0T[:], in_=F0T[:], func=AF.Exp), reads=[F0T], writes=[F0T])
                          kb.op("dve", lambda e: e.scalar_tensor_tensor(out=F0Ti[:], in0=rps, scalar=-1.0, in1=bc(NEGId[:], [P, 8, P], [1]), op0=ALU.mult, op1=ALU.add), reads=b8((6, 7)) + [NEGId], writes=[F0Ti])
                          kb.op("act", lambda e: e.activation(out=F0Ti[:], in_=F0Ti[:], func=AF.Exp), reads=[F0Ti], writes=[F0Ti])
                          for h in range(8):
                              kb.op("pe", lambda e, h=h: e.transpose(PS[2 + h // 4][:, (h % 4) * P:(h % 4 + 1) * P], bk8[:, h, :], ident_f[:]), reads=[bk8, ident_f],
                                    writes=[PS[2 + h // 4]] if h % 4 == 0 else (), pwrites=() if h % 4 == 0 else [PS[2 + h // 4]])
                          kb.op("act", lambda e: e.activation(out=bkT8[:], in_=kps, func=AF.Copy), reads=b8((2, 3)), writes=[bkT8])
                          for qd in range(2):
                              z = zq[qd]
                              for hh in range(4):
                                  h = qd * 4 + hh
                                  kb.op("pe", lambda e, h=h, hh=hh: e.matmul(PS[4][:, hh * P:(hh + 1) * P], bkT8[:, h, :], kT8[:, h, :], start=True, stop=True), reads=[bkT8, kT8],
                                        writes=[PS[4]] if hh == 0 else (), pwrites=() if hh == 0 else [PS[4]])
                                  kb.op("pe", lambda e, h=h, hh=hh: e.matmul(PS[5][:, hh * P:(hh + 1) * P], kT8[:, h, :], bkT8[:, h, :], start=True, stop=True), reads=[bkT8, kT8],
                                        writes=[PS[5]] if hh == 0 else (), pwrites=() if hh == 0 else [PS[5]])
                                  kb.op("pe", lambda e, h=h, hh=hh: e.matmul(PS[6][:, hh * P:(hh + 1) * P], kT8[:, h, :], qT8[:, h, :], start=True, stop=True), reads=[qT8, kT8],
                                        writes=[PS[6]] if hh == 0 else (), pwrites=() if hh == 0 else [PS[6]])
                              v4 = lambda b_: PS[b_][:, :].rearrange("p (h t) -> p h t", h=4)
                              kb.op("dve", lambda e, z=z, qd=qd: e.tensor_tensor(z[:, :, 256:384], v4(4), F0[:, qd * 4:qd * 4 + 4, :], ALU.mult), reads=[PS[4], F0], writes=[z])
                              kb.op("dve", lambda e, z=z, qd=qd: e.tensor_tensor(z[:, :, 128:256], v4(5), F0T[:, qd * 4:qd * 4 + 4, :], ALU.mult), reads=[PS[5], F0T], pwrites=[z])
                              kb.op("dve", lambda e, qd=qd: e.tensor_tensor(qkT8[:, qd * 4:qd * 4 + 4, :], v4(6), F0Ti[:, qd * 4:qd * 4 + 4, :], ALU.mult), reads=[PS[6], F0Ti],
                                    writes=[qkT8] if qd == 0 else (), pwrites=() if qd == 0 else [qkT8])
                              kb.op("pool", lambda e, z=z: e.tensor_tensor(z[:, :, 0:128], bc(ident_f[:], [P, 4, P], [1]), z[:, :, 128:256], ALU.subtract), reads=[ident_f, z], pwrites=[z])
                          emit_solve(zq, [0, 4])
                          for i2 in range(2):
                              kb.op("pool", lambda e, i2=i2: e.tensor_copy(Yg[:, 4 * i2:4 * i2 + 4, :], zq[i2][:, :, 0:128]), reads=[zq[i2]], pwrites=[Yg])
                          for h in range(8):
                              z = Yg
                              kb.op("pe", lambda e, h=h, z=z: e.matmul(PS[0 + h // 4][:, (h % 4) * P:(h % 4 + 1) * P], z[:, h, :], bv8[:, h, :], start=True, stop=True), reads=[z, bv8],
                                    writes=[PS[h // 4]] if h % 4 == 0 else (), pwrites=() if h % 4 == 0 else [PS[h // 4]])
                              kb.op("pe", lambda e, h=h, z=z: e.matmul(PS[2 + h // 4][:, (h % 4) * P:(h % 4 + 1) * P], bek8[:, h, :], z[:, h, :], start=True, stop=True), reads=[z, bek8],
                                    writes=[PS[2 + h // 4]] if h % 4 == 0 else (), pwrites=() if h % 4 == 0 else [PS[2 + h // 4]])
                          kb.op("act", lambda e: e.activation(out=u8[:], in_=PSA[:, 0:2, :].rearrange("p b (h t) -> p (b h) t", h=4), func=AF.Copy), reads=b8((0, 1)), writes=[u8])
                          kb.op("dve", lambda e: e.tensor_copy(wT8[:], kps), reads=b8((2, 3)), writes=[wT8])
                          for s in ((0, 1) if d == 0 else (1, 0)):
                              rs = slice(64 * s, 64 * s + 64)
                              for h in range(8):
                                  kb.op("pe", lambda e, h=h: e.matmul(PS[0 + h // 4][:, (h % 4) * P:(h % 4 + 1) * P], wT8[:, h, :], S8[:, h, :], start=True, stop=True), reads=[wT8, S8],
                                        writes=[PS[h // 4]] if h % 4 == 0 else (), pwrites=() if h % 4 == 0 else [PS[h // 4]])
                                  kb.op("pe", lambda e, h=h: e.matmul(PS[2 + h // 4][:, (h % 4) * P:(h % 4 + 1) * P], qT8[:, h, :], S8[:, h, :], start=True, stop=True), reads=[qT8, S8],
                                        writes=[PS[2 + h // 4]] if h % 4 == 0 else (), pwrites=() if h % 4 == 0 else [PS[2 + h // 4]])
                              kb.op("dve", lambda e, rs=rs: e.tensor_tensor(dl8[rs, :, :], u8[rs, :, :], PSA[rs, 0:2, :].rearrange("p b (h t) -> p (b h) t", h=4), ALU.subtract),
                                    reads=[u8] + b8((0, 1)), writes=[dl8])
                              kb.op("dve", lambda e, rs=rs: e.tensor_tensor(o18[rs, :, :], PSA[rs, 2:4, :].rearrange("p b (h t) -> p (b h) t", h=4), bc(sm8[rs, 8:16], [64, 8, P], [2]), ALU.mult),
                                    reads=[sm8] + b8((2, 3)), writes=[o18])
                              for h in range(8):
                                  kb.op("pe", lambda e, h=h, rs=rs: e.matmul(PS[4 + h // 4][:, (h % 4) * P:(h % 4 + 1) * P], qkT8[rs, h, :], dl8[rs, h, :], start=True, stop=True), reads=[qkT8, dl8],
                                        writes=[PS[4 + h // 4]] if h % 4 == 0 else (), pwrites=() if h % 4 == 0 else [PS[4 + h // 4]])
                                  kb.op("pe", lambda e, h=h, rs=rs: e.matmul(PS[6 + h // 4][:, (h % 4) * P:(h % 4 + 1) * P], kend8[rs, h, :], dl8[rs, h, :], start=True, stop=True), reads=[kend8, dl8],
                                        writes=[PS[6 + h // 4]] if h % 4 == 0 else (), pwrites=() if h % 4 == 0 else [PS[6 + h // 4]])
                              kb.op("dve", lambda e, rs=rs: e.tensor_tensor(O8[rs, :, :], o18[rs, :, :], PSA[rs, 4:6, :].rearrange("p b (h t) -> p (b h) t", h=4), ALU.add),
                                    reads=[o18] + b8((4, 5)), pwrites=[O8])
                              kb.op("pool", lambda e, s=s: e.tensor_tensor(S8[:], S8[:], bc(decb[:, s, :], [P, 8, P], [2]), ALU.mult), reads=[S8, decb], writes=[S8])
                              kb.op("dve", lambda e: e.tensor_tensor(S8[:], S8[:], rps, ALU.add), reads=[S8] + b8((6, 7)), writes=[S8])
                          if d == 0:
                              kb.dma("sp", OFD[t0:t0 + P, :].rearrange("t (h v) -> t h v", h=8), O8[:], reads=[O8], pwrites=[OFD])
                          else:
                              kb.op("dve", lambda e, of_=of_: e.tensor_tensor(O8[:], O8[:], of_[:], ALU.add), reads=[O8, of_], writes=[O8])
                              kb.op("pool", lambda e: e.tensor_tensor(sq_[:], O8[:], O8[:], ALU.mult), reads=[O8], writes=[sq_])
                              kb.op("dve", lambda e: e.tensor_reduce(out=sm8[:, 48:56], in_=sq_[:], axis=mybir.AxisListType.X, op=ALU.add), reads=[sq_], pwrites=[sm8])
                              kb.op("dve", lambda e: e.tensor_scalar(sm8[:, 48:56], sm8[:, 48:56], 1.0 / 128.0, 1e-6, ALU.mult, ALU.add), reads=[sm8], pwrites=[sm8])
                              kb.op("act", lambda e: e.activation(out=sm8[:, 48:56], in_=sm8[:, 48:56], func=AF.Sqrt), reads=[sm8], pwrites=[sm8])
                              kb.op("dve", lambda e: e.reciprocal(sm8[:, 48:56], sm8[:, 48:56]), reads=[sm8], pwrites=[sm8])
                              kb.op("dve", lambda e: e.tensor_tensor(O8[:], O8[:], bc(sm8[:, 48:56], [P, 8, P], [2]), ALU.mult), reads=[O8, sm8], writes=[O8])
                              for h in range(8):
                                  kb.op("pe", lambda e, h=h: e.transpose(PS[h // 4][:, (h % 4) * P:(h % 4 + 1) * P], O8[:, h, :], ident_f[:]), reads=[O8, ident_f],
                                        writes=[PS[h // 4]] if h % 4 == 0 else (), pwrites=() if h % 4 == 0 else [PS[h // 4]])
                              kb.op("act", lambda e, z_=z_: e.activation(out=z_[:], in_=z_[:], func=AF.Silu), reads=[z_], writes=[z_])
                              kb.op("dve", lambda e, z_=z_: e.scalar_tensor_tensor(out=sq_[:], in0=PSA[:, 0:2, :].rearrange("p b (h t) -> p (b h) t", h=4), scalar=nwg[:, 0:1], in1=z_[:], op0=ALU.mult, op1=ALU.mult),
                                    reads=b8((0, 1)) + [nwg, z_], writes=[sq_])
                              ob_ = ob8[ui % 2]
                              kb.op("act", lambda e, ob_=ob_: e.activation(out=ob_[:], in_=sq_[:], func=AF.Copy), reads=[sq_], writes=[ob_])
                              kb.dma("sp", OT[2048:3072, t0:t0 + P].rearrange("(h p) t -> p h t", p=P), ob_[:], reads=[ob_], pwrites=[OT])
                  kb.barrier()

        if "rwkv" in phases:
            r0_ = cfg.rwkv_off
            with ExitStack() as ph:
                PP = kb.sb(ph, "r_PP", [P, 27, 512], F32)
                xh = [kb.sb(ph, "r_xh%d" % i, [P, 514], F32) for i in range(2)]
                ssum = [kb.sb(ph, "r_ss%d" % i, [P, 512], F32) for i in range(2)]
                mu = kb.sb(ph, "r_mu", [P, 27, 2], F32)
                W2t = kb.sb(ph, "r_W2t", [P, 1024], F32)
                A2t = kb.sb(ph, "r_A2t", [P, 1024], F32)
                G2t = kb.sb(ph, "r_G2t", [P, 1024], F32)
                pr8 = kb.sb(ph, "r_pr8", [P, 7, 8], F32)
                w0a0 = kb.sb(ph, "r_w0a0", [P, 2, 2, 8], F32)
                bo64 = cmask(ph, "BD")
                tl = kb.sb(ph, "r_tl", [P, 512], F32)
                wk = [kb.sb(ph, "r_wk%d" % i, [P, 512], F32) for i in range(6)]
                kkc = kb.sb(ph, "r_kkc", [P, 8, 512], F32)
                mu0 = kb.sb(ph, "r_mu0", [P, 27], F32)
                kb.dma("sp", mu0[:], rwkv_muT[l, :, :], reads=[rwkv_muT], writes=[mu0])
                kb.op("dve", lambda e: e.tensor_scalar(mu[:, :, 1], mu0[:], 0.5, None, ALU.mult), reads=[mu0], writes=[mu])
                kb.op("dve", lambda e: e.tensor_scalar(mu[:, :, 0], mu0[:], -1.0, 1.0, ALU.mult, ALU.add), reads=[mu0], pwrites=[mu])
                kb.dma("sp", W2t[:], rwkv_w2[l, :, :, :].rearrange("d r c -> (d r) c"), reads=[rwkv_w2], writes=[W2t])
                kb.dma("sp", A2t[:], rwkv_a2[l, :, :, :].rearrange("d r c -> (d r) c"), reads=[rwkv_a2], writes=[A2t])
                kb.dma("sp", G2t[:], rwkv_g2[l, :, :], reads=[rwkv_g2], writes=[G2t])
                kb.dma("sp", pr8[:, 0:4, :], rwkv_p4T[l, :, :, :], reads=[rwkv_p4T], writes=[pr8])
                kb.dma("sp", w0a0[:], rwkv_w0a0T[l, :, :, :, :], reads=[rwkv_w0a0T], writes=[w0a0])
                kb.op("dve", lambda e: e.tensor_scalar(pr8[:, 3, :], pr8[:, 1, :], -1.0, 1.0, ALU.mult, ALU.add), reads=[pr8], pwrites=[pr8])
                seqs = [(0, cfg.n_ctx), (cfg.n_ctx, n)]
                blocks = []
                for (s0, s1) in seqs:
                    for t0 in range(s0, s1, 512):
                        blocks.append((t0, min(512, s1 - t0), s0, s1))
                xi = 0
                for (t0, tw, s0, s1) in blocks:
                    for c in range(27):
                        x_ = xh[xi % 2]
                        s_ = ssum[xi % 2]
                        xi += 1
                        lo, hi = max(t0 - 1, s0), min(t0 + tw + 1, s1)
                        if lo > t0 - 1 or hi < t0 + tw + 1:
                            kb.op("pool", lambda e, x_=x_: e.memset(x_[:], 0.0), writes=[x_])
                            wr = dict(pwrites=[x_])
                        else:
                            wr = dict(writes=[x_])
                        kb.dma("sp", x_[:, lo - (t0 - 1):hi - (t0 - 1)], PT[r0_ + c * P:r0_ + (c + 1) * P, lo:hi], reads=[PT], **wr)
                        kb.op("pool", lambda e, x_=x_, s_=s_: e.tensor_tensor(s_[:, 0:tw], x_[:, 0:tw], x_[:, 2:tw + 2], ALU.add), reads=[x_], writes=[s_])
                        kb.op("dve", lambda e, x_=x_, c=c: e.tensor_scalar(PP[:, c, 0:tw], x_[:, 1:tw + 1], mu[:, c, 0:1], None, ALU.mult), reads=[x_, mu], pwrites=[PP])
                        kb.op("dve", lambda e, s_=s_, c=c: e.scalar_tensor_tensor(out=PP[:, c, 0:tw], in0=s_[:, 0:tw], scalar=mu[:, c, 1:2], in1=PP[:, c, 0:tw], op0=ALU.mult, op1=ALU.add),
                              reads=[s_, mu, PP], pwrites=[PP])
                    sto = lambda dst, c, src_t: kb.dma("sp", dst[c * P:(c + 1) * P, t0:t0 + tw], src_t, reads=[], pwrites=[dst])
                    for c in range(8):
                        kb.dma("sp", RW["R"][c * P:(c + 1) * P, t0:t0 + tw], PP[:, c, 0:tw], reads=[PP], pwrites=[RW["R"]])
                        kb.dma("sp", RW["V"][c * P:(c + 1) * P, t0:t0 + tw], PP[:, 16 + c, 0:tw], reads=[PP], pwrites=[RW["V"]])
                    wi_ = 0
                    for c in range(8):
                        a_, b_ = wk[wi_ % 6], wk[(wi_ + 1) % 6]
                        wi_ += 2
                        kb.op("dve", lambda e, a_=a_, c=c: e.tensor_scalar(a_[:, 0:tw], PP[:, 8 + c, 0:tw], pr8[:, 0, c:c + 1], None, ALU.mult), reads=[PP, pr8], writes=[a_])
                        kb.op("pool", lambda e, a_=a_, b_=b_: e.tensor_tensor(b_[:, 0:tw], a_[:, 0:tw], a_[:, 0:tw], ALU.mult), reads=[a_], writes=[b_])
                        kb.op("pe", lambda e, b_=b_: e.matmul(PS[0][:, 0:tw], bo64[:, :], b_[:, 0:tw], start=True, stop=True), reads=[bo64, b_], writes=[PS[0]])
                        kb.op("dve", lambda e, b_=b_: e.tensor_scalar(b_[:, 0:tw], PS[0][:, 0:tw], 1e-6, None, ALU.add), reads=[PS[0]], writes=[b_])
                        kb.op("act", lambda e, b_=b_: e.activation(out=b_[:, 0:tw], in_=b_[:, 0:tw], func=AF.Sqrt), reads=[b_], writes=[b_])
                        kb.op("dve", lambda e, b_=b_: e.reciprocal(b_[:, 0:tw], b_[:, 0:tw]), reads=[b_], writes=[b_])
                        kb.op("dve", lambda e, a_=a_, b_=b_, c=c: e.tensor_tensor(kkc[:, c, 0:tw], a_[:, 0:tw], b_[:, 0:tw], ALU.mult), reads=[a_, b_], pwrites=[kkc])
                        kb.dma("sp", RW["KK"][c * P:(c + 1) * P, t0:t0 + tw], kkc[:, c, 0:tw], reads=[kkc], pwrites=[RW["KK"]])
                    kb.op("act", lambda e: e.activation(out=tl[:, 0:tw], in_=PP[:, 26, 0:tw], func=AF.Sigmoid), reads=[PP], writes=[tl])
                    for c in range(8):
                        a_ = wk[wi_ % 6]
                        wi_ += 1
                        ps = PS[1 + c % 2]
                        kb.op("pe", lambda e, ps=ps, c=c: e.matmul(ps[:, 0:tw], G2t[:, c * P:(c + 1) * P], tl[:, 0:tw], start=True, stop=True), reads=[G2t, tl], writes=[ps])
                        kb.op("act", lambda e, ps=ps, a_=a_: e.activation(out=a_[:, 0:tw], in_=ps[:, 0:tw], func=AF.Copy), reads=[ps], writes=[a_])
                        kb.dma("sp", RW["G"][c * P:(c + 1) * P, t0:t0 + tw], a_[:, 0:tw], reads=[a_], pwrites=[RW["G"]])
                        b_ = wk[wi_ % 6]
                        wi_ += 1
                        kb.op("dve", lambda e, b_=b_, c=c: e.scalar_tensor_tensor(out=b_[:, 0:tw], in0=PP[:, c, 0:tw], scalar=pr8[:, 2, c:c + 1], in1=PP[:, 8 + c, 0:tw], op0=ALU.mult, op1=ALU.mult),
                              reads=[PP, pr8], writes=[b_])
                        ps2 = PS[3 + c % 2]
                        kb.op("pe", lambda e, ps2=ps2, b_=b_: e.matmul(ps2[:, 0:tw], bo64[:, :], b_[:, 0:tw], start=True, stop=True), reads=[bo64, b_], writes=[ps2])
                        kb.op("dve", lambda e, ps2=ps2, b_=b_, c=c: e.tensor_tensor(b_[:, 0:tw], ps2[:, 0:tw], PP[:, 16 + c, 0:tw], ALU.mult), reads=[ps2, PP], writes=[b_])
                        kb.dma("sp", RW["BON"][c * P:(c + 1) * P, t0:t0 + tw], b_[:, 0:tw], reads=[b_], pwrites=[RW["BON"]])
                    for d in range(2):
                        hs = slice(64 * d, 64 * d + 64)
                        kb.fence_pe()
                        kb.op("act", lambda e, hs=hs: e.activation(out=tl[hs, 0:tw], in_=PP[hs, 24, 0:tw], func=AF.Tanh), reads=[PP], writes=[tl])
                        for c in range(8):
                            a_, b_, c_ = wk[wi_ % 6], wk[(wi_ + 1) % 6], wk[(wi_ + 2) % 6]
                            wi_ += 3
                            ps = PS[1 + c % 2]
                            kb.op("pe", lambda e, ps=ps, c=c, hs=hs: e.matmul(ps[:, 0:tw], W2t[hs, c * P:(c + 1) * P], tl[hs, 0:tw], start=True, stop=True), reads=[W2t, tl], writes=[ps])
                            kb.op("act", lambda e, ps=ps, a_=a_, c=c, d=d: e.activation(out=a_[:, 0:tw], in_=ps[:, 0:tw], func=AF.Sigmoid, bias=w0a0[:, 0, d, c:c + 1], scale=1.0), reads=[ps, w0a0], writes=[a_])
                            kb.op("dve", lambda e, a_=a_: e.tensor_scalar(a_[:, 0:tw], a_[:, 0:tw], -0.6065306597126334, None, ALU.mult), reads=[a_], writes=[a_])
                            kb.dma("sp", RW["LW%d" % d][c * P:(c + 1) * P, t0:t0 + tw], a_[:, 0:tw], reads=[a_], pwrites=[RW["LW%d" % d]])
                            ps2 = PS[3 + c % 2]
                            kb.op("pe", lambda e, ps2=ps2, c=c, hs=hs: e.matmul(ps2[:, 0:tw], A2t[hs, c * P:(c + 1) * P], PP[hs, 25, 0:tw], start=True, stop=True), reads=[A2t, PP], writes=[ps2])
                            kb.op("act", lambda e, ps2=ps2, b_=b_, c=c, d=d: e.activation(out=b_[:, 0:tw], in_=ps2[:, 0:tw], func=AF.Sigmoid, bias=w0a0[:, 1, d, c:c + 1], scale=1.0), reads=[ps2, w0a0], writes=[b_])
                            kb.op("dve", lambda e, b_=b_, c_=c_, c=c: e.tensor_scalar(c_[:, 0:tw], b_[:, 0:tw], pr8[:, 1, c:c + 1], pr8[:, 3, c:c + 1], ALU.mult, ALU.add), reads=[b_, pr8], writes=[c_])
                            kb.op("pool", lambda e, c_=c_, c=c: e.tensor_tensor(c_[:, 0:tw], c_[:, 0:tw], PP[:, 8 + c, 0:tw], ALU.mult), reads=[c_, PP], writes=[c_])
                            kb.dma("sp", RW["KH%d" % d][c * P:(c + 1) * P, t0:t0 + tw], c_[:, 0:tw], reads=[c_], pwrites=[RW["KH%d" % d]])
                            kb.op("pool", lambda e, b_=b_, c=c: e.tensor_tensor(b_[:, 0:tw], b_[:, 0:tw], kkc[:, c, 0:tw], ALU.mult), reads=[b_, kkc], writes=[b_])
                            kb.dma("sp", RW["BB%d" % d][c * P:(c + 1) * P, t0:t0 + tw], b_[:, 0:tw], reads=[b_], pwrites=[RW["BB%d" % d]])
                kb.barrier()

            with ExitStack() as ph:
              if not getattr(cfg, 'rwkv_only_pre', False):
               try:
                CUT = getattr(cfg, 'rwkv_cut', 0)
                def cut(k_):
                    if CUT == k_:
                        raise StopIteration
                mk = {k_: cmask(ph, k_) for k_ in ("BD", "LINC0", "LINC1", "LSTR0", "LSTR1")}
                CATR = [kb.sb(ph, "w_cat%d" % d, [P, 386], F32) for d in range(2)]
                for d in range(2):
                    kb.dma("sp", CATR[d][:, 0:128], c_masks[MI["LINC%d" % d], :, :], reads=[c_masks], writes=[CATR[d]])
                    kb.dma("sp", CATR[d][:, 128:256], c_masks[MI["LSTR%d" % d], :, :], reads=[c_masks], pwrites=[CATR[d]])
                    kb.dma("sp", CATR[d][:, 256:384], c_masks[MI["MEND%d" % d], :, :], reads=[c_masks], pwrites=[CATR[d]])
                    kb.dma("sp", CATR[d][:, 384:386], c_bdcol[:, :], reads=[c_bdcol], pwrites=[CATR[d]])
                lnp = kb.sb(ph, "w_lnp", [P, 2, 8], F32)
                kb.dma("sp", lnp[:], rwkv_lnT[l, :, :, :], reads=[rwkv_lnT], writes=[lnp])
                ldn = ("R", "V", "KK", "LW", "KH", "BB")
                ld = {nm: kb.sb(ph, "w_%s" % nm, [P, 8, P], F32) for nm in ldn}
                lwtm = kb.sb(ph, "w_lwtm", [P, 8, P], F32)
                Wg = [kb.sb(ph, "w_Wg%d" % i, [P, 4, P], F32) for i in range(4)]
                dec8 = kb.sb(ph, "w_dec8", [P, 8, 2], F32)
                AR = kb.sb(ph, "w_AR", [P, 8, 256], F32)
                BtT = kb.sb(ph, "w_BtT", [P, 8, P], F32)
                KtT = kb.sb(ph, "w_KtT", [P, 8, P], F32)
                BtT2 = kb.sb(ph, "w_BtT2", [P, 8, 2, P], F32)
                KtT2 = kb.sb(ph, "w_KtT2", [P, 8, 2, P], F32)
                bdc2 = kb.sb(ph, "w_bdc2", [P, 2], F32)
                kb.dma("sp", bdc2[:], c_bdcol[:, :], reads=[c_bdcol], writes=[bdc2])
                BeT = kb.sb(ph, "w_BeT", [P, 8, P], F32)
                KeT = kb.sb(ph, "w_KeT", [P, 8, P], F32)
                vtm = kb.sb(ph, "w_vtm", [P, 8, P], F32)
                Atm = kb.sb(ph, "w_Atm", [P, 8, P], F32)
                Betm = kb.sb(ph, "w_Betm", [P, 8, P], F32)
                Ketm = kb.sb(ph, "w_Ketm", [P, 8, P], F32)
                zq = [kb.sb(ph, "w_zq%d" % i, [P, 4, 384], BF16) for i in range(2)]
                Y16 = kb.sb(ph, "w_Y16", [P, 16, P], F32)
                PrbT = kb.sb(ph, "w_PrbT", [P, 16, P], F32)
                MakT = kb.sb(ph, "w_MakT", [P, 4, P], F32)
                PrkT = kb.sb(ph, "w_PrkT", [P, 4, P], F32)
                nmv = kb.sb(ph, "w_nmv", [P, 16, 64], F32)
                O0 = kb.sb(ph, "w_O0", [P, 16, 64], F32)
                U0 = kb.sb(ph, "w_U0", [P, 16, 64], F32)
                WmT8 = kb.sb(ph, "w_WmT8", [P, 8, P], F32)
                T8 = kb.sb(ph, "w_T8", [P, 8, P], F32)
                Uu = kb.sb(ph, "w_Uu", [P, 8, P], F32)
                tmp8 = kb.sb(ph, "w_tmp8", [P, 8, P], F32)
                Yo = kb.sb(ph, "w_Yo", [P, 16, 64], F32)
                ofr = kb.sb(ph, "w_ofr", [P, 16, 64], F32)
                bon = kb.sb(ph, "w_bon", [P, 8, P], F32)
                gg = kb.sb(ph, "w_gg", [P, 8, P], F32)
                st16 = kb.sb(ph, "w_st16", [P, 4, 16], F32)
                ob8 = [kb.sb(ph, "w_ob%d" % i, [P, 8, P], BF16) for i in range(2)]
                b8 = lambda t_: [PS[i] for i in t_]
                pv = lambda b_, nb: PSA[:, b_:b_ + nb, :].rearrange("p b (h t) -> p (b h) t", t=P)
                for d in range(2):
                    LINCd, LSTRd, LSTRt = mk["LINC%d" % d], mk["LSTR%d" % d], mk["LSTR%d" % (1 - d)]
                    kb.op("dve", lambda e: e.memset(T8[:], 0.0), writes=[T8])
                    for ui, u in enumerate(unit_order(d)):
                        t0 = u * P
                        for nm in ldn:
                            srcT = RW[nm] if nm in ("R", "V", "KK") else RW["%s%d" % (nm, d)]
                            kb.dma("sp", ld[nm][:], srcT[:, t0:t0 + P].rearrange("(c p) t -> p c t", p=P), reads=[srcT], writes=[ld[nm]])
                        if d == 1:
                            kb.dma("sp", ofr[:], OFR[t0:t0 + P, :].rearrange("t (h v) -> t h v", h=16), reads=[OFR], writes=[ofr])
                            kb.dma("sp", bon[:], RW["BON"][:, t0:t0 + P].rearrange("(c p) t -> p c t", p=P), reads=[RW["BON"]], writes=[bon])
                            kb.dma("sp", gg[:], RW["G"][:, t0:t0 + P].rearrange("(c p) t -> p c t", p=P), reads=[RW["G"]], writes=[gg])
                        for c in range(8):
                            kb.op("pe", lambda e, c=c: e.transpose(PS[c // 4][:, (c % 4) * P:(c % 4 + 1) * P], ld["LW"][:, c, :], ident_f[:]), reads=[ld["LW"], ident_f],
                                  writes=[PS[c // 4]] if c % 4 == 0 else (), pwrites=() if c % 4 == 0 else [PS[c // 4]])
                            kb.op("pe", lambda e, c=c: e.transpose(PS[2 + c // 4][:, (c % 4) * P:(c % 4 + 1) * P], ld["V"][:, c, :], ident_f[:]), reads=[ld["V"], ident_f],
                                  writes=[PS[2 + c // 4]] if c % 4 == 0 else (), pwrites=() if c % 4 == 0 else [PS[2 + c // 4]])
                        kb.op("act", lambda e: e.activation(out=lwtm[:], in_=pv(0, 2), func=AF.Copy), reads=b8((0, 1)), writes=[lwtm])
                        kb.op("act", lambda e: e.activation(out=vtm[:], in_=pv(2, 2), func=AF.Copy), reads=b8((2, 3)), writes=[vtm])
                        cut(1)
                        for g4 in range(2):
                            cs = slice(4 * g4, 4 * g4 + 4)
                            for cc in range(4):
                                c = 4 * g4 + cc
                                kb.op("pe", lambda e, c=c, cc=cc: e.matmul(PS[4 + cc][:, 0:386], lwtm[:, c, :], CATR[d][:, :], start=True, stop=True), reads=[lwtm, CATR[d]], writes=[PS[4 + cc]])
                            pb4 = b8((4, 5, 6, 7))
                            kb.op("act", lambda e: e.activation(out=Wg[0][:], in_=PSA[:, 4:8, 0:128], func=AF.Exp), reads=pb4, writes=[Wg[0]])
                            kb.op("act", lambda e: e.activation(out=Wg[1][:], in_=PSA[:, 4:8, 0:128], func=AF.Exp, scale=-1.0), reads=pb4, writes=[Wg[1]])
                            kb.op("act", lambda e: e.activation(out=Wg[2][:], in_=PSA[:, 4:8, 128:256], func=AF.Exp), reads=pb4, writes=[Wg[2]])
                            kb.op("act", lambda e: e.activation(out=Wg[3][:], in_=PSA[:, 4:8, 256:384], func=AF.Exp), reads=pb4, writes=[Wg[3]])
                            kb.op("act", lambda e, cs=cs: e.activation(out=dec8[:, cs, :], in_=PSA[:, 4:8, 384:386], func=AF.Exp), reads=pb4, pwrites=[dec8])
                            kb.op("dve", lambda e, cs=cs: e.tensor_tensor(AR[:, cs, 0:128], ld["KK"][:, cs, :], Wg[2][:], ALU.mult), reads=[ld["KK"], Wg[2]], pwrites=[AR])
                            kb.op("dve", lambda e, cs=cs: e.tensor_tensor(AR[:, cs, 128:256], ld["R"][:, cs, :], Wg[0][:], ALU.mult), reads=[ld["R"], Wg[0]], pwrites=[AR])
                            kb.op("pool", lambda e, cs=cs: e.tensor_tensor(BtT[:, cs, :], ld["BB"][:, cs, :], Wg[1][:], ALU.mult), reads=[ld["BB"], Wg[1]], pwrites=[BtT])
                            kb.op("pool", lambda e, cs=cs: e.tensor_tensor(KtT[:, cs, :], ld["KH"][:, cs, :], Wg[1][:], ALU.mult), reads=[ld["KH"], Wg[1]], pwrites=[KtT])
                            kb.op("dve", lambda e, cs=cs: e.tensor_tensor(BeT[:, cs, :], ld["BB"][:, cs, :], Wg[3][:], ALU.mult), reads=[ld["BB"], Wg[3]], pwrites=[BeT])
                            kb.op("pool", lambda e, cs=cs: e.tensor_tensor(KeT[:, cs, :], ld["KH"][:, cs, :], Wg[3][:], ALU.mult), reads=[ld["KH"], Wg[3]], pwrites=[KeT])
                        kb.op("dve", lambda e: e.tensor_tensor(BtT2[:], bc(BtT[:], [P, 8, 2, P], [2]), bc(bdc2[:], [P, 8, 2, P], [1, 3]), ALU.mult), reads=[BtT, bdc2], writes=[BtT2])
                        kb.op("pool", lambda e: e.tensor_tensor(KtT2[:], bc(KtT[:], [P, 8, 2, P], [2]), bc(bdc2[:], [P, 8, 2, P], [1, 3]), ALU.mult), reads=[KtT, bdc2], writes=[KtT2])
                        cut(2)
                        for srcT_, dst in ((AR, Atm), (BeT, Betm), (KeT, Ketm)):
                            for c in range(8):
                                in_ap = srcT_[:, c, 0:128] if srcT_ is AR else srcT_[:, c, :]
                                kb.op("pe", lambda e, c=c, in_ap=in_ap: e.transpose(PS[c // 4][:, (c % 4) * P:(c % 4 + 1) * P], in_ap, ident_f[:]), reads=[srcT_, ident_f],
                                      writes=[PS[c // 4]] if c % 4 == 0 else (), pwrites=() if c % 4 == 0 else [PS[c // 4]])
                            kb.op("act", lambda e, dst=dst: e.activation(out=dst[:], in_=pv(0, 2), func=AF.Copy), reads=b8((0, 1)), writes=[dst])
                        cut(3)
                        for qd in range(4):
                            z = zq[qd % 2]
                            for hh in range(4):
                                h = 4 * qd + hh
                                c, two = h // 2, h % 2
                                kb.op("pe", lambda e, c=c, two=two, hh=hh: e.matmul(PS[0][:, hh * P:(hh + 1) * P], AR[:, c, 0:128], BtT2[:, c, two, :], start=True, stop=True), reads=[AR, BtT2],
                                      writes=[PS[0]] if hh == 0 else (), pwrites=() if hh == 0 else [PS[0]])
                                pbb = PS[1 + hh // 2]
                                for x2 in range(2):
                                    kb.op("pe", lambda e, c=c, two=two, hh=hh, pbb=pbb, x2=x2: e.matmul(pbb[:, (hh % 2) * 256 + x2 * P:(hh % 2) * 256 + (x2 + 1) * P], BtT2[:, c, two, :], AR[:, c, x2 * P:(x2 + 1) * P], start=True, stop=True), reads=[AR, BtT2],
                                          writes=[pbb] if (hh % 2 == 0 and x2 == 0) else (), pwrites=() if (hh % 2 == 0 and x2 == 0) else [pbb])
                                pkk = PS[3 + hh // 2]
                                for x2 in range(2):
                                    kb.op("pe", lambda e, c=c, two=two, hh=hh, pkk=pkk, x2=x2: e.matmul(pkk[:, (hh % 2) * 256 + x2 * P:(hh % 2) * 256 + (x2 + 1) * P], KtT2[:, c, two, :], AR[:, c, x2 * P:(x2 + 1) * P], start=True, stop=True), reads=[AR, KtT2],
                                          writes=[pkk] if (hh % 2 == 0 and x2 == 0) else (), pwrites=() if (hh % 2 == 0 and x2 == 0) else [pkk])
                                h = 4 * qd + hh
                                c, hs = h // 2, slice(64 * (h % 2), 64 * (h % 2) + 64)
                                kb.op("pe", lambda e, c=c, hs=hs, hh=hh: e.matmul(PS[0][:, hh * P:(hh + 1) * P], AR[hs, c, 0:128], BtT[hs, c, :], start=True, stop=True), reads=[AR, BtT],
                                      writes=[PS[0]] if hh == 0 else (), pwrites=() if hh == 0 else [PS[0]])
                                pbb = PS[1 + hh // 2]
                                kb.op("pe", lambda e, c=c, hs=hs, hh=hh, pbb=pbb: e.matmul(pbb[:, (hh % 2) * 256:(hh % 2 + 1) * 256], BtT[hs, c, :], AR[hs, c, :], start=True, stop=True), reads=[AR, BtT],
                                      writes=[pbb] if hh % 2 == 0 else (), pwrites=() if hh % 2 == 0 else [pbb])
                                pkk = PS[3 + hh // 2]
                                kb.op("pe", lambda e, c=c, hs=hs, hh=hh, pkk=pkk: e.matmul(pkk[:, (hh % 2) * 256:(hh % 2 + 1) * 256], KtT[hs, c, :], AR[hs, c, :], start=True, stop=True), reads=[AR, KtT],
                                      writes=[pkk] if hh % 2 == 0 else (), pwrites=() if hh % 2 == 0 else [pkk])
                            kb.fence_pe()
                            pB = PSA[:, 1:3, :].rearrange("p b (h x) -> p (b h) x", x=256)
                            cut(41)
                            pK = PSA[:, 3:5, :].rearrange("p b (h x) -> p (b h) x", x=256)
                            kb.op("dve", lambda e, z=z: e.tensor_tensor(z[:, :, 256:384], PS[0][:, :].rearrange("p (h t) -> p h t", h=4), bc(LSTRt[:], [P, 4, P], [1]), ALU.mult), reads=[PS[0], LSTRt], writes=[z])
                            kb.op("dve", lambda e, z=z: e.tensor_tensor(z[:, :, 128:256], pB[:, :, 0:128], bc(LSTRd[:], [P, 4, P], [1]), ALU.mult), reads=b8((1, 2)) + [LSTRd], pwrites=[z])
                            kb.op("dve", lambda e, qd=qd: e.tensor_tensor(PrbT[:, 4 * qd:4 * qd + 4, :], pB[:, :, 128:256], bc(LINCd[:], [P, 4, P], [1]), ALU.mult), reads=b8((1, 2)) + [LINCd], pwrites=[PrbT])
                            kb.op("dve", lambda e: e.tensor_tensor(MakT[:], pK[:, :, 0:128], bc(LSTRd[:], [P, 4, P], [1]), ALU.mult), reads=b8((3, 4)) + [LSTRd], writes=[MakT])
                            kb.op("dve", lambda e: e.tensor_tensor(PrkT[:], pK[:, :, 128:256], bc(LINCd[:], [P, 4, P], [1]), ALU.mult), reads=b8((3, 4)) + [LINCd], writes=[PrkT])
                            kb.op("pool", lambda e, z=z: e.tensor_tensor(z[:, :, 0:128], bc(ident_f[:], [P, 4, P], [1]), z[:, :, 128:256], ALU.subtract), reads=[ident_f, z], pwrites=[z])
                            cut(42)
                            for hh in range(4):
                                h = 4 * qd + hh
                                kb.op("pe", lambda e, h=h, hh=hh: e.matmul(PS[5][:, hh * 64:(hh + 1) * 64], MakT[:, hh, :], vtm[:, h // 2, 64 * (h % 2):64 * (h % 2) + 64], start=True, stop=True), reads=[MakT, vtm],
                                      writes=[PS[5]] if hh == 0 else (), pwrites=() if hh == 0 else [PS[5]])
                                kb.op("pe", lambda e, h=h, hh=hh: e.matmul(PS[5][:, 256 + hh * 64:256 + (hh + 1) * 64], PrkT[:, hh, :], vtm[:, h // 2, 64 * (h % 2):64 * (h % 2) + 64], start=True, stop=True), reads=[PrkT, vtm],
                                      pwrites=[PS[5]])
                            cut(43)
                            if True:
                                kb.op("dve", lambda e, qd=qd: e.tensor_scalar(nmv[:, 4 * qd:4 * qd + 4, :], PS[5][:, 0:256].rearrange("p (h v) -> p h v", h=4), -1.0, None, ALU.mult), reads=[PS[5]], pwrites=[nmv])
                            if True:
                                kb.op("act", lambda e, qd=qd: e.activation(out=O0[:, 4 * qd:4 * qd + 4, :], in_=PS[5][:, 256:512].rearrange("p (h v) -> p h v", h=4), func=AF.Copy), reads=[PS[5]], pwrites=[O0])
                            cut(4)
                            if qd % 2 == 1:
                                emit_solve(zq, [0, 4])
                                for i2 in range(2):
                                    q2 = qd - 1 + i2
                                    kb.op("pool", lambda e, i2=i2, q2=q2: e.tensor_copy(Y16[:, 4 * q2:4 * q2 + 4, :], zq[i2][:, :, 0:128]), reads=[zq[i2]], pwrites=[Y16])
                                cut(5)
                        for h in range(16):
                            kb.op("pe", lambda e, h=h: e.matmul(PS[h // 8][:, (h % 8) * 64:(h % 8 + 1) * 64], Y16[:, h, :], nmv[:, h, :], start=True, stop=True), reads=[Y16, nmv],
                                  writes=[PS[h // 8]] if h % 8 == 0 else (), pwrites=() if h % 8 == 0 else [PS[h // 8]])
                            kb.op("pe", lambda e, h=h: e.matmul(PS[2 + h // 4][:, (h % 4) * P:(h % 4 + 1) * P], Atm[:, h // 2, :], Y16[:, h, :], start=True, stop=True), reads=[Y16, Atm],
                                  writes=[PS[2 + h // 4]] if h % 4 == 0 else (), pwrites=() if h % 4 == 0 else [PS[2 + h // 4]])
                        kb.op("act", lambda e: e.activation(out=U0[:], in_=PSA[:, 0:2, :].rearrange("p b (h v) -> p (b h) v", v=64), func=AF.Copy), reads=b8((0, 1)), writes=[U0])
                        wsrc = PSA[:, 2:6, :].rearrange("p b (h t) -> p (b h) t", t=P)
                        wsrc2 = wsrc.rearrange("p (c two) t -> p c two t", two=2)
                        kb.op("dve", lambda e: e.tensor_copy(WmT8[0:64, :, :], wsrc2[0:64, :, 0, :]), reads=b8((2, 3, 4, 5)), writes=[WmT8])
                        kb.op("dve", lambda e: e.tensor_copy(WmT8[64:128, :, :], wsrc2[64:128, :, 1, :]), reads=b8((2, 3, 4, 5)), pwrites=[WmT8])
                        cut(6)
                        U0p = U0[:].rearrange("p (c two) v -> p c (two v)", two=2)
                        O0p = O0[:].rearrange("p (c two) v -> p c (two v)", two=2)
                        Yop = Yo[:].rearrange("p (c two) v -> p c (two v)", two=2)
                        for s in ((0, 1) if d == 0 else (1, 0)):
                            rs = slice(64 * s, 64 * s + 64)
                            kb.fence_pe()
                            for c in range(8):
                                kb.op("pe", lambda e, c=c: e.matmul(PS[c // 4][:, (c % 4) * P:(c % 4 + 1) * P], WmT8[:, c, :], T8[:, c, :], start=True, stop=True), reads=[WmT8, T8],
                                      writes=[PS[c // 4]] if c % 4 == 0 else (), pwrites=() if c % 4 == 0 else [PS[c // 4]])
                            kb.op("dve", lambda e, rs=rs: e.tensor_tensor(Uu[rs, :, :], U0p[rs, :, :], PSA[rs, 0:2, :].rearrange("p b (h t) -> p (b h) t", t=P), ALU.subtract), reads=[U0] + b8((0, 1)), writes=[Uu])
                            for c in range(8):
                                po = PS[2 + c // 4]
                                for two in range(2):
                                    h = 2 * c + two
                                    first = (c % 4 == 0 and two == 0)
                                    kb.op("pe", lambda e, c=c, po=po, two=two: e.matmul(po[:, (c % 4) * P + two * 64:(c % 4) * P + two * 64 + 64], AR[:, c, 128:256], T8[:, c, two * 64:two * 64 + 64], start=True, stop=False), reads=[AR, T8],
                                          writes=[po] if first else (), pwrites=() if first else [po])
                                    kb.op("pe", lambda e, c=c, po=po, h=h, two=two, rs=rs: e.matmul(po[:, (c % 4) * P + two * 64:(c % 4) * P + two * 64 + 64], PrbT[rs, h, :], Uu[rs, c, two * 64:two * 64 + 64], start=False, stop=True),
                                          reads=[PrbT, Uu], pwrites=[po])
                            kb.op("dve", lambda e, rs=rs: e.tensor_tensor(Yop[rs, :, :], O0p[rs, :, :], PSA[rs, 2:4, :].rearrange("p b (h t) -> p (b h) t", t=P), ALU.add), reads=[O0] + b8((2, 3)), pwrites=[Yo])
                            for c in range(8):
                                pt_ = PS[4 + c // 4]
                                kb.op("pe", lambda e, c=c, pt_=pt_, rs=rs: e.matmul(pt_[:, (c % 4) * P:(c % 4 + 1) * P], Betm[rs, c, :], Uu[rs, c, :], start=True, stop=False), reads=[Betm, Uu],
                                      writes=[pt_] if c % 4 == 0 else (), pwrites=() if c % 4 == 0 else [pt_])
                                kb.op("pe", lambda e, c=c, pt_=pt_, rs=rs: e.matmul(pt_[:, (c % 4) * P:(c % 4 + 1) * P], Ketm[rs, c, :], vtm[rs, c, :], start=False, stop=True), reads=[Ketm, vtm], pwrites=[pt_])
                            kb.op("dve", lambda e: e.tensor_tensor(tmp8[:], pv(4, 2), bc(mk["BD"][:], [P, 8, P], [1]), ALU.mult), reads=b8((4, 5)) + [mk["BD"]], writes=[tmp8])
                            kb.op("pool", lambda e, s=s: e.tensor_tensor(T8[:], T8[:], bc(dec8[:, :, s], [P, 8, P], [2]), ALU.mult), reads=[T8, dec8], writes=[T8])
                            kb.op("dve", lambda e: e.tensor_tensor(T8[:], T8[:], tmp8[:], ALU.add), reads=[T8, tmp8], writes=[T8])
                        if d == 0:
                            kb.dma("sp", OFR[t0:t0 + P, :].rearrange("t (h v) -> t h v", h=16), Yo[:], reads=[Yo], pwrites=[OFR])
                        else:
                            kb.op("dve", lambda e: e.tensor_tensor(Yo[:], Yo[:], ofr[:], ALU.add), reads=[Yo, ofr], writes=[Yo])
                            kb.op("dve", lambda e: e.tensor_reduce(out=st16[:, 0, :], in_=Yo[:], axis=mybir.AxisListType.X, op=ALU.add), reads=[Yo], writes=[st16])
                            kb.op("pool", lambda e: e.tensor_tensor(ofr[:], Yo[:], Yo[:], ALU.mult), reads=[Yo], writes=[ofr])
                            kb.op("dve", lambda e: e.tensor_reduce(out=st16[:, 1, :], in_=ofr[:], axis=mybir.AxisListType.X, op=ALU.add), reads=[ofr], pwrites=[st16])
                            kb.op("dve", lambda e: e.tensor_scalar(st16[:, 0, :], st16[:, 0, :], 1.0 / 64.0, None, ALU.mult), reads=[st16], pwrites=[st16])
                            kb.op("dve", lambda e: e.tensor_tensor(st16[:, 2, :], st16[:, 0, :], st16[:, 0, :], ALU.mult), reads=[st16], pwrites=[st16])
                            kb.op("dve", lambda e: e.scalar_tensor_tensor(out=st16[:, 1, :], in0=st16[:, 1, :], scalar=1.0 / 64.0, in1=st16[:, 2, :], op0=ALU.mult, op1=ALU.subtract), reads=[st16], pwrites=[st16])
                            kb.op("dve", lambda e: e.tensor_scalar(st16[:, 1, :], st16[:, 1, :], 64e-5, None, ALU.add), reads=[st16], pwrites=[st16])
                            kb.op("act", lambda e: e.activation(out=st16[:, 1, :], in_=st16[:, 1, :], func=AF.Sqrt), reads=[st16], pwrites=[st16])
                            kb.op("dve", lambda e: e.reciprocal(st16[:, 1, :], st16[:, 1, :]), reads=[st16], pwrites=[st16])
                            kb.op("dve", lambda e: e.tensor_tensor(Yo[:], Yo[:], bc(st16[:, 0, :], [P, 16, 64], [2]), ALU.subtract), reads=[Yo, st16], writes=[Yo])
                            kb.op("dve", lambda e: e.tensor_tensor(Yo[:], Yo[:], bc(st16[:, 1, :], [P, 16, 64], [2]), ALU.mult), reads=[Yo, st16], writes=[Yo])
                            for c in range(8):
                                kb.op("pe", lambda e, c=c: e.transpose(PS[c // 4][:, (c % 4) * P:(c % 4 + 1) * P], Yop[:, c, :], ident_f[:]), reads=[Yo, ident_f],
                                      writes=[PS[c // 4]] if c % 4 == 0 else (), pwrites=() if c % 4 == 0 else [PS[c // 4]])
                            kb.op("dve", lambda e: e.tensor_tensor(tmp8[:], pv(0, 2), bc(lnp[:, 0, :], [P, 8, P], [2]), ALU.mult), reads=b8((0, 1)) + [lnp], writes=[tmp8])
                            kb.op("pool", lambda e: e.tensor_tensor(tmp8[:], tmp8[:], bc(lnp[:, 1, :], [P, 8, P], [2]), ALU.add), reads=[tmp8, lnp], writes=[tmp8])
                            kb.op("pool", lambda e: e.tensor_tensor(tmp8[:], tmp8[:], bon[:], ALU.add), reads=[tmp8, bon], writes=[tmp8])
                            ob_ = ob8[ui % 2]
                            kb.op("dve", lambda e, ob_=ob_: e.tensor_tensor(ob_[:], tmp8[:], gg[:], ALU.mult), reads=[tmp8, gg], writes=[ob_])
                            kb.dma("sp", OT[1024:2048, t0:t0 + P].rearrange("(c p) t -> p c t", p=P), ob_[:], reads=[ob_], pwrites=[OT])
               except StopIteration:
                pass
                kb.barrier()

        if "merge" in phases:
            with ExitStack() as ph:
                otb = [kb.sb(ph, "otb%d" % i, [P, 24, 512], BF16) for i in range(2)]
                wbr = [kb.sb(ph, "wbr%d" % i, [P, 24, 512], BF16) for i in range(2)]
                gp = [kb.sb(ph, "gp%d" % i, [P, 512], F32) for i in range(3)]
                sg = [kb.sb(ph, "sg%d" % i, [P, 512], F32) for i in range(3)]
                acc = [kb.sb(ph, "acc%d" % i, [P, 512], F32) for i in range(2)]
                tmp = [kb.sb(ph, "tmpm%d" % i, [P, 512], F32) for i in range(2)]
                accb = [kb.sb(ph, "accb%d" % i, [P, 512], BF16) for i in range(2)]
                bi = wi = gi = ai = pi = 0
                for t0 in range(0, n, 512):
                    tw = min(512, n - t0)
                    ot = otb[bi % 2]
                    bi += 1
                    kb.dma("sp", ot[:, :, 0:tw], OT[:, t0:t0 + tw].rearrange("(c p) t -> p c t", p=P),
                           reads=[OT], writes=[ot])
                    for fg in range(4):
                        w = wbr[wi % 2]
                        wi += 1
                        for m in range(3):
                            kb.dma("pool", w[:, m * 8:(m + 1) * 8, :],
                                   w_branch[l, m, :, fg * 512:(fg + 1) * 512].rearrange("(k p) c -> p k c", p=P),
                                   reads=[w_branch], writes=[w] if m == 0 else (), pwrites=() if m == 0 else [w])
                        for f in range(4):
                            fo = fg * 4 + f
                            a_t, a_b = acc[ai % 2], accb[ai % 2]
                            ai += 1
                            for m in range(3):
                                ps = PS[pi % 4]
                                pi += 1
                                g_t, s_t = gp[gi % 3], sg[gi % 3]
                                gi += 1
                                r0 = cfg.gate_off + m * D + fo * P
                                kb.dma("sp", g_t[:, 0:tw], PT[r0:r0 + P, t0:t0 + tw], reads=[PT], writes=[g_t])
                                kb.op("act", lambda e, g_t=g_t, s_t=s_t: e.activation(out=s_t[:, 0:tw], in_=g_t[:, 0:tw], func=AF.Sigmoid),
                                      reads=[g_t], writes=[s_t])
                                for k in range(8):
                                    kb.op("pe", lambda e, ps=ps, w=w, ot=ot, m=m, k=k, f=f: e.matmul(
                                        ps[:, 0:tw], w[:, m * 8 + k, f * P:(f + 1) * P], ot[:, m * 8 + k, 0:tw],
                                        start=(k == 0), stop=(k == 7)),
                                        reads=[w, ot], writes=[ps] if k == 0 else (), pwrites=() if k == 0 else [ps])
                                if m == 0:
                                    kb.op("dve", lambda e, a_t=a_t, s_t=s_t, ps=ps: e.tensor_tensor(a_t[:, 0:tw], s_t[:, 0:tw], ps[:, 0:tw], ALU.mult),
                                          reads=[s_t, ps], writes=[a_t])
                                else:
                                    t_t = tmp[m % 2]
                                    kb.op("dve", lambda e, t_t=t_t, s_t=s_t, ps=ps: e.tensor_tensor(t_t[:, 0:tw], s_t[:, 0:tw], ps[:, 0:tw], ALU.mult),
                                          reads=[s_t, ps], writes=[t_t])
                                    if m == 1:
                                        kb.op("pool", lambda e, a_t=a_t, t_t=t_t: e.tensor_tensor(a_t[:, 0:tw], a_t[:, 0:tw], t_t[:, 0:tw], ALU.add),
                                              reads=[a_t, t_t], writes=[a_t])
                                    else:
                                        kb.op("pool", lambda e, a_t=a_t, a_b=a_b, t_t=t_t: e.tensor_tensor(a_b[:, 0:tw], a_t[:, 0:tw], t_t[:, 0:tw], ALU.add),
                                              reads=[a_t, t_t], writes=[a_b])
                            kb.dma("sp", ACC[fo * P:(fo + 1) * P, t0:t0 + tw], a_b[:, 0:tw], reads=[a_b], pwrites=[ACC])
                kb.barrier()

        if "merge" in phases:
            xsrc = xin if l == 0 else XR
            with ExitStack() as ph:
                accs = [kb.sb(ph, "accs%d" % i, [P, 16, 512], BF16) for i in range(2)]
                wob = [kb.sb(ph, "wob%d" % i, [P, 16, 512], BF16) for i in range(2)]
                xts = [kb.sb(ph, "xts%d" % i, [P, D], F32) for i in range(4)]
                g1b = [kb.sb(ph, "g1b%d" % r, [P, D], F32) for r in range(2)]
                s2b = [kb.sb(ph, "s2b%d" % r, [P, D], F32) for r in range(2)]
                sh2b = [kb.sb(ph, "sh2b%d" % r, [P, D], F32) for r in range(2)]
                lng = kb.sb(ph, "lng", [P, D], F32)
                lnb = kb.sb(ph, "lnb", [P, D], F32)
                tm2 = [kb.sb(ph, "tm2%d" % i, [P, 512], F32) for i in range(2)]
                hh = [kb.sb(ph, "hh%d" % i, [P, D], F32) for i in range(2)]
                stt = [kb.sb(ph, "stt%d" % i, [P, 4, 6], F32) for i in range(2)]
                mvt = [kb.sb(ph, "mvt%d" % i, [P, 4], F32) for i in range(2)]
                for r in range(2):
                    kb.dma("sp", g1b[r][:], MODD[r:r + 1, 2 * D:3 * D].partition_broadcast(P), reads=[MODD], writes=[g1b[r]])
                    kb.dma("sp", s2b[r][:], MODD[r:r + 1, 4 * D:5 * D].partition_broadcast(P), reads=[MODD], writes=[s2b[r]])
                    kb.dma("sp", sh2b[r][:], MODD[r:r + 1, 3 * D:4 * D].partition_broadcast(P), reads=[MODD], writes=[sh2b[r]])
                kb.dma("sp", lng[:], ln1_g[l, :, :].partition_broadcast(P), reads=[ln1_g], writes=[lng])
                kb.dma("sp", lnb[:], ln1_b[l, :, :].partition_broadcast(P), reads=[ln1_b], writes=[lnb])
                bi = wi = pi = ti2 = hi = 0
                for t0 in range(0, n, 512):
                    tw = min(512, n - t0)
                    ntl = tw // P
                    a_s = accs[bi % 2]
                    bi += 1
                    kb.dma("sp", a_s[:, :, 0:tw], ACC[:, t0:t0 + tw].rearrange("(k p) t -> p k t", p=P), reads=[ACC], writes=[a_s])
                    for tt in range(ntl):
                        gt = t0 // P + tt
                        kb.dma("sp", xts[tt][:], xsrc[gt * P:(gt + 1) * P, :], reads=[xsrc], writes=[xts[tt]])
                    for og in range(4):
                        wo = wob[wi % 2]
                        wi += 1
                        kb.dma("pool", wo[:], w_out[l, :, og * 512:(og + 1) * 512].rearrange("(k p) c -> p k c", p=P),
                               reads=[w_out], writes=[wo])
                        for tt in range(ntl):
                            gt = t0 // P + tt
                            r = 1 if tile_is_ctx(gt) else 0
                            ps = PS[pi % 4]
                            pi += 1
                            for k in range(16):
                                kb.op("pe", lambda e, ps=ps, a_s=a_s, wo=wo, k=k, tt=tt: e.matmul(
                                    ps[:, :], a_s[:, k, tt * P:(tt + 1) * P], wo[:, k, :], start=(k == 0), stop=(k == 15)),
                                    reads=[a_s, wo], writes=[ps] if k == 0 else (), pwrites=() if k == 0 else [ps])
                            t_t = tm2[ti2 % 2]
                            ti2 += 1
                            x_t = xts[tt]
                            kb.op("dve", lambda e, t_t=t_t, ps=ps, r=r, og=og: e.tensor_tensor(t_t[:], ps[:, :], g1b[r][:, og * 512:(og + 1) * 512], ALU.mult),
                                  reads=[ps, g1b[r]], writes=[t_t])
                            kb.op("dve", lambda e, t_t=t_t, x_t=x_t, og=og: e.scalar_tensor_tensor(
                                out=x_t[:, og * 512:(og + 1) * 512], in0=x_t[:, og * 512:(og + 1) * 512], scalar=cfg_alpha,
                                in1=t_t[:], op0=ALU.mult, op1=ALU.add), reads=[t_t, x_t], writes=[x_t])
                    for tt in range(ntl):
                        gt = t0 // P + tt
                        r = 1 if tile_is_ctx(gt) else 0
                        x_t = xts[tt]
                        h_t = hh[hi % 2]
                        st_t, mv_t = stt[hi % 2], mvt[hi % 2]
                        hi += 1
                        layer_norm(kb, x_t, st_t, mv_t, lng, lnb, 1e-5)
                        kb.dma("sp", XM[gt * P:(gt + 1) * P, :], x_t[:], reads=[x_t], pwrites=[XM])
                        kb.op("pool", lambda e, h_t=h_t, x_t=x_t, r=r: e.tensor_tensor(h_t[:], x_t[:], s2b[r][:], ALU.mult),
                              reads=[x_t, s2b[r]], writes=[h_t])
                        kb.op("pool", lambda e, h_t=h_t, r=r: e.tensor_tensor(h_t[:], h_t[:], sh2b[r][:], ALU.add),
                              reads=[h_t, sh2b[r]], writes=[h_t])
                        kb.dma("sp", H2[gt * P:(gt + 1) * P, :], h_t[:], reads=[h_t], pwrites=[H2])
                kb.barrier()


        if "moe" in phases:
            NT = n // P
            B = cfg.dblk
            de = cfg.d_expert
            nfc = de // P
            with ExitStack() as ph:
                h2t = [kb.sb(ph, "h2t%d" % i, [P, D], F32) for i in range(2)]
                h2T = [kb.sb(ph, "h2T%d" % i, [P, 16, P], F32) for i in range(2)]
                rw = kb.sb(ph, "rw", [P, 16, 16], F32)
                rbb = kb.sb(ph, "rbb", [P, 16], F32)
                E12 = kb.sb(ph, "E12", [P, NT, 32], F32)
                W12 = kb.sb(ph, "W12", [P, NT, 2], F32)
                ET = kb.sb(ph, "ET", [16, n], F32)
                RK = kb.sb(ph, "RK", [16, n], F32)
                on16 = kb.sb(ph, "on16", [16, 2048], F32)
                sm = [kb.sb(ph, "sm%d" % i, [P, 160], F32) for i in range(2)]
                cst = kb.sb(ph, "cst", [16, 8], F32)
                ut16 = kb.sb(ph, "ut16", [16, 16], F32)
                bbrow = kb.sb(ph, "bbrow", [16, cfg.n_blk], F32)
                ge = kb.sb(ph, "ge", [16, cfg.n_blk], F32)
                on16p = kb.sb(ph, "on16p", [16, P], F32)
                eoff = kb.sb(ph, "eoff", [P, cfg.n_blk], F32)
                det = kb.sb(ph, "det", [P, 16], F32)
                slf = [kb.sb(ph, "slf%d" % i, [P, 20], F32) for i in range(2)]
                sli = [[kb.sb(ph, "sli%d_%d" % (i, j), [P, 1], I32) for j in range(2)] for i in range(2)]
                kb.dma("sp", rw[:], router_w[:, :].rearrange("(k p) e -> p k e", p=P), reads=[router_w], writes=[rw])
                kb.dma("sp", rbb[:], router_bias[:, :].partition_broadcast(P), reads=[router_bias], writes=[rbb])
                kb.dma("sp", ut16[:], c_ut16[:, :], reads=[c_ut16], writes=[ut16])
                kb.dma("sp", bbrow[:], c_bbrow[:, :], reads=[c_bbrow], writes=[bbrow])
                kb.op("dve", lambda e: e.memset(on16[:], 1.0), writes=[on16])
                kb.op("dve", lambda e: e.memset(on16p[:], 1.0), writes=[on16p])
                for tt in range(NT):
                    x_t, xT = h2t[tt % 2], h2T[tt % 2]
                    s = sm[tt % 2]
                    kb.dma("sp", x_t[:], H2[tt * P:(tt + 1) * P, :], reads=[H2], writes=[x_t])
                    for q4 in range(4):
                        ps = PS[4 + q4 % 2]
                        for kk in range(4):
                            k = q4 * 4 + kk
                            kb.op("pe", lambda e, ps=ps, x_t=x_t, k=k, kk=kk: e.transpose(ps[:, kk * P:(kk + 1) * P], x_t[:, k * P:(k + 1) * P], ident_f[:]),
                                  reads=[x_t, ident_f], writes=[ps] if kk == 0 else (), pwrites=() if kk == 0 else [ps])
                        kb.op("act", lambda e, ps=ps, xT=xT, q4=q4: e.activation(out=xT[:, q4 * 4:(q4 + 1) * 4, :], in_=ps[:, :].rearrange("p (k t) -> p k t", k=4), func=AF.Copy),
                              reads=[ps], writes=[xT] if q4 == 0 else (), pwrites=() if q4 == 0 else [xT])
                    pl = PS[6]
                    for k in range(16):
                        kb.op("pe", lambda e, pl=pl, xT=xT, k=k: e.matmul(pl[:, 0:16], xT[:, k, :], rw[:, k, :], start=(k == 0), stop=(k == 15)),
                              reads=[xT, rw], writes=[pl] if k == 0 else (), pwrites=() if k == 0 else [pl])
                    E1 = E12[:, tt, 0:16]
                    E2 = E12[:, tt, 16:32]
                    kb.op("act", lambda e, s=s, pl=pl: e.activation(out=s[:, 0:16], in_=pl[:, 0:16], func=AF.Sigmoid), reads=[pl], writes=[s])
                    kb.op("dve", lambda e, s=s: e.tensor_tensor(s[:, 16:32], s[:, 0:16], rbb[:], ALU.add), reads=[s, rbb], writes=[s])
                    kb.op("dve", lambda e, s=s: e.tensor_reduce(out=s[:, 64:68], in_=s[:, 16:32].rearrange("p (g e) -> p g e", g=4), axis=mybir.AxisListType.X, op=ALU.max),
                          reads=[s], writes=[s])
                    for g4 in range(4):
                        kb.op("dve", lambda e, s=s, g4=g4: e.tensor_scalar(s[:, 32 + 4 * g4:36 + 4 * g4], s[:, 16 + 4 * g4:20 + 4 * g4], s[:, 64 + g4:65 + g4], None, ALU.is_equal),
                              reads=[s], writes=[s])
                    kb.op("dve", lambda e, s=s: e.scalar_tensor_tensor(out=s[:, 48:64], in0=s[:, 32:48], scalar=-1.0e9, in1=s[:, 16:32], op0=ALU.mult, op1=ALU.add),
                          reads=[s], writes=[s])
                    kb.op("dve", lambda e, s=s: e.tensor_reduce(out=s[:, 68:72], in_=s[:, 48:64].rearrange("p (g e) -> p g e", g=4), axis=mybir.AxisListType.X, op=ALU.max),
                          reads=[s], writes=[s])
                    kb.op("dve", lambda e, s=s: e.tensor_tensor(s[:, 72:76], s[:, 64:68], s[:, 68:72], ALU.add), reads=[s], writes=[s])
                    kb.op("dve", lambda e, s=s: e.tensor_reduce(out=s[:, 76:77], in_=s[:, 72:76], axis=mybir.AxisListType.X, op=ALU.max), reads=[s], writes=[s])
                    kb.op("dve", lambda e, s=s: e.tensor_scalar(s[:, 80:84], s[:, 72:76], s[:, 76:77], None, ALU.is_ge), reads=[s], writes=[s])
                    for g4 in range(4):
                        kb.op("dve", lambda e, s=s, g4=g4, E1=E1: e.tensor_scalar(E1[:, 4 * g4:4 * g4 + 4], s[:, 32 + 4 * g4:36 + 4 * g4], s[:, 80 + g4:81 + g4], None, ALU.mult),
                              reads=[s], pwrites=[E12])
                        kb.op("dve", lambda e, s=s, g4=g4, E2=E2: e.tensor_scalar(E2[:, 4 * g4:4 * g4 + 4], s[:, 48 + 4 * g4:52 + 4 * g4], s[:, 68 + g4:69 + g4], s[:, 80 + g4:81 + g4], ALU.is_equal, ALU.mult),
                              reads=[s], pwrites=[E12])
                    kb.op("dve", lambda e, s=s, E1=E1: e.tensor_tensor(s[:, 96:112], E1, s[:, 0:16], ALU.mult), reads=[s, E12], writes=[s])
                    kb.op("dve", lambda e, s=s: e.tensor_reduce(out=s[:, 112:113], in_=s[:, 96:112], axis=mybir.AxisListType.X, op=ALU.add), reads=[s], writes=[s])
                    kb.op("dve", lambda e, s=s, E2=E2: e.tensor_tensor(s[:, 96:112], E2, s[:, 0:16], ALU.mult), reads=[s, E12], writes=[s])
                    kb.op("dve", lambda e, s=s: e.tensor_reduce(out=s[:, 113:114], in_=s[:, 96:112], axis=mybir.AxisListType.X, op=ALU.add), reads=[s], writes=[s])
                    kb.op("dve", lambda e, s=s: e.tensor_tensor(s[:, 114:115], s[:, 112:113], s[:, 113:114], ALU.add), reads=[s], writes=[s])
                    kb.op("dve", lambda e, s=s: e.reciprocal(s[:, 115:116], s[:, 114:115]), reads=[s], writes=[s])
                    kb.op("dve", lambda e, s=s, tt=tt: e.tensor_scalar(W12[:, tt, 0:2], s[:, 112:114], s[:, 115:116], None, ALU.mult), reads=[s], pwrites=[W12])
                    kb.op("dve", lambda e, s=s, E1=E1, E2=E2: e.tensor_tensor(s[:, 96:112], E1, E2, ALU.add), reads=[s, E12], writes=[s])
                    pt_ = PS[7]
                    kb.op("pe", lambda e, pt_=pt_, s=s: e.transpose(pt_[0:16, 0:P], s[:, 96:112], ident_f[:]), reads=[s, ident_f], writes=[pt_])
                    kb.op("act", lambda e, pt_=pt_, tt=tt: e.activation(out=ET[:, tt * P:(tt + 1) * P], in_=pt_[0:16, 0:P], func=AF.Copy), reads=[pt_], pwrites=[ET])
                for s0 in range(0, n, 2048):
                    sw = min(2048, n - s0)
                    init = 0.0 if s0 == 0 else RK[:, s0 - 1:s0]
                    kb.op("dve", lambda e, s0=s0, sw=sw, init=init: e.tensor_tensor_scan(out=RK[:, s0:s0 + sw], data0=on16[:, 0:sw], data1=ET[:, s0:s0 + sw], initial=init, op0=ALU.mult, op1=ALU.add),
                          reads=[on16, ET, RK], pwrites=[RK])
                kb.op("dve", lambda e: e.tensor_scalar(ge[:], bbrow[:], RK[:, n - 1:n], None, ALU.is_lt), reads=[bbrow, RK], writes=[ge])
                kb.op("dve", lambda e: e.tensor_reduce(out=cst[:, 1:2], in_=ge[:], axis=mybir.AxisListType.X, op=ALU.add), reads=[ge], writes=[cst])
                kb.op("dve", lambda e: e.tensor_scalar(cst[:, 3:4], cst[:, 1:2], float(B), None, ALU.mult), reads=[cst], writes=[cst])
                pq = PS[6]
                kb.op("pe", lambda e: e.matmul(pq[0:16, 0:1], ut16[:], cst[:, 3:4], start=True, stop=True), reads=[ut16, cst], writes=[pq])
                kb.op("dve", lambda e: e.tensor_copy(cst[:, 4:5], pq[0:16, 0:1]), reads=[pq], writes=[cst])
                kb.op("dve", lambda e: e.tensor_tensor(cst[:, 5:6], cst[:, 4:5], cst[:, 3:4], ALU.add), reads=[cst], writes=[cst])
                kb.op("dve", lambda e: e.tensor_scalar(RK[:], RK[:], cst[:, 4:5], -1.0, ALU.add, ALU.add), reads=[RK, cst], writes=[RK])
                kb.op("dve", lambda e: e.tensor_scalar(ge[:], bbrow[:], cst[:, 5:6], None, ALU.is_ge), reads=[bbrow, cst], writes=[ge])
                kb.op("pe", lambda e: e.matmul(pq[:, 0:cfg.n_blk], on16p[:], ge[:], start=True, stop=True), reads=[on16p, ge], writes=[pq])
                kb.op("dve", lambda e: e.tensor_scalar(eoff[:], pq[:, 0:cfg.n_blk], 15.0, None, ALU.min), reads=[pq], writes=[eoff])
                kb.dma("sp", EOFF[:, :], eoff[:], reads=[eoff], writes=[EOFF])
                for tt in range(NT):
                    pt_ = PS[7]
                    kb.op("pe", lambda e, pt_=pt_, tt=tt: e.transpose(pt_[:, 0:16], RK[:, tt * P:(tt + 1) * P], ident_f[0:16, 0:16]), reads=[RK, ident_f], writes=[pt_])
                    sf = slf[tt % 2]
                    kb.op("dve", lambda e, pt_=pt_: e.tensor_copy(det[:], pt_[:, 0:16]), reads=[pt_], writes=[det])
                    for j in range(2):
                        kb.op("dve", lambda e, sf=sf, j=j, tt=tt: e.tensor_tensor(sf[:, 0:16], det[:], E12[:, tt, 16 * j:16 * j + 16], ALU.mult), reads=[det, E12], writes=[sf])
                        kb.op("dve", lambda e, sf=sf, j=j: e.tensor_reduce(out=sf[:, 16 + j:17 + j], in_=sf[:, 0:16], axis=mybir.AxisListType.X, op=ALU.add), reads=[sf], writes=[sf])
                        si = sli[tt % 2][j]
                        kb.op("dve", lambda e, sf=sf, j=j, si=si: e.tensor_copy(si[:], sf[:, 16 + j:17 + j]), reads=[sf], writes=[si])
                        kb.dma("sp", SLOT[tt, j, :, :], si[:], reads=[si], pwrites=[SLOT])
                kb.dma("sp", W12D[:, :, :], W12[:], reads=[W12], writes=[W12D])
                kb.barrier()

            with ExitStack() as ph:
                h2t = [kb.sb(ph, "h2u%d" % i, [P, D], F32) for i in range(2)]
                h2b = [kb.sb(ph, "h2b%d" % i, [P, D], BF16) for i in range(2)]
                sli = [[kb.sb(ph, "slj%d_%d" % (i, j), [P, 1], I32) for j in range(2)] for i in range(2)]
                for tt in range(NT):
                    x_t, x_b = h2t[tt % 2], h2b[tt % 2]
                    kb.dma("sp", x_t[:], H2[tt * P:(tt + 1) * P, :], reads=[H2], writes=[x_t])
                    kb.op("act", lambda e, x_t=x_t, x_b=x_b: e.activation(out=x_b[:], in_=x_t[:], func=AF.Copy), reads=[x_t], writes=[x_b])
                    for j in range(2):
                        si = sli[tt % 2][j]
                        kb.dma("sp", si[:], SLOT[tt, j, :, :], reads=[SLOT], writes=[si])
                        kb.idma(XS[:, :], bass.IndirectOffsetOnAxis(ap=si[:, :], axis=0), x_b[:, :], None, cfg.n_slot,
                                reads=[x_b, si], pwrites=[XS])
                kb.barrier()

            with ExitStack() as ph:
                eo = kb.sb(ph, "eo", [P, cfg.n_blk], F32)
                eo1 = kb.sb(ph, "eo1", [P, cfg.n_blk], F32)
                eo2 = kb.sb(ph, "eo2", [P, cfg.n_blk], F32)
                idb = kb.sb(ph, "idb", [P, 32], F32)
                idf = [kb.sb(ph, "idf%d" % i, [P, 16 + 4 * nfc], F32) for i in range(2)]
                idi = [[kb.sb(ph, "idi%d_%d" % (i, j), [P, 1], I32) for j in range(16 + 4 * nfc)] for i in range(2)]
                xs = [kb.sb(ph, "xs%d" % i, [P, D], BF16) for i in range(2)]
                xT = kb.sb(ph, "xTb", [P, 16, B], BF16)
                wA = kb.sb(ph, "wA", [P, 16, de], BF16)
                wB = kb.sb(ph, "wB", [P, 16, de], BF16)
                w2t = [kb.sb(ph, "w2t%d" % i, [P, nfc, 512], BF16) for i in range(2)]
                s1T = kb.sb(ph, "s1T", [P, nfc, B], BF16)
                actT = kb.sb(ph, "actT", [P, nfc, B], BF16)
                yo = [kb.sb(ph, "yo%d" % i, [P, 512], F32) for i in range(2)]
                kb.dma("sp", eo[:], EOFF[:, :], reads=[EOFF], writes=[eo])
                kb.dma("sp", idb[:], c_idb[:, :], reads=[c_idb], writes=[idb])
                kb.op("dve", lambda e: e.tensor_scalar(eo1[:], eo[:], float(D), float(l * 16 * D), ALU.mult, ALU.add), reads=[eo], writes=[eo1])
                kb.op("dve", lambda e: e.tensor_scalar(eo2[:], eo[:], float(4 * de), float(l * 16 * de * 4), ALU.mult, ALU.add), reads=[eo], writes=[eo2])
                xi = w2i = yi = pi = 0
                for b in range(cfg.n_blk):
                    ii = b % 2
                    kb.op("dve", lambda e, b=b, ii=ii: e.tensor_scalar(idf[ii][:, 0:16], idb[:, 0:16], eo1[:, b:b + 1], None, ALU.add),
                          reads=[eo1, idb], writes=[idf[ii]])
                    for og in range(4):
                        kb.op("dve", lambda e, b=b, ii=ii, og=og: e.tensor_scalar(idf[ii][:, 16 + og * nfc:16 + (og + 1) * nfc], idb[:, 0:nfc], 4.0, eo2[:, b:b + 1], ALU.mult, ALU.add),
                              reads=[eo2, idb], pwrites=[idf[ii]])
                        if og:
                            kb.op("dve", lambda e, ii=ii, og=og: e.tensor_scalar(idf[ii][:, 16 + og * nfc:16 + (og + 1) * nfc], idf[ii][:, 16 + og * nfc:16 + (og + 1) * nfc], float(og), None, ALU.add),
                                  reads=[idf[ii]], pwrites=[idf[ii]])
                    for j in range(16 + 4 * nfc):
                        kb.op("dve", lambda e, ii=ii, j=j: e.tensor_copy(idi[ii][j][:], idf[ii][:, j:j + 1]), reads=[idf[ii]], writes=[idi[ii][j]])
                    for st_ in range(B // P):
                        x_s = xs[xi % 2]
                        xi += 1
                        kb.dma("sp", x_s[:], XS[b * B + st_ * P:b * B + (st_ + 1) * P, :], reads=[XS], writes=[x_s])
                        for q4 in range(4):
                            ps = PS[4 + q4 % 2]
                            for kk in range(4):
                                k = q4 * 4 + kk
                                o = ps[:].bitcast(BF16)[:, kk * P:(kk + 1) * P]
                                kb.op("pe", lambda e, o=o, x_s=x_s, k=k: e.transpose(o, x_s[:, k * P:(k + 1) * P], ident_b[:]),
                                      reads=[x_s, ident_b], writes=[ps] if kk == 0 else (), pwrites=() if kk == 0 else [ps])
                            src_ = ps[:].bitcast(BF16)[:, 0:4 * P].rearrange("p (k t) -> p k t", k=4)
                            kb.op("act", lambda e, src_=src_, q4=q4, st_=st_: e.activation(out=xT[:, q4 * 4:(q4 + 1) * 4, st_ * P:(st_ + 1) * P], in_=src_, func=AF.Copy),
                                  reads=[ps], pwrites=[xT])
                    for wsel, (w, wt_) in enumerate(((wA, moe_w1), (wB, moe_w3))):
                        for k in range(16):
                            kb.idma(w[:, k, :], None, wt_[:, :], bass.IndirectOffsetOnAxis(ap=idi[ii][k][:, :], axis=0), L * 16 * D,
                                    reads=[wt_, idi[ii][k]], writes=[w] if k == 0 else (), pwrites=() if k == 0 else [w])
                        for fc in range(nfc):
                            ps = PS[pi % 4]
                            pi += 1
                            for k in range(16):
                                kb.op("pe", lambda e, ps=ps, w=w, k=k, fc=fc: e.matmul(ps[:, 0:B], w[:, k, fc * P:(fc + 1) * P], xT[:, k, :], start=(k == 0), stop=(k == 15)),
                                      reads=[w, xT], writes=[ps] if k == 0 else (), pwrites=() if k == 0 else [ps])
                            if wsel == 0:
                                kb.op("act", lambda e, ps=ps, fc=fc: e.activation(out=s1T[:, fc, :], in_=ps[:, 0:B], func=AF.Silu), reads=[ps], pwrites=[s1T])
                            else:
                                kb.op("dve", lambda e, ps=ps, fc=fc: e.tensor_tensor(actT[:, fc, :], s1T[:, fc, :], ps[:, 0:B], ALU.mult), reads=[s1T, ps], pwrites=[actT])
                    for og in range(4):
                        w2_ = w2t[w2i % 2]
                        w2i += 1
                        for fc in range(nfc):
                            ix = idi[ii][16 + og * nfc + fc]
                            kb.idma(w2_[:, fc, :], None, moe_w2[:, :], bass.IndirectOffsetOnAxis(ap=ix[:, :], axis=0), L * 16 * de * 4,
                                    reads=[moe_w2, ix], writes=[w2_] if fc == 0 else (), pwrites=() if fc == 0 else [w2_])
                        for st_ in range(B // P):
                            ps = PS[6 + pi % 2]
                            pi += 1
                            for fc in range(nfc):
                                kb.op("pe", lambda e, ps=ps, w2_=w2_, fc=fc, st_=st_: e.matmul(ps[:, :], actT[:, fc, st_ * P:(st_ + 1) * P], w2_[:, fc, :], start=(fc == 0), stop=(fc == nfc - 1)),
                                      reads=[actT, w2_], writes=[ps] if fc == 0 else (), pwrites=() if fc == 0 else [ps])
                            y_t = yo[yi % 2]
                            yi += 1
                            kb.op("act", lambda e, y_t=y_t, ps=ps: e.activation(out=y_t[:], in_=ps[:, :], func=AF.Copy), reads=[ps], writes=[y_t])
                            kb.dma("sp", YS[b * B + st_ * P:b * B + (st_ + 1) * P, og * 512:(og + 1) * 512], y_t[:], reads=[y_t], pwrites=[YS])
                kb.barrier()

            with ExitStack() as ph:
                y1 = [kb.sb(ph, "y1_%d" % i, [P, D], F32) for i in range(2)]
                y2 = [kb.sb(ph, "y2_%d" % i, [P, D], F32) for i in range(2)]
                xm = [kb.sb(ph, "xm_%d" % i, [P, D], F32) for i in range(2)]
                g2b = [kb.sb(ph, "g2b%d" % r, [P, D], F32) for r in range(2)]
                lng = kb.sb(ph, "lng2", [P, D], F32)
                lnb = kb.sb(ph, "lnb2", [P, D], F32)
                w12 = kb.sb(ph, "w12", [P, NT, 2], F32)
                sli = [[kb.sb(ph, "slk%d_%d" % (i, j), [P, 1], I32) for j in range(2)] for i in range(2)]
                stt = [kb.sb(ph, "stu%d" % i, [P, 4, 6], F32) for i in range(2)]
                mvt = [kb.sb(ph, "mvu%d" % i, [P, 4], F32) for i in range(2)]
                for r in range(2):
                    kb.dma("sp", g2b[r][:], MODD[r:r + 1, 5 * D:6 * D].partition_broadcast(P), reads=[MODD], writes=[g2b[r]])
                kb.dma("sp", lng[:], ln2_g[l, :, :].partition_broadcast(P), reads=[ln2_g], writes=[lng])
                kb.dma("sp", lnb[:], ln2_b[l, :, :].partition_broadcast(P), reads=[ln2_b], writes=[lnb])
                kb.dma("sp", w12[:], W12D[:, :, :], reads=[W12D], writes=[w12])
                for tt in range(NT):
                    r = 1 if tile_is_ctx(tt) else 0
                    a1, a2, x_m = y1[tt % 2], y2[tt % 2], xm[tt % 2]
                    for j, dst in ((0, a1), (1, a2)):
                        si = sli[tt % 2][j]
                        kb.dma("sp", si[:], SLOT[tt, j, :, :], reads=[SLOT], writes=[si])
                        kb.idma(dst[:, :], None, YS[:, :], bass.IndirectOffsetOnAxis(ap=si[:, :], axis=0), cfg.n_slot, reads=[YS, si], writes=[dst])
                    kb.dma("sp", x_m[:], XM[tt * P:(tt + 1) * P, :], reads=[XM], writes=[x_m])
                    kb.op("dve", lambda e, a1=a1, tt=tt: e.tensor_scalar(a1[:], a1[:], w12[:, tt, 0:1], None, ALU.mult), reads=[a1, w12], writes=[a1])
                    kb.op("dve", lambda e, a1=a1, a2=a2, tt=tt: e.scalar_tensor_tensor(out=a1[:], in0=a2[:], scalar=w12[:, tt, 1:2], in1=a1[:], op0=ALU.mult, op1=ALU.add),
                          reads=[a1, a2, w12], writes=[a1])
                    kb.op("pool", lambda e, a1=a1, r=r: e.tensor_tensor(a1[:], a1[:], g2b[r][:], ALU.mult), reads=[a1, g2b[r]], writes=[a1])
                    kb.op("dve", lambda e, a1=a1, x_m=x_m: e.scalar_tensor_tensor(out=x_m[:], in0=x_m[:], scalar=cfg_alpha, in1=a1[:], op0=ALU.mult, op1=ALU.add),
                          reads=[a1, x_m], writes=[x_m])
                    layer_norm(kb, x_m, stt[tt % 2], mvt[tt % 2], lng, lnb, 1e-5)
                    if l == L - 1 and not per_layer:
                        if tt * P >= cfg.n_ctx:
                            kb.dma("sp", yout[tt * P - cfg.n_ctx:(tt + 1) * P - cfg.n_ctx, :], x_m[:], reads=[x_m], pwrites=[yout])
                    else:
                        kb.dma("sp", XR[tt * P:(tt + 1) * P, :], x_m[:], reads=[x_m], pwrites=[XR])
                kb.barrier()

    kb.finish()
    es.close()
    return nc, kb


ALL_PHASES = ("mod", "inproj", "gla", "gdn", "rwkv", "merge", "moe")
_PROG = {}


def _layer_inputs(inp, l, consts):
    f32 = lambda a: np.ascontiguousarray(np.asarray(a, dtype=np.float32))
    D = 2048
    d = {}
    d["w_ada"] = f32(inp["w_ada"][l:l + 1])
    d["b_ada"] = f32(inp["b_ada"][l:l + 1]).reshape(1, 1, 6 * D)
    d["w_in"] = f32(inp["w_in"][l:l + 1])
    b_pad = np.zeros((132 * P,), np.float32)
    b_pad[:inp["b_in"].shape[1]] = np.asarray(inp["b_in"][l], np.float32)
    d["b_inT"] = np.ascontiguousarray(b_pad.reshape(1, 132, P).transpose(0, 2, 1))
    d["gla_dec_w"] = f32(inp["gla_dec_w"][l:l + 1])
    d["gla_dec_b"] = f32(inp["gla_dec_b"][l:l + 1]).reshape(1, 2, 1, 512)
    d["gla_norm_wT"] = np.ascontiguousarray(f32(inp["gla_norm_w"][l:l + 1]).reshape(1, 2, P).transpose(0, 2, 1))
    prm = dict(mu=inp["rwkv_mu"][l:l + 1], w2=inp["rwkv_w2"][l:l + 1], w0=inp["rwkv_w0"][l:l + 1], a2=inp["rwkv_a2"][l:l + 1],
               a0=inp["rwkv_a0"][l:l + 1], g2=inp["rwkv_g2"][l:l + 1], kk=inp["rwkv_kk"][l:l + 1], ka=inp["rwkv_ka"][l:l + 1],
               rk=inp["rwkv_rk"][l:l + 1], ln_w=inp["rwkv_ln_w"][l:l + 1], ln_b=inp["rwkv_ln_b"][l:l + 1])
    prm = {k: f32(v) for k, v in prm.items()}
    d.update(rwkv_layout(prm))
    d["gdn_conv_wT"] = np.ascontiguousarray(f32(inp["gdn_conv_w"][l:l + 1]).reshape(1, 9, 24, P).transpose(0, 2, 3, 1))
    d["gdn_a_log"] = f32(inp["gdn_a_log"][l:l + 1]).reshape(1, 1, 16)
    d["gdn_dt_bias"] = f32(inp["gdn_dt_bias"][l:l + 1]).reshape(1, 1, 16)
    d["gdn_norm_wT"] = f32(inp["gdn_norm_w"][l:l + 1]).reshape(1, P, 1)
    d["w_branch"] = f32(inp["w_branch"][l:l + 1])
    d["w_out"] = f32(inp["w_out"][l:l + 1])
    for k in ("ln1_g", "ln1_b", "ln2_g", "ln2_b"):
        d[k] = f32(inp[k][l:l + 1]).reshape(1, 1, D)
    d["router_w"] = f32(inp["router_w"])
    d["router_bias"] = f32(inp["router_bias"]).reshape(1, 16)
    de = inp["moe_w1"].shape[-1]
    d["moe_w1"] = f32(inp["moe_w1"][l]).reshape(16 * D, de)
    d["moe_w3"] = f32(inp["moe_w3"][l]).reshape(16 * D, de)
    d["moe_w2"] = f32(inp["moe_w2"][l]).reshape(16 * de * 4, 512)
    d.update(consts)
    return d


def kernel(**inputs):
    x = np.asarray(inputs["x"], np.float32)
    ctx = np.asarray(inputs["ctx"], np.float32)
    n_lat, n_ctx = x.shape[1], ctx.shape[1]
    depth = inputs["w_ada"].shape[0]
    de = inputs["moe_w1"].shape[-1]
    key = (n_ctx, n_lat, de)
    if key not in _PROG:
        cfg = Cfg(n_ctx=n_ctx, n_lat=n_lat, depth=1, d_expert=de)
        cfg.per_layer = True
        nc, kb = build_program(cfg, phases=ALL_PHASES, dbg=("XR",))
        _PROG[key] = (cfg, nc)
    cfg, nc = _PROG[key]
    consts = make_consts(cfg)
    X = np.ascontiguousarray(np.concatenate([ctx[0], x[0]], axis=0))
    ccT = np.ascontiguousarray(np.stack([np.asarray(inputs["c"], np.float32)[0], np.asarray(inputs["c_ctx"], np.float32)], axis=1))
    for l in range(depth):
        ins = _layer_inputs(inputs, l, consts)
        ins["xin"] = X
        ins["ccT"] = ccT
        res = run_bass_kernel_spmd(nc, [ins], core_ids=[0])
        X = np.ascontiguousarray(np.asarray(res.results[0]["XR"], np.float32))
    return X[n_ctx:].reshape(1, n_lat, -1).astype(np.float32)


def make_consts(cfg):
    c = {}
    c["identD"] = np.eye(P, dtype=np.float32)
    c["c_ut16"] = np.triu(np.ones((16, 16), np.float32), 1)
    c["c_bbrow"] = np.tile((np.arange(cfg.n_blk, dtype=np.float32) * cfg.dblk)[None, :], (16, 1))
    c["c_idb"] = (np.arange(32, dtype=np.float32)[None, :] * P + np.arange(P, dtype=np.float32)[:, None]).astype(np.float32)
    idx = np.arange(P)
    sub = idx // 64
    BD = (sub[:, None] == sub[None, :]).astype(np.float32)
    m = {"BD": BD}
    for d in range(2):
        le = (idx[:, None] <= idx[None, :]) if d == 0 else (idx[:, None] >= idx[None, :])
        lt = (idx[:, None] < idx[None, :]) if d == 0 else (idx[:, None] > idx[None, :])
        LINC = BD * le
        LSTR = BD * lt
        mid = sub * 64 + (31 if d == 0 else 32)
        m["LINC%d" % d] = LINC
        m["LSTR%d" % d] = LSTR
        m["MREL%d" % d] = LINC - LINC[:, mid]
        m["MEND%d" % d] = BD - LINC
        m["NEG%d" % d] = (LSTR - 1.0) * 3.0e4
        m["NEGI%d" % d] = (LINC - 1.0) * 3.0e4
    c["c_masks"] = np.stack([m[k] for k in MASKS]).astype(np.float32)
    c["c_bdcol"] = np.stack([(sub == 0), (sub == 1)], 1).astype(np.float32)
    return c


def rwkv_layout(prm):
    L = prm["mu"].shape[0]
    t8 = lambda a: np.ascontiguousarray(a.reshape(L, 8, P).transpose(0, 2, 1))
    out = {}
    out["rwkv_muT"] = np.ascontiguousarray(prm["mu"].reshape(L, 27, P).transpose(0, 2, 1))
    out["rwkv_w2"] = np.ascontiguousarray(prm["w2"])
    out["rwkv_a2"] = np.ascontiguousarray(prm["a2"])
    out["rwkv_g2"] = np.ascontiguousarray(prm["g2"])
    z8 = np.zeros((L, P, 8), np.float32)
    out["rwkv_p4T"] = np.ascontiguousarray(np.stack([t8(prm["kk"]), t8(prm["ka"]), t8(prm["rk"].reshape(L, 1024)), z8], 2))
    w0 = np.stack([t8(prm["w0"][:, d]) for d in range(2)], 2)
    a0 = np.stack([t8(prm["a0"][:, d]) for d in range(2)], 2)
    out["rwkv_w0a0T"] = np.ascontiguousarray(np.stack([w0, a0], 2))
    out["rwkv_lnT"] = np.ascontiguousarray(np.stack([t8(prm["ln_w"]), t8(prm["ln_b"])], 2))
    return out
```
